# Optimizing a Trainium2 kernel written in Bass

```python
import math
import jax, jax.numpy as jnp
from jax import lax
import numpy as np

D_MODEL = 2048
BATCH = 2
SEQ = 4096
DEPTH = 1

DN_HEADS = 8
DN_DK = 128
DN_DV = 128
DN_CONV = 4
DN_CHUNK = 64
RT_HEADS = 8
RT_DK = 64
RT_DV = 128
RT_CHUNK = 64
ROPE_BASE = 10000.0
DN_WIDTH = DN_HEADS * DN_DV
RT_WIDTH = RT_HEADS * RT_DV
MIX_WIDTH = DN_WIDTH + RT_WIDTH
DN_CONV_CH = 2 * DN_HEADS * DN_DK + DN_WIDTH
IN_SPLITS = (DN_HEADS * DN_DK, DN_HEADS * DN_DK, DN_WIDTH, DN_WIDTH, DN_HEADS, DN_HEADS,
             RT_HEADS * RT_DK, RT_HEADS * RT_DK, RT_WIDTH, RT_WIDTH)
IN_WIDTH = sum(IN_SPLITS)
N_EXPERTS = 64
TOP_K = 8
N_GROUPS = 8
TOPK_GROUPS = 4
EXPERT_FF = 512
SHARED_FF = 512
ROUTED_SCALE = 2.5
MOE_BLOCK = 128
NORM_EPS = 1e-6

kernel_name = "hybrid_deltanet_retention_moe_layer"


def rms_norm(x, w):
    xf = x.astype(jnp.float32)
    y = xf * lax.rsqrt(jnp.mean(xf * xf, axis=-1, keepdims=True) + NORM_EPS)
    return (y * w.astype(jnp.float32)).astype(x.dtype)


def l2_normalize(x):
    xf = x.astype(jnp.float32)
    return xf * lax.rsqrt(jnp.sum(xf * xf, axis=-1, keepdims=True) + NORM_EPS)


def head_group_norm(o, w):
    mu = jnp.mean(o, axis=-1, keepdims=True)
    var = jnp.mean(jnp.square(o - mu), axis=-1, keepdims=True)
    y = (o - mu) * lax.rsqrt(var + NORM_EPS)
    b, s, h, d = o.shape
    return y.reshape(b, s, h * d) * w.astype(jnp.float32)


def swiglu(x, w_gate, w_up, w_down):
    return (jax.nn.silu(x @ w_gate) * (x @ w_up)) @ w_down


def to_chunks(t, chunk):
    b, s = t.shape[:2]
    t = t.reshape((b, s // chunk, chunk) + t.shape[2:])
    return jnp.moveaxis(t, 2, 3)


def from_chunks(o):
    b, n, h, c, d = o.shape
    return jnp.moveaxis(o, 3, 2).reshape(b, n * c, h, d)


def causal_depthwise_conv(x, w):
    k, ch = w.shape
    return lax.conv_general_dilated(x, w[:, None, :].astype(x.dtype), window_strides=(1,),
                                    padding=[(k - 1, 0)], dimension_numbers=('NWC', 'WIO', 'NWC'),
                                    feature_group_count=ch)


def rotary(x, positions):
    half = x.shape[-1] // 2
    theta = 1.0 / (ROPE_BASE ** jnp.linspace(0.0, 1.0, half, dtype=jnp.float32))
    ang = positions.astype(jnp.float32)[..., None] * theta
    cos, sin = jnp.cos(ang)[:, :, None, :], jnp.sin(ang)[:, :, None, :]
    x1, x2 = x[..., :half], x[..., half:]
    return jnp.concatenate([x1 * cos - x2 * sin, x2 * cos + x1 * sin], axis=-1)


def gated_delta_rule_chunked(q, k, v, g, beta):
    b, s, h, dk = q.shape
    dv = v.shape[-1]
    c = DN_CHUNK
    qc = to_chunks(q * (dk ** -0.5), c)
    kc = to_chunks(k, c)
    vc = to_chunks(v, c)
    gc = jnp.cumsum(to_chunks(g, c), axis=-1)
    bc = to_chunks(beta, c)
    causal = jnp.tril(jnp.ones((c, c), dtype=bool))
    strict = jnp.tril(jnp.ones((c, c), dtype=bool), -1)
    diff = gc[..., :, None] - gc[..., None, :]
    decay = jnp.where(causal, jnp.exp(jnp.where(causal, diff, 0.0)), 0.0)
    kb = kc * bc[..., None]
    a_mat = jnp.where(strict, jnp.einsum('bnhcd,bnhsd->bnhcs', kb, kc) * decay, 0.0)
    t_mat = a_mat + jnp.eye(c, dtype=a_mat.dtype)
    rhs = jnp.concatenate([kb * jnp.exp(gc)[..., None], vc * bc[..., None]], axis=-1)
    sol = lax.linalg.triangular_solve(t_mat, rhs, left_side=True, lower=True, unit_diagonal=True)
    w_c, u_c = sol[..., :dk], sol[..., dk:]
    attn = jnp.where(causal, jnp.einsum('bnhcd,bnhsd->bnhcs', qc, kc) * decay, 0.0)
    g_last = gc[..., -1:]
    q_dec = qc * jnp.exp(gc)[..., None]
    k_dec = kc * jnp.exp(g_last - gc)[..., None]
    chunk_decay = jnp.exp(g_last[..., 0])
    xs = tuple(jnp.moveaxis(t, 1, 0) for t in (q_dec, k_dec, w_c, u_c, attn, chunk_decay))

    def step(state, inp):
        qd, kd, wc, uc, at, cd = inp
        v_new = uc - jnp.einsum('bhcd,bhde->bhce', wc, state)
        out = jnp.einsum('bhcd,bhde->bhce', qd, state) + jnp.einsum('bhcs,bhse->bhce', at, v_new)
        state = state * cd[..., None, None] + jnp.einsum('bhcd,bhce->bhde', kd, v_new)
        return state, out

    s0 = jnp.zeros((b, h, dk, dv), jnp.float32)
    _, o = lax.scan(step, s0, xs)
    return from_chunks(jnp.moveaxis(o, 0, 1))


def retention_chunked(q, k, v, log_gamma):
    c = RT_CHUNK
    qc, kc, vc = to_chunks(q, c), to_chunks(k, c), to_chunks(v, c)
    idx = jnp.arange(c, dtype=jnp.float32)
    causal = jnp.tril(jnp.ones((c, c), dtype=bool))
    rel = jnp.where(causal, idx[:, None] - idx[None, :], 0.0)
    d_intra = jnp.where(causal, jnp.exp(rel[None] * log_gamma[:, None, None]), 0.0)
    intra = jnp.einsum('bnhcs,bnhse->bnhce', jnp.einsum('bnhcd,bnhsd->bnhcs', qc, kc) * d_intra, vc)
    q_dec = qc * jnp.exp((idx + 1.0)[None, :] * log_gamma[:, None])[..., None]
    k_dec = kc * jnp.exp((c - 1.0 - idx)[None, :] * log_gamma[:, None])[..., None]
    chunk_decay = jnp.exp(c * log_gamma)
    xs = tuple(jnp.moveaxis(t, 1, 0) for t in (q_dec, k_dec, vc))

    def step(state, inp):
        qd, kd, vv = inp
        out = jnp.einsum('bhcd,bhde->bhce', qd, state)
        state = state * chunk_decay[:, None, None] + jnp.einsum('bhcd,bhce->bhde', kd, vv)
        return state, out

    b, h, dk, dv = q.shape[0], q.shape[2], q.shape[3], v.shape[3]
    _, inter = lax.scan(step, jnp.zeros((b, h, dk, dv), jnp.float32), xs)
    return from_chunks(intra + jnp.moveaxis(inter, 0, 1))


def hybrid_mixer(h, positions, log_gamma, w_in, conv_w, a_log, dt_bias, dn_norm_w, rt_norm_w, w_out):
    b, s, _ = h.shape
    f32 = jnp.float32
    proj = h @ w_in
    split_at = np.cumsum(IN_SPLITS)[:-1].tolist()
    dq, dk_, dv_, dz, da, db, rq, rk, rv, rg = jnp.split(proj, split_at, axis=-1)
    qkv = jax.nn.silu(causal_depthwise_conv(jnp.concatenate([dq, dk_, dv_], axis=-1), conv_w))
    dq, dk_, dv_ = jnp.split(qkv, [DN_HEADS * DN_DK, 2 * DN_HEADS * DN_DK], axis=-1)
    q_a = l2_normalize(dq.reshape(b, s, DN_HEADS, DN_DK))
    k_a = l2_normalize(dk_.reshape(b, s, DN_HEADS, DN_DK))
    v_a = dv_.reshape(b, s, DN_HEADS, DN_DV).astype(f32)
    beta = jax.nn.sigmoid(db.astype(f32))
    g = -jnp.exp(a_log.astype(f32)) * jax.nn.softplus(da.astype(f32) + dt_bias.astype(f32))
    o_a = gated_delta_rule_chunked(q_a, k_a, v_a, g, beta)
    o_a = rms_norm(o_a, dn_norm_w).reshape(b, s, DN_WIDTH) * jax.nn.silu(dz.astype(f32))
    q_b = rotary(rq.reshape(b, s, RT_HEADS, RT_DK).astype(f32), positions)
    k_b = rotary(rk.reshape(b, s, RT_HEADS, RT_DK).astype(f32), positions) * (RT_DK ** -0.5)
    v_b = rv.reshape(b, s, RT_HEADS, RT_DV).astype(f32)
    o_b = retention_chunked(q_b, k_b, v_b, log_gamma)
    o_b = head_group_norm(o_b, rt_norm_w) * jax.nn.silu(rg.astype(f32))
    o = jnp.concatenate([o_a, o_b], axis=-1).astype(h.dtype)
    return o @ w_out


def moe_ffn(h, w_router, router_bias, w_gate_exp, w_up_exp, w_down_exp, w_gate_sh, w_up_sh, w_down_sh):
    b, s, d = h.shape
    n_tok = b * s
    hf = h.reshape(n_tok, d)
    f32 = jnp.float32
    scores = jax.nn.sigmoid(hf.astype(f32) @ w_router.astype(f32))
    sel = scores + router_bias.astype(f32)
    grp = sel.reshape(n_tok, N_GROUPS, N_EXPERTS // N_GROUPS)
    grp_score = jnp.sum(lax.top_k(grp, 2)[0], axis=-1)
    _, g_idx = lax.top_k(grp_score, TOPK_GROUPS)
    g_keep = jnp.any(g_idx[:, :, None] == jnp.arange(N_GROUPS)[None, None, :], axis=1)
    sel = jnp.where(jnp.repeat(g_keep, N_EXPERTS // N_GROUPS, axis=1), sel, -jnp.inf)
    _, e_idx = lax.top_k(sel, TOP_K)
    wts = jnp.take_along_axis(scores, e_idx, axis=1)
    wts = wts / jnp.sum(wts, axis=-1, keepdims=True) * ROUTED_SCALE
    n_assign = n_tok * TOP_K
    flat_e = e_idx.reshape(n_assign)
    flat_tok = jnp.repeat(jnp.arange(n_tok, dtype=jnp.int32), TOP_K)
    flat_w = wts.reshape(n_assign)
    order = jnp.argsort(flat_e)
    se, stok, sw = flat_e[order], flat_tok[order], flat_w[order]
    counts = jnp.zeros((N_EXPERTS,), jnp.int32).at[flat_e].add(1)
    starts = jnp.cumsum(counts) - counts
    padded = (counts + MOE_BLOCK - 1) // MOE_BLOCK * MOE_BLOCK
    pad_end = jnp.cumsum(padded)
    pad_start = pad_end - padded
    dest = pad_start[se] + (jnp.arange(n_assign, dtype=jnp.int32) - starts[se])
    n_blocks = -(-n_assign // MOE_BLOCK) + N_EXPERTS
    n_rows = n_blocks * MOE_BLOCK
    row_tok = jnp.zeros((n_rows,), jnp.int32).at[dest].set(stok)
    row_w = jnp.zeros((n_rows,), f32).at[dest].set(sw)
    block_start = jnp.arange(n_blocks, dtype=jnp.int32) * MOE_BLOCK
    block_e = jnp.minimum(jnp.searchsorted(pad_end, block_start, side='right'), N_EXPERTS - 1)

    def expert_block(args):
        tok, wt, e = args
        y = swiglu(hf[tok], w_gate_exp[e], w_up_exp[e], w_down_exp[e])
        return y * wt[:, None].astype(y.dtype)

    ys = lax.map(expert_block, (row_tok.reshape(n_blocks, MOE_BLOCK), row_w.reshape(n_blocks, MOE_BLOCK), block_e))
    routed = jax.ops.segment_sum(ys.reshape(n_rows, d), row_tok, num_segments=n_tok)
    shared = swiglu(hf, w_gate_sh, w_up_sh, w_down_sh)
    return (routed + shared).reshape(b, s, d)


def setup_inputs(seed: int = 0) -> dict:
    key = jax.random.key(seed)
    ks = jax.random.split(key, 24)
    f32 = jnp.float32
    L, D = DEPTH, D_MODEL

    def nrm(k, shape, scale):
        return jax.random.normal(k, shape, f32) * scale

    def gain(k, shape):
        return 1.0 + 0.05 * jax.random.normal(k, shape, f32)

    x = nrm(ks[0], (BATCH, SEQ, D), 1.0)
    c = nrm(ks[1], (BATCH, D), 1.0)
    positions = (jnp.arange(SEQ, dtype=jnp.int32)[None, :]
                 + jax.random.randint(ks[2], (BATCH, 1), 0, 1024, dtype=jnp.int32))
    w_ada = nrm(ks[3], (L, D, 6 * D), 0.5 * D ** -0.5)
    b_ada = nrm(ks[4], (L, 6 * D), 0.02)
    pre_norm_mix = gain(ks[5], (L, D))
    post_norm_mix = gain(ks[6], (L, D))
    w_in = nrm(ks[7], (L, D, IN_WIDTH), D ** -0.5)
    conv_w = nrm(ks[8], (L, DN_CONV, DN_CONV_CH), DN_CONV ** -0.5)
    a_log = jnp.log(jax.random.uniform(ks[9], (L, DN_HEADS), f32, 1.0, 16.0))
    dt = jnp.exp(jax.random.uniform(ks[10], (L, DN_HEADS), f32, math.log(1e-3), math.log(1e-1)))
    dt_bias = dt + jnp.log(-jnp.expm1(-dt))
    dn_norm_w = gain(ks[11], (L, DN_DV))
    rt_norm_w = gain(ks[12], (L, RT_WIDTH))
    w_out = nrm(ks[13], (L, MIX_WIDTH, D), MIX_WIDTH ** -0.5)
    pre_norm_ffn = gain(ks[14], (L, D))
    post_norm_ffn = gain(ks[15], (L, D))
    w_router = nrm(ks[16], (L, D, N_EXPERTS), D ** -0.5)
    router_bias = nrm(ks[17], (L, N_EXPERTS), 0.01)
    w_gate_exp = nrm(ks[18], (L, N_EXPERTS, D, EXPERT_FF), D ** -0.5)
    w_up_exp = nrm(ks[19], (L, N_EXPERTS, D, EXPERT_FF), D ** -0.5)
    w_down_exp = nrm(ks[20], (L, N_EXPERTS, EXPERT_FF, D), EXPERT_FF ** -0.5)
    w_gate_sh = nrm(ks[21], (L, D, SHARED_FF), D ** -0.5)
    w_up_sh = nrm(ks[22], (L, D, SHARED_FF), D ** -0.5)
    w_down_sh = nrm(ks[23], (L, SHARED_FF, D), SHARED_FF ** -0.5)
    return {"x": x, "c": c, "positions": positions, "w_ada": w_ada, "b_ada": b_ada,
            "pre_norm_mix": pre_norm_mix, "post_norm_mix": post_norm_mix, "w_in": w_in,
            "conv_w": conv_w, "a_log": a_log, "dt_bias": dt_bias, "dn_norm_w": dn_norm_w,
            "rt_norm_w": rt_norm_w, "w_out": w_out, "pre_norm_ffn": pre_norm_ffn,
            "post_norm_ffn": post_norm_ffn, "w_router": w_router, "router_bias": router_bias,
            "w_gate_exp": w_gate_exp, "w_up_exp": w_up_exp, "w_down_exp": w_down_exp,
            "w_gate_sh": w_gate_sh, "w_up_sh": w_up_sh, "w_down_sh": w_down_sh}


def reference(x, c, positions, w_ada, b_ada, pre_norm_mix, post_norm_mix, w_in, conv_w, a_log,
              dt_bias, dn_norm_w, rt_norm_w, w_out, pre_norm_ffn, post_norm_ffn, w_router,
              router_bias, w_gate_exp, w_up_exp, w_down_exp, w_gate_sh, w_up_sh, w_down_sh):
    log_gamma = jnp.log(1.0 - 2.0 ** (-5.0 - jnp.arange(RT_HEADS, dtype=jnp.float32)))
    c_act = jax.nn.silu(c)
    for l in range(DEPTH):
        mod = (c_act @ w_ada[l] + b_ada[l])[:, None, :]
        shift_m, scale_m, gate_m, shift_f, scale_f, gate_f = jnp.split(mod, 6, axis=-1)
        h = rms_norm(x, pre_norm_mix[l]) * (1.0 + scale_m) + shift_m
        y = hybrid_mixer(h, positions, log_gamma, w_in[l], conv_w[l], a_log[l], dt_bias[l],
                         dn_norm_w[l], rt_norm_w[l], w_out[l])
        x = x + gate_m * rms_norm(y, post_norm_mix[l])
        h = rms_norm(x, pre_norm_ffn[l]) * (1.0 + scale_f) + shift_f
        y = moe_ffn(h, w_router[l], router_bias[l], w_gate_exp[l], w_up_exp[l], w_down_exp[l],
                    w_gate_sh[l], w_up_sh[l], w_down_sh[l])
        x = x + gate_f * rms_norm(y, post_norm_ffn[l])
    return x
```

```python
import numpy as np
import concourse.bass as bass
import concourse.mybir as mybir
from concourse.bass_utils import run_bass_kernel_spmd
from contextlib import ExitStack

F32 = mybir.dt.float32
BF16 = mybir.dt.bfloat16
I32 = mybir.dt.int32
ALU = mybir.AluOpType
AF = mybir.ActivationFunctionType
AX = mybir.AxisListType

D = 2048
S = 4096
NT = 32
OWN = 1024
NKC = 16
EPS = 1e-6
DBG = None
DBG_CORES = None
DN_TILES = NT
DN_DUMP = False
DN_STAGE = 6
DN_SUB = 0
MOE_EXPERTS = None


class Res:
    __slots__ = ("name", "w", "r", "excl")

    def __init__(self, name="", excl=False):
        self.name = name
        self.w = None
        self.r = []
        self.excl = excl


class Prog:
    COMPUTE = ("pe", "act", "dve", "pool")
    ENG = ("pe", "act", "dve", "pool", "sp")
    ND = 48

    def __init__(self, nc, es):
        self.nc = nc
        self.es = es
        self.ops = {e: [] for e in self.ENG}
        self.cnt = {e: 0 for e in self.COMPUTE}
        self.sems = {}
        for e in self.COMPUTE:
            self.sems[e] = es.enter_context(nc.semaphore("cs_" + e))
        for i in range(self.ND):
            self.sems[("d", i)] = es.enter_context(nc.semaphore("ds%d" % i))
        self.duse = [0] * self.ND
        self.dnext = 0
        self.waited = {e: {} for e in self.ENG}
        self.final = []
        self.nops = 0

    def _wait(self, eng, tok):
        key, val = tok
        if key == eng and eng == "pe":
            return
        if self.waited[eng].get(key, 0) >= val:
            return
        self.waited[eng][key] = val
        self.ops[eng].append(("w", key, val))

    def op(self, eng, fn, reads=(), writes=(), dma=False, final=False):
        for r in reads:
            if r.w is not None:
                self._wait(eng, r.w)
            if r.excl:
                for t in r.r:
                    if t[0] != eng:
                        self._wait(eng, t)
        for w in writes:
            if w.w is not None:
                self._wait(eng, w.w)
            for t in w.r:
                self._wait(eng, t)
        if dma:
            s = self.dnext % self.ND
            self.dnext += 1
            u = self.duse[s]
            if u > 0:
                self._wait(eng, (("d", s), 16 * u))
            self.duse[s] = u + 1
            tok = (("d", s), 16 * (u + 1))
            self.ops[eng].append(("i", fn, ("d", s), 16))
        else:
            self.cnt[eng] += 1
            tok = (eng, self.cnt[eng])
            self.ops[eng].append(("i", fn, eng, 1))
        for r in reads:
            r.r.append(tok)
            if len(r.r) > 24:
                r.r = r.r[-24:] if False else r.r
        for w in writes:
            w.w = tok
            w.r = []
        if final:
            self.final.append(tok)
        self.nops += 1
        return tok

    def dma(self, eng, out, in_, reads=(), writes=(), final=False, **kw):
        return self.op(eng, lambda e: e.dma_start(out=out, in_=in_, **kw), reads, writes, dma=True, final=final)

    def barrier(self):
        toks = [(e, self.cnt[e]) for e in self.COMPUTE if self.cnt[e] > 0]
        toks += [(("d", s), 16 * self.duse[s]) for s in range(self.ND) if self.duse[s] > 0]
        for e in self.ENG:
            for t in toks:
                self._wait(e, t)

    def run(self):
        for t in self.final:
            self._wait("sp", t)
        nc = self.nc
        sems = self.sems

        def replay(eng_name):
            def body(e):
                for o in self.ops[eng_name]:
                    if o[0] == "w":
                        e.wait_ge(sems[o[1]], o[2])
                    else:
                        o[1](e).then_inc(sems[o[2]], o[3])
            return body

        with nc.Block() as block:
            block.tensor(replay("pe"))
            block.scalar(replay("act"))
            block.vector(replay("dve"))
            block.gpsimd(replay("pool"))
            block.sync(replay("sp"))


class Arena:
    def __init__(self, nc, es, words):
        self.t = es.enter_context(nc.sbuf_tensor("arena", [128, words], F32))
        self.words = words
        self.off = 0

    def alloc(self, free, dtype=F32, parts=128):
        n = 1
        for f in free:
            n *= f
        w = n if dtype != BF16 else (n + 1) // 2
        w = (w + 7) // 8 * 8
        assert self.off + w <= self.words, ("arena overflow", self.off, w, self.words)
        ap = self.t[0:parts, self.off:self.off + w]
        self.off += w
        if dtype == BF16:
            ap = ap.bitcast(BF16)
        elif dtype == I32:
            ap = ap.bitcast(I32)
        ap = ap[:, 0:n]
        if len(free) == 2:
            ap = ap.rearrange("p (a b) -> p a b", a=free[0], b=free[1])
        elif len(free) == 3:
            ap = ap.rearrange("p (a b c) -> p a b c", a=free[0], b=free[1], c=free[2])
        return ap

    def mark(self):
        return self.off

    def release(self, m):
        self.off = m


def bc(ap, shape):
    return ap.to_broadcast(list(shape))


def build_program(dbg=None):
    nc = bass.Bass("TRN2", target_bir_lowering=False)
    dbg_out = {}

    def din(name, shape, dt=F32):
        return nc.dram_tensor(name, list(shape), dt, kind="ExternalInput").ap()

    def scratch(name, shape, dt=F32, dump=False):
        kind = "ExternalOutput" if (dbg is not None and dump) else "Internal"
        t = nc.dram_tensor(name, list(shape), dt, kind=kind).ap()
        if kind == "ExternalOutput":
            dbg_out[name] = t
        return t

    xb = din("xb", [S, D])
    xo = din("xo", [OWN, D])
    cT = din("cT", [128, NKC])
    pos = din("pos", [128, NT], I32)
    w_ada = din("w_ada", [D, 6 * D])
    b_ada = din("b_ada", [1, 6 * D])
    g_pre1 = din("g_pre1", [128, NKC])
    g_pre2 = din("g_pre2", [128, NKC])
    g_post1 = din("g_post1", [1, D])
    g_post2 = din("g_post2", [1, D])
    w_in = din("w_in", [D, 7184])
    conv_w = din("conv_w", [128, 24, 4])
    a_log = din("a_log", [1, 8])
    dt_bias = din("dt_bias", [1, 8])
    dn_norm_w = din("dn_norm_w", [1, 128])
    rt_norm_w = din("rt_norm_w", [1, 1024])
    w_out = din("w_out", [D, D])
    w_router = din("w_router", [D, 64])
    router_bias = din("router_bias", [1, 64])
    if dbg in (None, "moe"):
        w_gate = din("w_gate", [65, D, 512])
        w_up = din("w_up", [65, D, 512])
        w_down = din("w_down", [65, 512, D])
    own_idx = din("own_idx", [128, 8], I32)
    c_ident = din("c_ident", [128, 128])
    c_utri = din("c_utri", [128, 128])
    c_maskS = din("c_maskS", [128, 128])
    c_maskT = din("c_maskT", [128, 128])
    c_onehot8 = din("c_onehot8", [128, 8, 128])
    c_rtmask = din("c_rtmask", [128, 8, 128])
    c_gq = din("c_gq", [64, 8, 128])
    c_gk = din("c_gk", [128, 8])
    c_gC = din("c_gC", [64, 8])
    c_theta = din("c_theta", [1, 32])
    c_mBD = din("c_mBD", [2, 128, 128])
    c_mC = din("c_mC", [8, 128, 128])

    out = nc.dram_tensor("out", [OWN, D], F32, kind="ExternalOutput").ap()

    mod_scr = scratch("mod_scr", [1, 6 * D], dump=True)
    pFM = scratch("pFM", [24, 128, S], dump=(dbg == "a1"))
    pTM = scratch("pTM", [S, 4112], dump=(dbg == "a1"))
    dn_qT = scratch("dn_qT", [8, 128, S], BF16)
    dn_kT = scratch("dn_kT", [8, 128, S], BF16)
    dn_ktm = scratch("dn_ktm", [S, 8, 128], BF16)
    dn_vtm = scratch("dn_vtm", [S, 8, 128], BF16)
    o_scr = scratch("o_scr", [S, D], F32, dump=(dbg in ("rt", "dn")))
    x1_scr = scratch("x1_scr", [OWN, D], dump=(dbg == "b"))
    dbg_a = scratch("dbg_a", [128, 2048], dump=True)

    with ExitStack() as es:
        P = Prog(nc, es)
        A = Arena(nc, es, 50 * 1024)
        psum = [es.enter_context(nc.psum_tensor("ps%d" % i, [128, 1024], F32)) for i in range(4)]
        PR = [[Res("ps%d_%d" % (i, k), excl=True) for k in range(2)] for i in range(4)]

        def pbank(i, k):
            return psum[i][:, k * 512:(k + 1) * 512]

        ident_f = A.alloc((128,))
        ident_b = A.alloc((128,), BF16)
        utri = A.alloc((128,))
        ones_f = A.alloc((128,))
        ones_b = A.alloc((128,), BF16)
        R_const = Res("const")
        P.dma("sp", ident_f, c_ident[:, :], writes=[R_const])
        P.dma("sp", utri, c_utri[:, :], writes=[R_const])
        R_c2 = Res("c2")
        P.op("act", lambda e: e.copy(ident_b, ident_f), [R_const], [R_c2])
        P.op("pool", lambda e: e.memset(ones_f, 1.0), [], [R_c2])
        P.op("pool", lambda e: e.memset(ones_b, 1.0), [], [R_c2])
        epsc = A.alloc((8,))
        P.op("pool", lambda e: e.memset(epsc, EPS), [], [R_c2])

        base_mark = A.mark()

        cTt = A.alloc((NKC,))
        cTb = A.alloc((NKC,), BF16)
        R_c = Res("c")
        P.dma("sp", cTt, cT[:, :], writes=[R_c])
        P.op("act", lambda e: e.activation(cTb, cTt, AF.Silu), [R_c], [R_c])
        wab = [A.alloc((NKC, 512), BF16) for _ in range(2)]
        R_wab = [Res("wab0"), Res("wab1")]
        modrow = A.alloc((6 * D,), parts=1)
        badar = A.alloc((6 * D,), parts=1)
        R_mod = Res("modrow")
        R_bada = Res("bada")
        P.dma("sp", badar, b_ada[:, :], writes=[R_bada])
        w_ada_v = w_ada.rearrange("(c p) n -> p c n", p=128)
        for jb in range(24):
            wb = wab[jb % 2]
            rw = R_wab[jb % 2]
            P.dma("pool", wb, w_ada_v[:, :, jb * 512:(jb + 1) * 512], writes=[rw])
            pi, pk = (jb % 4) // 2, jb % 2
            for kc in range(NKC):
                P.op("pe", lambda e, kc=kc, wb=wb, pi=pi, pk=pk: e.matmul(
                    pbank(pi, pk)[0:1, :], cTb[:, kc:kc + 1], wb[:, kc, :], start=(kc == 0), stop=(kc == NKC - 1)),
                    [R_c, rw], [PR[pi][pk]])
            P.op("dve", lambda e, jb=jb, pi=pi, pk=pk: e.tensor_tensor(
                modrow[:, jb * 512:(jb + 1) * 512], pbank(pi, pk)[0:1, :], badar[:, jb * 512:(jb + 1) * 512], ALU.add),
                [PR[pi][pk], R_bada], [R_mod])
        R_modscr = Res("modscr")
        P.dma("sp", mod_scr[:, :], modrow, reads=[R_mod], writes=[R_modscr])
        P.barrier()
        A.release(base_mark)

        modfm = A.alloc((96,))
        R_modfm = Res("modfm")
        P.dma("sp", modfm, mod_scr.rearrange("o (c p) -> p (o c)", p=128), reads=[R_modscr], writes=[R_modfm],
              allow_slow_non_contiguous=True)
        gp1 = A.alloc((NKC,))
        gp2 = A.alloc((NKC,))
        P.dma("sp", gp1, g_pre1[:, :], writes=[R_modfm])
        P.dma("sp", gp2, g_pre2[:, :], writes=[R_modfm])
        a1 = A.alloc((NKC,))
        a2 = A.alloc((NKC,))
        R_a = Res("a12")
        P.op("dve", lambda e: e.scalar_tensor_tensor(a1, modfm[:, 16:32], 1.0, gp1, ALU.add, ALU.mult), [R_modfm], [R_a])
        P.op("dve", lambda e: e.scalar_tensor_tensor(a2, modfm[:, 64:80], 1.0, gp2, ALU.add, ALU.mult), [R_modfm], [R_a])
        s1 = modfm[:, 0:16]
        s2 = modfm[:, 48:64]
        const_mark = A.mark()

        hT = A.alloc((NKC, S), BF16)
        R_hT = [Res("hT%d" % t) for t in range(NT)]
        hT_mark = A.mark()
        xt = [A.alloc((D,)) for _ in range(2)]
        R_xt = [Res("xt0"), Res("xt1")]
        sqj = A.alloc((D,), BF16)
        R_sqj = Res("sqj")
        xn = [A.alloc((D,), BF16) for _ in range(2)]
        R_xn = [Res("xn0"), Res("xn1")]
        ss = A.alloc((2, 2))
        R_ss = [Res("ss0"), Res("ss1")]
        tmpT = A.alloc((D,))
        R_tmpT = Res("tmpT")

        def rms_rstd(eng_sq, src, rss, ssap, reads):
            P.op("act", lambda e: e.activation(sqj, src, AF.Square, accum_out=ssap[:, 0:1]), reads, [R_sqj, rss])
            P.op("act", lambda e: e.activation(ssap[:, 1:2], ssap[:, 0:1], AF.Sqrt, bias=epsc[:, 0:1], scale=1.0 / D), [rss, R_c2], [rss])
            P.op("dve", lambda e: e.reciprocal(ssap[:, 0:1], ssap[:, 1:2]), [rss], [rss])

        for t in range(NT):
            x_ = xt[t % 2]
            rx = R_xt[t % 2]
            ssap = ss[:, t % 2, :]
            P.dma("sp", x_, xb[t * 128:(t + 1) * 128, :], writes=[rx])
            P.op("pool", lambda e, ssap=ssap: e.memset(ssap, 0.0), [], [R_ss[t % 2]])
            rms_rstd("act", x_, R_ss[t % 2], ssap, [rx])
            xn_ = xn[t % 2]
            P.op("dve", lambda e, x_=x_, xn_=xn_, ssap=ssap: e.tensor_scalar(xn_, x_, ssap[:, 0:1], None, ALU.mult),
                 [rx, R_ss[t % 2]], [R_xn[t % 2]])
            for half in range(2):
                pv = psum[half][:, :].bitcast(BF16)
                for q in range(8):
                    kc = half * 8 + q
                    P.op("pe", lambda e, pv=pv, q=q, kc=kc, xn_=xn_: e.transpose(
                        pv[:, q * 128:(q + 1) * 128], xn_[:, kc * 128:(kc + 1) * 128], ident_b),
                        [R_xn[t % 2], R_c2], [PR[half][0]])
                pvv = pv[:, 0:1024].rearrange("p (c n) -> p c n", c=8)
                tv = tmpT[:, half * 1024:(half + 1) * 1024].rearrange("p (c n) -> p c n", c=8)
                P.op("dve", lambda e, pvv=pvv, tv=tv, half=half: e.tensor_tensor(
                    tv, pvv, bc(a1[:, half * 8:(half + 1) * 8].unsqueeze(2), (128, 8, 128)), ALU.mult),
                    [PR[half][0], R_a], [R_tmpT])
                P.op("pool", lambda e, tv=tv, half=half, t=t: e.tensor_tensor(
                    hT[:, half * 8:(half + 1) * 8, t * 128:(t + 1) * 128], tv,
                    bc(s1[:, half * 8:(half + 1) * 8].unsqueeze(2), (128, 8, 128)), ALU.add),
                    [R_tmpT, R_modfm], [R_hT[t]])

        P.barrier()
        A.release(hT_mark)
        wfm = [A.alloc((NKC, 128), BF16) for _ in range(2)]
        R_wfm = [Res("wfm0"), Res("wfm1")]
        stg = [A.alloc((1024,)) for _ in range(3)]
        R_stg = [Res("stg%d" % i) for i in range(3)]
        gcnt = 0
        w_in_v = w_in.rearrange("(c p) n -> p c n", p=128)
        R_pFM = [Res("pFM%d" % c) for c in range(24)]
        pcnt = 0
        for cc in range(24):
            wt = wfm[cc % 2]
            rw = R_wfm[cc % 2]
            P.dma("pool", wt, w_in_v[:, :, cc * 128:(cc + 1) * 128], writes=[rw])
            for tb in range(8):
                if tb % 2 == 0:
                    st = stg[gcnt % 3]
                    rs = R_stg[gcnt % 3]
                    gcnt += 1
                pi, pk = (pcnt % 4) // 2 + 2, pcnt % 2
                pcnt += 1
                for kc in range(NKC):
                    P.op("pe", lambda e, kc=kc, wt=wt, tb=tb, pi=pi, pk=pk: e.matmul(
                        pbank(pi, pk), wt[:, kc, :], hT[:, kc, tb * 512:(tb + 1) * 512], start=(kc == 0), stop=(kc == NKC - 1)),
                        [rw] + R_hT[tb * 4:(tb + 1) * 4], [PR[pi][pk]])
                P.op("act", lambda e, st=st, tb=tb, pi=pi, pk=pk: e.copy(st[:, (tb % 2) * 512:(tb % 2 + 1) * 512], pbank(pi, pk)),
                     [PR[pi][pk]], [rs])
                if tb % 2 == 1:
                    P.dma("sp", pFM[cc][:, (tb - 1) * 512:(tb + 1) * 512], st, reads=[rs], writes=[R_pFM[cc]])
        P.barrier()
        A.release(hT_mark)
        wtm = [A.alloc((NKC, 512), BF16) for _ in range(2)]
        R_wtm = [Res("wtm0"), Res("wtm1")]
        R_pTM = Res("pTM")
        stq = [A.alloc((512,)) for _ in range(4)]
        R_stq = [Res("stq%d" % i) for i in range(4)]
        scnt = 0
        for cb in range(9):
            ncol = 512 if cb < 8 else 16
            c0 = 3072 + cb * 512
            wt = wtm[cb % 2]
            rw = R_wtm[cb % 2]
            P.dma("pool", wt[:, :, 0:ncol], w_in_v[:, :, c0:c0 + ncol], writes=[rw])
            for t in range(NT):
                pi, pk = (pcnt % 4) // 2 + 2, pcnt % 2
                pcnt += 1
                for kc in range(NKC):
                    P.op("pe", lambda e, kc=kc, wt=wt, t=t, pi=pi, pk=pk, ncol=ncol: e.matmul(
                        pbank(pi, pk)[:, 0:ncol], hT[:, kc, t * 128:(t + 1) * 128], wt[:, kc, 0:ncol],
                        start=(kc == 0), stop=(kc == NKC - 1)),
                        [rw, R_hT[t]], [PR[pi][pk]])
                sq_ = stq[scnt % 4]
                rq = R_stq[scnt % 4]
                scnt += 1
                P.op("act" if t % 2 == 0 else "dve", lambda e, sq_=sq_, pi=pi, pk=pk, ncol=ncol, t=t: (
                    e.copy(sq_[:, 0:ncol], pbank(pi, pk)[:, 0:ncol]) if t % 2 == 0 else
                    e.tensor_copy(sq_[:, 0:ncol], pbank(pi, pk)[:, 0:ncol])),
                    [PR[pi][pk]], [rq])
                P.dma("sp", pTM[t * 128:(t + 1) * 128, cb * 512:cb * 512 + ncol], sq_[:, 0:ncol], reads=[rq], writes=[R_pTM])
        P.barrier()
        A.release(const_mark)
        if dbg == "a1":
            P.dma("sp", out[0:128, 0:1024], hT[:, 0, 0:2048].bitcast(F32), final=True)
            P.run()
            return nc, dbg_out


        R_oscr = Res("oscr")
        rt_mark = A.mark()
        theta_bc = A.alloc((32,))
        posi = A.alloc((NT,), I32)
        posf = A.alloc((NT,))
        ang = A.alloc((NT, 32))
        tmpa = A.alloc((NT, 32))
        sinT = A.alloc((NT, 32))
        cosT = A.alloc((NT, 32))
        R_tab = Res("rt_tab")
        P.dma("sp", theta_bc, c_theta.partition_broadcast(128), writes=[R_tab])
        P.dma("sp", posi, pos[:, :], writes=[R_tab])
        P.op("dve", lambda e: e.tensor_copy(posf, posi), [R_tab], [R_tab])
        P.op("dve", lambda e: e.tensor_tensor(ang, bc(posf.unsqueeze(2), (128, NT, 32)),
                                              bc(theta_bc.unsqueeze(1), (128, NT, 32)), ALU.mult), [R_tab], [R_tab])
        TWO_PI = 2.0 * np.pi
        pic = A.alloc((8,))
        P.op("pool", lambda e: e.memset(pic, -np.pi), [], [R_tab])
        ki = A.alloc((NT, 32), I32)
        kf = A.alloc((NT, 32))

        def sin_table(dst, shift):
            P.op("dve", lambda e: e.tensor_scalar(tmpa, ang, shift, 1.0 / TWO_PI, ALU.add, ALU.mult), [R_tab], [R_tab])
            P.op("dve", lambda e: e.tensor_copy(ki, tmpa), [R_tab], [R_tab])
            P.op("dve", lambda e: e.tensor_copy(kf, ki), [R_tab], [R_tab])
            P.op("dve", lambda e: e.tensor_scalar(tmpa, ang, shift, None, ALU.add), [R_tab], [R_tab])
            P.op("dve", lambda e: e.scalar_tensor_tensor(tmpa, kf, -TWO_PI, tmpa, ALU.mult, ALU.add), [R_tab], [R_tab])
            P.op("dve", lambda e: e.tensor_scalar(kf, tmpa, np.pi, TWO_PI, ALU.is_gt, ALU.mult), [R_tab], [R_tab])
            P.op("dve", lambda e: e.tensor_tensor(tmpa, tmpa, kf, ALU.subtract), [R_tab], [R_tab])
            P.op("dve", lambda e: e.tensor_scalar(kf, tmpa, -np.pi, TWO_PI, ALU.is_lt, ALU.mult), [R_tab], [R_tab])
            P.op("dve", lambda e: e.tensor_tensor(tmpa, tmpa, kf, ALU.add), [R_tab], [R_tab])
            P.op("act", lambda e: e.activation(dst, tmpa, AF.Sin), [R_tab], [R_tab])

        sin_table(sinT, 0.0)
        sin_table(cosT, 0.5 * np.pi)
        rtmask = A.alloc((8, 128))
        gq = A.alloc((8, 128), parts=64)
        gk = A.alloc((8,))
        gC = A.alloc((8,), parts=64)
        rtw = A.alloc((1024,))
        P.dma("sp", rtmask, c_rtmask[:, :, :], writes=[R_tab])
        P.dma("sp", gq, c_gq[:, :, :], writes=[R_tab])
        P.dma("sp", gk, c_gk[:, :], writes=[R_tab])
        P.dma("sp", gC, c_gC[:, :], writes=[R_tab])
        P.dma("sp", rtw, rt_norm_w.partition_broadcast(128), writes=[R_tab])
        Sst = A.alloc((8, 128), parts=64)
        Sbf = A.alloc((8, 128), BF16, parts=64)
        R_S = Res("S")
        R_Sbf = Res("Sbf")
        P.op("pool", lambda e: e.memset(Sst, 0.0), [], [R_S])
        P.op("pool", lambda e: e.memset(Sbf, 0.0), [], [R_Sbf])
        ld = [A.alloc((3072,)) for _ in range(2)]
        R_ld = [Res("ld0"), Res("ld1")]
        qr = A.alloc((8, 64), BF16)
        kr = A.alloc((8, 64), BF16)
        kd = A.alloc((8, 64), BF16)
        vb = A.alloc((8, 128), BF16)
        sgt = A.alloc((1024,))
        ta = A.alloc((8, 32))
        tb_ = A.alloc((8, 32))
        qT = A.alloc((8, 128), BF16, parts=64)
        qdT = A.alloc((8, 128), BF16, parts=64)
        kT = A.alloc((8, 128), BF16, parts=64)
        PT = A.alloc((8, 128), BF16)
        osb = A.alloc((8, 128))
        sqb = A.alloc((8, 128))
        st8 = A.alloc((6, 8))
        R_qr, R_kr, R_kd, R_vb, R_sg, R_ta, R_tb, R_qT, R_qdT, R_kT, R_PT, R_osb, R_sqb, R_st8 = [Res() for _ in range(14)]

        def rotary(src, dst, rdst, t, rl):
            x1 = src[:, :, 0:32]
            x2 = src[:, :, 32:64]
            cs = bc(cosT[:, t, :].unsqueeze(1), (128, 8, 32))
            sn = bc(sinT[:, t, :].unsqueeze(1), (128, 8, 32))
            P.op("dve", lambda e: e.tensor_tensor(ta, x1, cs, ALU.mult), [rl, R_tab], [R_ta])
            P.op("pool", lambda e: e.tensor_tensor(tb_, x2, sn, ALU.mult), [rl, R_tab], [R_tb])
            P.op("dve", lambda e: e.tensor_tensor(dst[:, :, 0:32], ta, tb_, ALU.subtract), [R_ta, R_tb], [rdst])
            P.op("dve", lambda e: e.tensor_tensor(ta, x2, cs, ALU.mult), [rl, R_tab], [R_ta])
            P.op("pool", lambda e: e.tensor_tensor(tb_, x1, sn, ALU.mult), [rl, R_tab], [R_tb])
            P.op("dve", lambda e: e.tensor_tensor(dst[:, :, 32:64], ta, tb_, ALU.add), [R_ta, R_tb], [rdst])

        for t in range(NT):
            l_ = ld[t % 2]
            rl = R_ld[t % 2]
            P.dma("sp", l_, pTM[t * 128:(t + 1) * 128, 1024:4096], reads=[R_pTM], writes=[rl])
            qv = l_[:, 0:512].rearrange("p (h d) -> p h d", h=8)
            kv = l_[:, 512:1024].rearrange("p (h d) -> p h d", h=8)
            vv = l_[:, 1024:2048].rearrange("p (h d) -> p h d", h=8)
            gv = l_[:, 2048:3072]
            rotary(qv, qr, R_qr, t, rl)
            rotary(kv, kr, R_kr, t, rl)
            P.op("pool", lambda e: e.tensor_tensor(kd, kr, bc(gk.unsqueeze(2), (128, 8, 64)), ALU.mult), [R_kr, R_tab], [R_kd])
            P.op("act", lambda e, vv=vv: e.copy(vb, vv), [rl], [R_vb])
            P.op("act", lambda e, gv=gv: e.activation(sgt, gv, AF.Silu), [rl], [R_sg])
            pq = psum[0][:, 0:512].bitcast(BF16)
            pk_ = psum[0][:, 512:1024].bitcast(BF16)
            for h in range(8):
                P.op("pe", lambda e, h=h: e.transpose(pq[0:64, h * 128:(h + 1) * 128], qr[:, h, :], ident_b), [R_qr, R_c2], [PR[0][0]])
            for h in range(8):
                P.op("pe", lambda e, h=h: e.transpose(pk_[0:64, h * 128:(h + 1) * 128], kr[:, h, :], ident_b), [R_kr, R_c2], [PR[0][1]])
            pq3 = pq[0:64, :].rearrange("p (h n) -> p h n", h=8)
            pk3 = pk_[0:64, :].rearrange("p (h n) -> p h n", h=8)
            P.op("act", lambda e: e.copy(qT, pq3), [PR[0][0]], [R_qT])
            P.op("dve", lambda e: e.tensor_tensor(qdT, pq3, gq, ALU.mult), [PR[0][0], R_tab], [R_qdT])
            P.op("act", lambda e: e.copy(kT, pk3), [PR[0][1]], [R_kT])
            for h in range(8):
                P.op("pe", lambda e, h=h: e.matmul(psum[1][:, h * 128:(h + 1) * 128], kT[:, h, :], qT[:, h, :], start=True, stop=True),
                     [R_kT, R_qT], [PR[1][h // 4]])
            for k in range(2):
                P.op("dve", lambda e, k=k: e.tensor_tensor(
                    PT[:, k * 4:(k + 1) * 4, :], psum[1][:, k * 512:(k + 1) * 512].rearrange("p (h n) -> p h n", h=4),
                    rtmask[:, k * 4:(k + 1) * 4, :], ALU.mult), [PR[1][k], R_tab], [R_PT])
            for h in range(8):
                P.op("pe", lambda e, h=h: e.matmul(psum[2][:, h * 128:(h + 1) * 128], PT[:, h, :], vb[:, h, :], start=True, stop=False),
                     [R_PT, R_vb], [PR[2][h // 4]])
                P.op("pe", lambda e, h=h: e.matmul(psum[2][:, h * 128:(h + 1) * 128], qdT[:, h, :], Sbf[:, h, :], start=False, stop=True),
                     [R_qdT, R_Sbf], [PR[2][h // 4]])
            for h in range(8):
                P.op("pe", lambda e, h=h: e.matmul(psum[3][0:64, h * 128:(h + 1) * 128], kd[:, h, :], vb[:, h, :], start=True, stop=True),
                     [R_kd, R_vb], [PR[3][h // 4]])
            P.op("dve", lambda e: e.tensor_tensor(Sst, Sst, bc(gC.unsqueeze(2), (64, 8, 128)), ALU.mult), [R_tab], [R_S])
            for k in range(2):
                P.op("dve", lambda e, k=k: e.tensor_tensor(
                    Sst[:, k * 4:(k + 1) * 4, :], Sst[:, k * 4:(k + 1) * 4, :],
                    psum[3][0:64, k * 512:(k + 1) * 512].rearrange("p (h n) -> p h n", h=4), ALU.add), [PR[3][k]], [R_S])
            P.op("act", lambda e: e.copy(Sbf, Sst), [R_S], [R_Sbf])
            for k in range(2):
                P.op("act", lambda e, k=k: e.copy(osb[:, k * 4:(k + 1) * 4, :],
                                                  psum[2][:, k * 512:(k + 1) * 512].rearrange("p (h n) -> p h n", h=4)),
                     [PR[2][k]], [R_osb])
            P.op("dve", lambda e: e.tensor_reduce(st8[:, 0, :], osb, AX.X, ALU.add), [R_osb], [R_st8])
            P.op("pool", lambda e: e.tensor_tensor(sqb, osb, osb, ALU.mult), [R_osb], [R_sqb])
            P.op("dve", lambda e: e.tensor_reduce(st8[:, 1, :], sqb, AX.X, ALU.add), [R_sqb], [R_st8])
            P.op("dve", lambda e: e.tensor_scalar(st8[:, 2, :], st8[:, 0, :], 1.0 / 128, None, ALU.mult), [R_st8], [R_st8])
            P.op("dve", lambda e: e.tensor_tensor(st8[:, 3, :], st8[:, 2, :], st8[:, 2, :], ALU.mult), [R_st8], [R_st8])
            P.op("dve", lambda e: e.scalar_tensor_tensor(st8[:, 4, :], st8[:, 1, :], 1.0 / 128, st8[:, 3, :], ALU.mult, ALU.subtract),
                 [R_st8], [R_st8])
            P.op("act", lambda e: e.activation(st8[:, 5, :], st8[:, 4, :], AF.Sqrt, bias=epsc[:, 0:1], scale=1.0), [R_st8, R_c2], [R_st8])
            P.op("dve", lambda e: e.reciprocal(st8[:, 4, :], st8[:, 5, :]), [R_st8], [R_st8])
            P.op("dve", lambda e: e.tensor_tensor(osb, osb, bc(st8[:, 2, :].unsqueeze(2), (128, 8, 128)), ALU.subtract), [R_st8], [R_osb])
            P.op("dve", lambda e: e.tensor_tensor(osb, osb, bc(st8[:, 4, :].unsqueeze(2), (128, 8, 128)), ALU.mult), [R_st8], [R_osb])
            osf = osb.rearrange("p h n -> p (h n)")
            P.op("pool", lambda e, osf=osf: e.tensor_tensor(osf, osf, rtw, ALU.mult), [R_tab], [R_osb])
            P.op("pool", lambda e, osf=osf: e.tensor_tensor(sqb.rearrange("p h n -> p (h n)"), osf, sgt, ALU.mult), [R_osb, R_sg], [R_sqb])
            P.dma("sp", o_scr[t * 128:(t + 1) * 128, 1024:2048], sqb.rearrange("p h n -> p (h n)"), reads=[R_sqb], writes=[R_oscr])
        P.barrier()
        A.release(rt_mark)
        if dbg == "rt":
            P.dma("sp", out[0:128, 0:1024], rtw, final=True)
            P.run()
            return nc, dbg_out


        dn_mark = A.mark()
        R_dt = Res("dn_tab")
        ab = A.alloc((NT, 16))
        for q4 in range(4):
            P.dma("sp", ab[:, q4 * 8:(q4 + 1) * 8, :], pTM[q4 * 1024:(q4 + 1) * 1024, 4096:4112].rearrange("(t p) c -> p t c", p=128),
                  reads=[R_pTM], writes=[R_dt])
        dtb = A.alloc((8,))
        alg = A.alloc((8,))
        P.dma("sp", dtb, dt_bias.partition_broadcast(128), writes=[R_dt])
        P.dma("sp", alg, a_log.partition_broadcast(128), writes=[R_dt])
        maskS = A.alloc((128,))
        maskT = A.alloc((128,))
        oh8 = A.alloc((8, 128))
        dnw = A.alloc((128,))
        mBD = A.alloc((128,))
        mBDT = A.alloc((128,))
        mCs = [A.alloc((128,)) for _ in range(4)]
        mCTs = [A.alloc((128,)) for _ in range(4)]
        P.dma("sp", mBD, c_mBD[0], writes=[R_dt])
        P.dma("sp", mBDT, c_mBD[1], writes=[R_dt])
        for bi in range(4):
            P.dma("sp", mCs[bi], c_mC[bi], writes=[R_dt])
            P.dma("sp", mCTs[bi], c_mC[4 + bi], writes=[R_dt])
        P.dma("sp", maskS, c_maskS[:, :], writes=[R_dt])
        P.dma("sp", maskT, c_maskT[:, :], writes=[R_dt])
        P.dma("sp", oh8, c_onehot8[:, :, :], writes=[R_dt])
        P.dma("sp", dnw, dn_norm_w.partition_broadcast(128), writes=[R_dt])
        xg = A.alloc((NT, 8))
        t1_ = A.alloc((NT, 8))
        t2_ = A.alloc((NT, 8))
        gg = A.alloc((NT, 8))
        beta = A.alloc((NT, 8))
        negb = A.alloc((NT, 8))
        gc_ = A.alloc((NT, 8))
        glb = A.alloc((NT, 8))
        egc = A.alloc((NT, 8))
        kds = A.alloc((NT, 8))
        ecd = A.alloc((NT, 8))
        bw = A.alloc((NT, 8))
        nA = A.alloc((8,))
        gcT = A.alloc((S,))
        P.op("pool", lambda e: e.memset(gcT, 0.0), [], [R_dt])
        av = ab[:, :, 0:8]
        bv = ab[:, :, 8:16]
        D_ = lambda fn, rd=(), wr=(): P.op("dve", fn, list(rd) + [R_dt], list(wr) + [R_dt])
        A_ = lambda fn, rd=(), wr=(): P.op("act", fn, list(rd) + [R_dt], list(wr) + [R_dt])
        D_(lambda e: e.tensor_tensor(xg, av, bc(dtb.unsqueeze(1), (128, NT, 8)), ALU.add))
        A_(lambda e: e.activation(t1_, xg, AF.Abs))
        A_(lambda e: e.activation(t2_, t1_, AF.Exp, scale=-1.0))
        A_(lambda e: e.activation(t1_, t2_, AF.Ln, bias=ones_f[:, 0:1], scale=1.0))
        D_(lambda e: e.scalar_tensor_tensor(t2_, xg, 0.0, t1_, ALU.max, ALU.add))
        A_(lambda e: e.activation(nA, alg, AF.Exp))
        D_(lambda e: e.tensor_scalar(nA, nA, -1.0, None, ALU.mult))
        D_(lambda e: e.tensor_tensor(gg, t2_, bc(nA.unsqueeze(1), (128, NT, 8)), ALU.mult))
        A_(lambda e: e.activation(beta, bv, AF.Sigmoid))
        D_(lambda e: e.tensor_scalar(negb, beta, -1.0, None, ALU.mult))
        gflat = gg.rearrange("p t h -> p (t h)")
        P.op("pe", lambda e: e.matmul(psum[0][:, 0:256], utri, gflat, start=True, stop=True), [R_dt, R_const], [PR[0][0]])
        P.op("pe", lambda e: e.matmul(psum[0][:, 512:768], ones_f, gflat, start=True, stop=True), [R_dt, R_c2], [PR[0][1]])
        A_(lambda e: e.copy(gc_.rearrange("p t h -> p (t h)"), psum[0][:, 0:256]), [PR[0][0]])
        A_(lambda e: e.copy(glb.rearrange("p t h -> p (t h)"), psum[0][:, 512:768]), [PR[0][1]])
        A_(lambda e: e.activation(egc, gc_, AF.Exp))
        A_(lambda e: e.activation(ecd, glb, AF.Exp))
        D_(lambda e: e.tensor_tensor(t1_, glb, gc_, ALU.subtract))
        A_(lambda e: e.activation(kds, t1_, AF.Exp))
        D_(lambda e: e.tensor_tensor(bw, beta, egc, ALU.mult))
        for grp in range(4):
            pi = 1 + grp % 2
            for q in range(8):
                t = grp * 8 + q
                P.op("pe", lambda e, t=t, q=q, pi=pi: e.matmul(psum[pi][0:8, q * 128:(q + 1) * 128], gg[:, t, :], utri, start=True, stop=True),
                     [R_dt, R_const], [PR[pi][q // 4]])
            A_(lambda e, grp=grp, pi=pi: e.copy(gcT[0:8, grp * 1024:grp * 1024 + 512], psum[pi][0:8, 0:512]), [PR[pi][0]])
            A_(lambda e, grp=grp, pi=pi: e.copy(gcT[0:8, grp * 1024 + 512:(grp + 1) * 1024], psum[pi][0:8, 512:1024]), [PR[pi][1]])

        p1_mark = A.mark()
        cwt = A.alloc((24, 4))
        P.dma("sp", cwt, conv_w[:, :, :], writes=[R_dt])
        raw = A.alloc((3, S + 3))
        cv = A.alloc((3, S))
        qkn = A.alloc((2, S), BF16)
        vb16 = A.alloc((S,), BF16)
        sq5 = A.alloc((512,), BF16)
        rs5 = A.alloc((512,))
        tmst = A.alloc((2, NT, 128), BF16)
        R_raw, R_cv, R_qkn, R_vb16, R_sq5, R_rs5, R_tmst = [Res() for _ in range(7)]
        R_dnq, R_dnk, R_ktm, R_vtm = Res(), Res(), Res(), Res()
        P.op("pool", lambda e: e.memset(raw[:, :, 0:3], 0.0), [], [R_raw])
        for h in range(8):
            for i in range(3):
                P.dma("sp", raw[:, i, 3:S + 3], pFM[i * 8 + h], reads=[R_pFM[i * 8 + h]], writes=[R_raw])
            for i in range(3):
                cc = i * 8 + h
                eng = "dve"
                P.op(eng, lambda e, i=i, cc=cc: e.tensor_scalar(cv[:, i, :], raw[:, i, 0:S], cwt[:, cc, 0:1], None, ALU.mult), [R_raw, R_dt], [R_cv])
                for jj in range(1, 4):
                    P.op(eng, lambda e, i=i, cc=cc, jj=jj: e.scalar_tensor_tensor(
                        cv[:, i, :], raw[:, i, jj:S + jj], cwt[:, cc, jj:jj + 1], cv[:, i, :], ALU.mult, ALU.add), [R_raw, R_dt], [R_cv])
                P.op("act", lambda e, i=i: e.activation(cv[:, i, :], cv[:, i, :], AF.Silu), [], [R_cv])
            for i in range(2):
                for blk in range(8):
                    sl = slice(blk * 512, (blk + 1) * 512)
                    pi, pk = 3, blk % 2
                    P.op("act", lambda e, i=i, sl=sl: e.activation(sq5, cv[:, i, sl], AF.Square), [R_cv], [R_sq5])
                    P.op("pe", lambda e, pi=pi, pk=pk: e.matmul(pbank(pi, pk), ones_b, sq5, start=True, stop=True), [R_sq5, R_c2], [PR[pi][pk]])
                    P.op("act", lambda e, pi=pi, pk=pk: e.activation(rs5, pbank(pi, pk), AF.Sqrt, bias=epsc[:, 0:1], scale=1.0), [PR[pi][pk], R_c2], [R_rs5])
                    P.op("dve", lambda e: e.reciprocal(rs5, rs5), [], [R_rs5])
                    scl = (128.0 ** -0.5) if i == 0 else 1.0
                    P.op("dve", lambda e, i=i, sl=sl, scl=scl: e.scalar_tensor_tensor(qkn[:, i, sl], cv[:, i, sl], scl, rs5, ALU.mult, ALU.mult),
                         [R_cv, R_rs5], [R_qkn])
            P.op("pool", lambda e: e.tensor_copy(vb16, cv[:, 2, :]), [R_cv], [R_vb16])
            P.dma("sp", dn_qT[h], qkn[:, 0, :], reads=[R_qkn], writes=[R_dnq])
            P.dma("sp", dn_kT[h], qkn[:, 1, :], reads=[R_qkn], writes=[R_dnk])
            for which, src, rsrc in ((0, qkn[:, 1, :], R_qkn), (1, vb16, R_vb16)):
                for g4 in range(4):
                    pi, pk = g4 % 2, g4 // 2
                    pv = psum[pi][:, pk * 512:(pk + 1) * 512].bitcast(BF16)
                    for q in range(8):
                        t = g4 * 8 + q
                        P.op("pe", lambda e, pv=pv, q=q, t=t, src=src: e.transpose(pv[:, q * 128:(q + 1) * 128], src[:, t * 128:(t + 1) * 128], ident_b),
                             [rsrc, R_c2], [PR[pi][pk]])
                    P.op("act" if g4 % 2 == 0 else "dve", lambda e, pv=pv, g4=g4, which=which: (
                        e.copy if g4 % 2 == 0 else e.tensor_copy)(tmst[:, which, g4 * 8:(g4 + 1) * 8, :], pv.rearrange("p (t d) -> p t d", t=8)),
                        [PR[pi][pk]], [R_tmst])
            for q4 in range(4):
                P.dma("sp", dn_ktm[q4 * 1024:(q4 + 1) * 1024].rearrange("(t p) h d -> p t h d", p=128)[:, :, h, :],
                      tmst[:, 0, q4 * 8:(q4 + 1) * 8, :], reads=[R_tmst], writes=[R_ktm])
                P.dma("sp", dn_vtm[q4 * 1024:(q4 + 1) * 1024].rearrange("(t p) h d -> p t h d", p=128)[:, :, h, :],
                      tmst[:, 1, q4 * 8:(q4 + 1) * 8, :], reads=[R_tmst], writes=[R_vtm])
        P.barrier()
        A.release(p1_mark)

        qTt = [A.alloc((8, 128), BF16) for _ in range(2)]
        kTt = [A.alloc((8, 128), BF16) for _ in range(2)]
        ktm = [A.alloc((8, 128), BF16) for _ in range(2)]
        vtm = [A.alloc((8, 128), BF16) for _ in range(2)]
        zt = [A.alloc((1024,)) for _ in range(2)]
        R_in = [Res("dnin0"), Res("dnin1")]
        names = ["dd", "dS_", "GS", "GT", "KKg", "Bm", "attnT", "egb", "qgT", "Xw", "Xu", "kdec", "Pa", "Pb", "Ma", "Mb", "Mta", "Mtb",
                 "WT", "U", "vnew", "osb2", "sq2", "szl", "Da", "Db", "Bd", "Bdt", "Cb", "Cbt", "G", "G2", "Mtc"]
        T_ = {}
        RR = {}
        for nme in names:
            dtp = BF16 if nme in ("Bm", "attnT", "qgT", "Xw", "Xu", "kdec", "Pa", "Pb", "Ma", "Mb", "Mta", "Mtb", "WT", "vnew",
                                   "Da", "Db", "Bd", "Bdt", "Cb", "Cbt", "G", "G2", "Mtc") else F32
            T_[nme] = A.alloc((8, 128), dtp)
            RR[nme] = Res(nme)
        st9 = A.alloc((4, 8))
        R_st9 = Res("st9")
        Sd = A.alloc((8, 128))
        Sdb = A.alloc((8, 128), BF16)
        R_Sd, R_Sdb = Res("Sd"), Res("Sdb")
        P.op("pool", lambda e: e.memset(Sd, 0.0), [], [R_Sd])
        P.op("pool", lambda e: e.memset(Sdb, 0.0), [], [R_Sdb])
        psc = [0]

        def nps():
            psc[0] = (psc[0] + 1) % 4
            return psc[0]

        def v4(ap, k):
            return ap[:, k * 4:(k + 1) * 4, :]

        def pv4(i, k):
            return psum[i][:, k * 512:(k + 1) * 512].rearrange("p (h n) -> p h n", h=4)

        def mm8(pi, lhs, rhs, reads, start=True, stop=True, lslice=None):
            for h in range(8):
                l_ap, r_ap = lhs(h), rhs(h)
                P.op("pe", lambda e, h=h, l_ap=l_ap, r_ap=r_ap: e.matmul(psum[pi][:, h * 128:(h + 1) * 128], l_ap, r_ap, start=start, stop=stop),
                     reads, [PR[pi][h // 4]])

        def ew2(eng, fn, reads, writes):
            for k in range(2):
                P.op(eng, lambda e, k=k: fn(e, k), reads(k) if callable(reads) else reads, writes)

        def dn_tile(t):
            b_ = t % 2
            rin = R_in[b_]
            tsl = slice(t * 128, (t + 1) * 128)
            P.dma("sp", qTt[b_], dn_qT[:, :, tsl].rearrange("h d n -> d h n"), reads=[R_dnq], writes=[rin])
            P.dma("sp", kTt[b_], dn_kT[:, :, tsl].rearrange("h d n -> d h n"), reads=[R_dnk], writes=[rin])
            P.dma("sp", ktm[b_], dn_ktm[tsl], reads=[R_ktm], writes=[rin])
            P.dma("sp", vtm[b_], dn_vtm[tsl], reads=[R_vtm], writes=[rin])
            P.dma("sp", zt[b_], pTM[tsl, 0:1024], reads=[R_pTM], writes=[rin])
            q_, k_, km_, vm_ = qTt[b_], kTt[b_], ktm[b_], vtm[b_]
            if DN_STAGE < 0.15:
                return
            pKK, pQK, pBC = nps(), nps(), nps()
            mm8(pKK, lambda h: k_[:, h, :], lambda h: k_[:, h, :], [rin])
            mm8(pQK, lambda h: k_[:, h, :], lambda h: q_[:, h, :], [rin])
            if DN_STAGE < 0.25:
                return
            mm8(pBC, lambda h: oh8[:, h, :], lambda h: gcT[:, tsl], [R_dt])
            if DN_STAGE < 0.35:
                return
            gcb = lambda k: bc(gc_[:, t, k * 4:(k + 1) * 4].unsqueeze(2), (128, 4, 128))
            if DN_SUB != 2:
                ew2("dve", lambda e, k: e.tensor_tensor(v4(T_["dd"], k), pv4(pBC, k), gcb(k), ALU.subtract), lambda k: [PR[pBC][k], R_dt], [RR["dd"]])
            if DN_SUB != 1:
                ew2("act", lambda e, k: e.activation(v4(T_["egb"], k), pv4(pBC, k), AF.Exp), lambda k: [PR[pBC][k]], [RR["egb"]])
            if DN_STAGE < 0.45:
                return
            P.op("dve", lambda e: e.tensor_scalar(T_["dS_"], T_["dd"], 0.0, None, ALU.max), [RR["dd"]], [RR["dS_"]])
            P.op("act", lambda e: e.activation(T_["GS"], T_["dS_"], AF.Exp, scale=-1.0), [RR["dS_"]], [RR["GS"]])
            P.op("dve", lambda e: e.tensor_scalar(T_["dS_"], T_["dd"], 0.0, None, ALU.min), [RR["dd"], RR["GS"]], [RR["dS_"]])
            P.op("act", lambda e: e.activation(T_["GT"], T_["dS_"], AF.Exp), [RR["dS_"]], [RR["GT"]])
            if DN_STAGE < 0.55:
                return
            P.op("dve", lambda e: e.tensor_tensor(T_["GS"], T_["GS"], bc(maskS.unsqueeze(1), (128, 8, 128)), ALU.mult), [R_dt], [RR["GS"]])
            P.op("dve", lambda e: e.tensor_tensor(T_["GT"], T_["GT"], bc(maskT.unsqueeze(1), (128, 8, 128)), ALU.mult), [R_dt], [RR["GT"]])
            if DN_STAGE < 0.65:
                return
            ew2("dve", lambda e, k: e.tensor_tensor(v4(T_["KKg"], k), pv4(pKK, k), v4(T_["GS"], k), ALU.mult), lambda k: [PR[pKK][k], RR["GS"]], [RR["KKg"]])
            P.op("dve", lambda e: e.tensor_tensor(T_["Bm"], T_["KKg"], bc(negb[:, t, :].unsqueeze(2), (128, 8, 128)), ALU.mult), [RR["KKg"], R_dt], [RR["Bm"]])
            ew2("dve", lambda e, k: e.tensor_tensor(v4(T_["attnT"], k), pv4(pQK, k), v4(T_["GT"], k), ALU.mult), lambda k: [PR[pQK][k], RR["GT"]], [RR["attnT"]])
            if DN_STAGE < 0.75:
                return
            P.op("dve", lambda e: e.tensor_tensor(T_["qgT"], q_, T_["egb"], ALU.mult), [rin, RR["egb"]], [RR["qgT"]])
            if DN_STAGE < 0.85:
                return
            P.op("dve", lambda e: e.tensor_tensor(T_["Xw"], km_, bc(bw[:, t, :].unsqueeze(2), (128, 8, 128)), ALU.mult), [rin, R_dt], [RR["Xw"]])
            P.op("dve", lambda e: e.tensor_tensor(T_["Xu"], vm_, bc(beta[:, t, :].unsqueeze(2), (128, 8, 128)), ALU.mult), [rin, R_dt], [RR["Xu"]])
            P.op("dve", lambda e: e.tensor_tensor(T_["kdec"], km_, bc(kds[:, t, :].unsqueeze(2), (128, 8, 128)), ALU.mult), [rin, R_dt], [RR["kdec"]])
            if DN_STAGE < 2:
                return
            pTr = nps()
            ptv = psum[pTr][:, 0:512].bitcast(BF16)
            for h in range(8):
                P.op("pe", lambda e, h=h: e.transpose(ptv[:, h * 128:(h + 1) * 128], T_["Bm"][:, h, :], ident_b), [RR["Bm"], R_c2], [PR[pTr][0]])
            P.op("act", lambda e: e.copy(T_["Mta"], ptv.rearrange("p (h n) -> p h n", h=8)), [PR[pTr][0]], [RR["Mta"]])
            I8 = bc(ident_b.unsqueeze(1), (128, 8, 128))
            bcm = lambda m: bc(m.unsqueeze(1), (128, 8, 128))
            P.op("dve", lambda e: e.tensor_tensor(T_["Bd"], T_["Bm"], bcm(mBD), ALU.mult), [RR["Bm"], R_dt], [RR["Bd"]])
            P.op("pool", lambda e: e.tensor_tensor(T_["Bdt"], T_["Mta"], bcm(mBDT), ALU.mult), [RR["Mta"], R_dt], [RR["Bdt"]])
            P.op("dve", lambda e: e.tensor_tensor(T_["Pa"], T_["Bdt"], I8, ALU.add), [RR["Bdt"], R_c2], [RR["Pa"]])
            P.op("pool", lambda e: e.tensor_tensor(T_["Da"], T_["Bd"], I8, ALU.add), [RR["Bd"], R_c2], [RR["Da"]])
            if DN_STAGE < 3:
                return
            Mc, Mtc, Ec, Dc = "Bd", "Bdt", "Pa", "Da"
            for lev in range(2):
                Mn = "Ma" if Mc != "Ma" else "Mb"
                Mtn = "Mtb" if Mtc != "Mtb" else "Mtc"
                En = "Pb" if Ec != "Pb" else "Pa"
                Dn = "Db" if Dc != "Db" else "Da"
                pM, pMt = nps(), nps()
                mm8(pM, lambda h, Mtc=Mtc: T_[Mtc][:, h, :], lambda h, Mc=Mc: T_[Mc][:, h, :], [RR[Mtc], RR[Mc]])
                mm8(pMt, lambda h, Mc=Mc: T_[Mc][:, h, :], lambda h, Mtc=Mtc: T_[Mtc][:, h, :], [RR[Mtc], RR[Mc]])
                ew2("act", lambda e, k, Mn=Mn, pM=pM: e.copy(v4(T_[Mn], k), pv4(pM, k)), lambda k, pM=pM: [PR[pM][k]], [RR[Mn]])
                ew2("dve", lambda e, k, Mtn=Mtn, pMt=pMt: e.tensor_copy(v4(T_[Mtn], k), pv4(pMt, k)), lambda k, pMt=pMt: [PR[pMt][k]], [RR[Mtn]])
                pE, pDd = nps(), nps()
                mm8(pE, lambda h, Mn=Mn: T_[Mn][:, h, :], lambda h, Ec=Ec: T_[Ec][:, h, :], [RR[Mn], RR[Ec]])
                mm8(pDd, lambda h, Mtn=Mtn: T_[Mtn][:, h, :], lambda h, Dc=Dc: T_[Dc][:, h, :], [RR[Mtn], RR[Dc]])
                ew2("dve", lambda e, k, En=En, Ec=Ec, pE=pE: e.tensor_tensor(v4(T_[En], k), pv4(pE, k), v4(T_[Ec], k), ALU.add),
                    lambda k, pE=pE, Ec=Ec: [PR[pE][k], RR[Ec]], [RR[En]])
                ew2("dve", lambda e, k, Dn=Dn, Dc=Dc, pDd=pDd: e.tensor_tensor(v4(T_[Dn], k), pv4(pDd, k), v4(T_[Dc], k), ALU.add),
                    lambda k, pDd=pDd, Dc=Dc: [PR[pDd][k], RR[Dc]], [RR[Dn]])
                Mc, Mtc, Ec, Dc = Mn, Mtn, En, Dn
            for bi in range(4):
                En = "Pb" if Ec != "Pb" else "Pa"
                Dn = "Db" if Dc != "Db" else "Da"
                P.op("dve", lambda e, bi=bi: e.tensor_tensor(T_["Cb"], T_["Bm"], bcm(mCs[bi]), ALU.mult), [RR["Bm"], R_dt], [RR["Cb"]])
                pG = nps()
                mm8(pG, lambda h: T_["Cb"][:, h, :], lambda h, Ec=Ec: T_[Ec][:, h, :], [RR["Cb"], RR[Ec]])
                ew2("act", lambda e, k, pG=pG: e.copy(v4(T_["G"], k), pv4(pG, k)), lambda k, pG=pG: [PR[pG][k]], [RR["G"]])
                pH = nps()
                mm8(pH, lambda h, Dc=Dc: T_[Dc][:, h, :], lambda h: T_["G"][:, h, :], [RR[Dc], RR["G"]])
                ew2("dve", lambda e, k, En=En, Ec=Ec, pH=pH: e.tensor_tensor(v4(T_[En], k), pv4(pH, k), v4(T_[Ec], k), ALU.add),
                    lambda k, pH=pH, Ec=Ec: [PR[pH][k], RR[Ec]], [RR[En]])
                if bi < 3:
                    P.op("pool", lambda e, bi=bi: e.tensor_tensor(T_["Cbt"], T_["Mta"], bcm(mCTs[bi]), ALU.mult), [RR["Mta"], R_dt], [RR["Cbt"]])
                    pG2 = nps()
                    mm8(pG2, lambda h: T_["Cbt"][:, h, :], lambda h, Dc=Dc: T_[Dc][:, h, :], [RR["Cbt"], RR[Dc]])
                    ew2("act", lambda e, k, pG2=pG2: e.copy(v4(T_["G2"], k), pv4(pG2, k)), lambda k, pG2=pG2: [PR[pG2][k]], [RR["G2"]])
                    pH2 = nps()
                    mm8(pH2, lambda h, Ec=Ec: T_[Ec][:, h, :], lambda h: T_["G2"][:, h, :], [RR[Ec], RR["G2"]])
                    ew2("dve", lambda e, k, Dn=Dn, Dc=Dc, pH2=pH2: e.tensor_tensor(v4(T_[Dn], k), pv4(pH2, k), v4(T_[Dc], k), ALU.add),
                        lambda k, pH2=pH2, Dc=Dc: [PR[pH2][k], RR[Dc]], [RR[Dn]])
                    Dc = Dn
                Ec = En
            Pc = Ec
            if DN_STAGE < 4:
                return
            pW, pU = nps(), nps()
            mm8(pW, lambda h: T_["Xw"][:, h, :], lambda h, Pc=Pc: T_[Pc][:, h, :], [RR["Xw"], RR[Pc]])
            mm8(pU, lambda h, Pc=Pc: T_[Pc][:, h, :], lambda h: T_["Xu"][:, h, :], [RR["Xu"], RR[Pc]])
            ew2("act", lambda e, k: e.copy(v4(T_["WT"], k), pv4(pW, k)), lambda k: [PR[pW][k]], [RR["WT"]])
            ew2("act", lambda e, k: e.copy(v4(T_["U"], k), pv4(pU, k)), lambda k: [PR[pU][k]], [RR["U"]])
            if DN_STAGE < 5:
                return
            pWS = nps()
            mm8(pWS, lambda h: T_["WT"][:, h, :], lambda h: Sdb[:, h, :], [RR["WT"], R_Sdb])
            ew2("dve", lambda e, k: e.tensor_tensor(v4(T_["vnew"], k), v4(T_["U"], k), pv4(pWS, k), ALU.subtract), lambda k: [PR[pWS][k], RR["U"]], [RR["vnew"]])
            pO = nps()
            for h in range(8):
                P.op("pe", lambda e, h=h: e.matmul(psum[pO][:, h * 128:(h + 1) * 128], T_["attnT"][:, h, :], T_["vnew"][:, h, :], start=True, stop=False),
                     [RR["attnT"], RR["vnew"]], [PR[pO][h // 4]])
                P.op("pe", lambda e, h=h: e.matmul(psum[pO][:, h * 128:(h + 1) * 128], T_["qgT"][:, h, :], Sdb[:, h, :], start=False, stop=True),
                     [RR["qgT"], R_Sdb], [PR[pO][h // 4]])
            pD = nps()
            mm8(pD, lambda h: T_["kdec"][:, h, :], lambda h: T_["vnew"][:, h, :], [RR["kdec"], RR["vnew"]])
            P.op("dve", lambda e: e.tensor_tensor(Sd, Sd, bc(ecd[:, t, :].unsqueeze(2), (128, 8, 128)), ALU.mult), [R_dt], [R_Sd])
            ew2("dve", lambda e, k: e.tensor_tensor(v4(Sd, k), v4(Sd, k), pv4(pD, k), ALU.add), lambda k: [PR[pD][k]], [R_Sd])
            P.op("act", lambda e: e.copy(Sdb, Sd), [R_Sd], [R_Sdb])
            if DN_STAGE < 6:
                return
            ew2("act", lambda e, k: e.copy(v4(T_["osb2"], k), pv4(pO, k)), lambda k: [PR[pO][k]], [RR["osb2"]])
            P.op("dve", lambda e: e.tensor_tensor(T_["sq2"], T_["osb2"], T_["osb2"], ALU.mult), [RR["osb2"]], [RR["sq2"]])
            P.op("dve", lambda e: e.tensor_reduce(st9[:, 0, :], T_["sq2"], AX.X, ALU.add), [RR["sq2"]], [R_st9])
            P.op("act", lambda e: e.activation(st9[:, 1, :], st9[:, 0, :], AF.Sqrt, bias=epsc[:, 0:1], scale=1.0 / 128), [R_st9, R_c2], [R_st9])
            P.op("dve", lambda e: e.reciprocal(st9[:, 2, :], st9[:, 1, :]), [R_st9], [R_st9])
            P.op("dve", lambda e: e.tensor_tensor(T_["osb2"], T_["osb2"], bc(st9[:, 2, :].unsqueeze(2), (128, 8, 128)), ALU.mult), [R_st9], [RR["osb2"]])
            P.op("dve", lambda e: e.tensor_tensor(T_["osb2"], T_["osb2"], bc(dnw.unsqueeze(1), (128, 8, 128)), ALU.mult), [R_dt], [RR["osb2"]])
            szf = T_["szl"].rearrange("p h n -> p (h n)")
            P.op("act", lambda e, szf=szf, b_=b_: e.activation(szf, zt[b_], AF.Silu), [rin], [RR["szl"]])
            P.op("dve", lambda e: e.tensor_tensor(T_["sq2"], T_["osb2"], T_["szl"], ALU.mult), [RR["osb2"], RR["szl"]], [RR["sq2"]])
            P.dma("sp", o_scr[tsl, 0:1024], T_["sq2"].rearrange("p h n -> p (h n)"), reads=[RR["sq2"]], writes=[R_oscr])
            if dbg == "dn" and t == 0 and DN_DUMP:
                for ci, nme in enumerate(["GS", "GT", "KKg", "U", "osb2", "egb"]):
                    P.dma("act", dbg_a[:, ci * 128:(ci + 1) * 128], T_[nme][:, 0, :], reads=[RR[nme]])
                P.dma("act", dbg_a[:, 1024:1024 + 256], gc_.rearrange("p t h -> p (t h)"), reads=[R_dt])
                P.dma("act", dbg_a[:, 1280:1280 + 256], beta.rearrange("p t h -> p (t h)"), reads=[R_dt])

        for t in range(DN_TILES):
            dn_tile(t)
        P.barrier()
        A.release(dn_mark)
        if dbg == "dn":
            P.dma("sp", out[0:128, 0:128], dnw, final=True)
            P.run()
            return nc, dbg_out


        b_mark = A.mark()
        R_bc = Res("b_const")
        idxt = A.alloc((8,), I32)
        P.dma("sp", idxt, own_idx[:, :], writes=[R_bc])
        wr = A.alloc((NKC, 64))
        P.dma("sp", wr, w_router.rearrange("(c p) n -> p c n", p=128), writes=[R_bc])
        rb = A.alloc((64,))
        P.dma("sp", rb, router_bias.partition_broadcast(128), writes=[R_bc])
        Wc = A.alloc((8, 65))
        R_Wc = Res("Wc")
        P.op("pool", lambda e: e.memset(Wc, 1.0), [], [R_Wc])
        h2T = A.alloc((NKC, OWN), BF16)
        R_h2T = [Res("h2T%d" % i) for i in range(8)]
        moe_mark = A.mark()
        gm = A.alloc((D,))
        tmpr = A.alloc((D,))
        P.dma("sp", gm, mod_scr[0:1, 2 * D:3 * D].partition_broadcast(128), reads=[R_modscr], writes=[R_bc])
        P.dma("sp", tmpr, g_post1.partition_broadcast(128), writes=[R_bc])
        P.op("dve", lambda e: e.tensor_tensor(gm, gm, tmpr, ALU.mult), [], [R_bc])
        wo = A.alloc((NKC, D), BF16)
        R_wo = Res("wo")
        w_out_v = w_out.rearrange("(c p) n -> p c n", p=128)
        for q4 in range(4):
            P.dma("pool", wo[:, :, q4 * 512:(q4 + 1) * 512], w_out_v[:, :, q4 * 512:(q4 + 1) * 512], writes=[R_wo])
        og = A.alloc((D,))
        ogb = A.alloc((D,), BF16)
        oT = A.alloc((NKC, 128), BF16)
        ysb = A.alloc((D,))
        xot = A.alloc((D,))
        x1t = A.alloc((D,))
        xn2 = A.alloc((D,))
        hTf = A.alloc((NKC, 128))
        rsb = A.alloc((4,))
        rt_ = A.alloc((16, 64))
        R_og, R_ogb, R_oT, R_ysb, R_xot, R_x1t, R_xn2, R_hTf, R_rsb, R_rt = [Res() for _ in range(10)]
        R_x1scr = Res("x1scr")
        sc_, sel_, eq_, sel2_, selm_, ch_ = [rt_[:, i, :] for i in range(6)]
        m1_ = rt_[:, 6, 0:8]
        m2_ = rt_[:, 6, 8:16]
        gs_ = rt_[:, 6, 16:24]
        keep_ = rt_[:, 6, 24:32]
        kt_ = rt_[:, 6, 32:40]
        top8 = rt_[:, 7, 0:8]
        top8b = rt_[:, 7, 8:16]
        den_ = rt_[:, 7, 16:17]
        rden_ = rt_[:, 7, 17:18]

        def rms_rstd2(src, rsrc, col):
            P.op("pool", lambda e: e.memset(rsb[:, col:col + 2], 0.0), [], [R_rsb])
            P.op("act", lambda e: e.activation(xn2, src, AF.Square, accum_out=rsb[:, col:col + 1]), [rsrc], [R_xn2, R_rsb])
            P.op("act", lambda e: e.activation(rsb[:, col + 1:col + 2], rsb[:, col:col + 1], AF.Sqrt, bias=epsc[:, 0:1], scale=1.0 / D), [R_c2], [R_rsb])
            P.op("dve", lambda e: e.reciprocal(rsb[:, col:col + 1], rsb[:, col + 1:col + 2]), [], [R_rsb])

        def phase_b_tile(tt):
            tsl = slice(tt * 128, (tt + 1) * 128)
            P.op("pool", lambda e: e.indirect_dma_start(out=og, out_offset=None, in_=o_scr[:, :],
                                                        in_offset=bass.IndirectOffsetOnAxis(ap=idxt[:, tt:tt + 1], axis=0)),
                 [R_oscr, R_bc], [R_og], dma=True)
            P.dma("sp", xot, xo[tsl, :], writes=[R_xot])
            P.op("act", lambda e: e.copy(ogb, og), [R_og], [R_ogb])
            for half in range(2):
                pv = psum[0][:, half * 512:(half + 1) * 512].bitcast(BF16)
                for q in range(8):
                    kc = half * 8 + q
                    P.op("pe", lambda e, pv=pv, q=q, kc=kc: e.transpose(pv[:, q * 128:(q + 1) * 128], ogb[:, kc * 128:(kc + 1) * 128], ident_b),
                         [R_ogb, R_c2], [PR[0][half]])
                P.op("act" if half == 0 else "dve", lambda e, pv=pv, half=half: (e.copy if half == 0 else e.tensor_copy)(
                    oT[:, half * 8:(half + 1) * 8, :], pv.rearrange("p (c n) -> p c n", c=8)), [PR[0][half]], [R_oT])
            for db in range(4):
                pi, pk = 1 + db // 2, db % 2
                for kc in range(NKC):
                    P.op("pe", lambda e, kc=kc, db=db, pi=pi, pk=pk: e.matmul(pbank(pi, pk), oT[:, kc, :], wo[:, kc, db * 512:(db + 1) * 512],
                                                                            start=(kc == 0), stop=(kc == NKC - 1)), [R_oT, R_wo], [PR[pi][pk]])
                P.op("act", lambda e, db=db, pi=pi, pk=pk: e.copy(ysb[:, db * 512:(db + 1) * 512], pbank(pi, pk)), [PR[pi][pk]], [R_ysb])
            rms_rstd2(ysb, R_ysb, 0)
            P.op("dve", lambda e: e.scalar_tensor_tensor(ysb, ysb, rsb[:, 0:1], gm, ALU.mult, ALU.mult), [R_rsb, R_bc], [R_ysb])
            P.op("dve", lambda e: e.tensor_tensor(x1t, ysb, xot, ALU.add), [R_ysb, R_xot], [R_x1t])
            P.dma("sp", x1_scr[tsl, :], x1t, reads=[R_x1t], writes=[R_x1scr])
            rms_rstd2(x1t, R_x1t, 2)
            P.op("dve", lambda e: e.tensor_scalar(xn2, x1t, rsb[:, 2:3], None, ALU.mult), [R_x1t, R_rsb], [R_xn2])
            for g4 in range(4):
                pi, pk = (g4 % 2), (g4 // 2)
                for q in range(4):
                    kc = g4 * 4 + q
                    P.op("pe", lambda e, pi=pi, pk=pk, q=q, kc=kc: e.transpose(pbank(pi, pk)[:, q * 128:(q + 1) * 128], xn2[:, kc * 128:(kc + 1) * 128], ident_f),
                         [R_xn2, R_const], [PR[pi][pk]])
                sl4 = slice(g4 * 4, (g4 + 1) * 4)
                P.op("dve", lambda e, pi=pi, pk=pk, sl4=sl4: e.tensor_tensor(
                    hTf[:, sl4, :], pbank(pi, pk).rearrange("p (c n) -> p c n", c=4), bc(a2[:, sl4].unsqueeze(2), (128, 4, 128)), ALU.mult),
                    [PR[pi][pk], R_a], [R_hTf])
            P.op("pool", lambda e: e.tensor_tensor(hTf, hTf, bc(s2.unsqueeze(2), (128, NKC, 128)), ALU.add), [R_modfm], [R_hTf])
            P.op("act", lambda e: e.copy(h2T[:, :, tsl], hTf), [R_hTf], [R_h2T[tt]])
            for kc in range(NKC):
                P.op("pe", lambda e, kc=kc: e.matmul(pbank(2, 0)[:, 0:64], hTf[:, kc, :], wr[:, kc, :], start=(kc == 0), stop=(kc == NKC - 1)),
                     [R_hTf, R_bc], [PR[2][0]])
            V = lambda fn, rd=(): P.op("dve", fn, list(rd) + [R_rt, R_bc], [R_rt])
            P.op("act", lambda e: e.activation(sc_, pbank(2, 0)[:, 0:64], AF.Sigmoid), [PR[2][0]], [R_rt])
            V(lambda e: e.tensor_tensor(sel_, sc_, rb, ALU.add))
            g3 = lambda ap: ap.rearrange("p (g k) -> p g k", g=8)
            V(lambda e: e.tensor_reduce(m1_, g3(sel_), AX.X, ALU.max))
            V(lambda e: e.tensor_tensor(g3(eq_), g3(sel_), bc(m1_.unsqueeze(2), (128, 8, 8)), ALU.is_equal))
            V(lambda e: e.scalar_tensor_tensor(sel2_, eq_, -1.0e9, sel_, ALU.mult, ALU.add))
            V(lambda e: e.tensor_reduce(m2_, g3(sel2_), AX.X, ALU.max))
            V(lambda e: e.tensor_tensor(gs_, m1_, m2_, ALU.add))
            V(lambda e: e.max(top8, gs_))
            V(lambda e: e.tensor_scalar(keep_, gs_, top8[:, 3:4], None, ALU.is_ge))
            V(lambda e: e.tensor_scalar(kt_, keep_, 1.0e3, -1.0e3, ALU.mult, ALU.add))
            V(lambda e: e.tensor_tensor(g3(selm_), g3(sel_), bc(keep_.unsqueeze(2), (128, 8, 8)), ALU.mult))
            V(lambda e: e.tensor_tensor(g3(selm_), g3(selm_), bc(kt_.unsqueeze(2), (128, 8, 8)), ALU.add))
            V(lambda e: e.max(top8b, selm_))
            V(lambda e: e.tensor_scalar(ch_, selm_, top8b[:, 7:8], None, ALU.is_ge))
            V(lambda e: e.tensor_tensor(ch_, ch_, sc_, ALU.mult))
            V(lambda e: e.tensor_reduce(den_, ch_, AX.X, ALU.add))
            V(lambda e: e.reciprocal(rden_, den_))
            P.op("dve", lambda e: e.tensor_scalar(Wc[:, tt, 0:64], ch_, rden_, 2.5, ALU.mult, ALU.mult), [R_rt], [R_Wc])

        for tt in range(8):
            phase_b_tile(tt)
        if dbg == "b":
            P.dma("sp", dbg_a[:, 0:520], Wc.rearrange("p t e -> p (t e)"), reads=[R_Wc], final=True)
            P.dma("sp", dbg_a[:, 1024:1536], h2T[:, 0, :].bitcast(F32), reads=R_h2T, final=True)
            P.dma("sp", out[0:128, :], x1t, reads=[R_x1t], final=True)
            P.run()
            return nc, dbg_out
        P.barrier()
        A.release(moe_mark)


        NE = 65 if MOE_EXPERTS is None else MOE_EXPERTS
        acc = A.alloc((8, D))
        R_acc = [Res("acc%d" % i) for i in range(8)]
        P.op("pool", lambda e: e.memset(acc, 0.0), [], R_acc)
        wg = [A.alloc((NKC, 512), BF16) for _ in range(2)]
        wu = [A.alloc((NKC, 512), BF16) for _ in range(2)]
        wd = [A.alloc((4, D), BF16) for _ in range(1)]
        R_wg = [Res("wg0"), Res("wg1")]
        R_wu = [Res("wu0"), Res("wu1")]
        R_wd = [Res("wd0")]
        actT = A.alloc((4, OWN), BF16)
        R_act = [[Res("act%d_%d" % (f, tb)) for tb in range(2)] for f in range(4)]
        sgm = [A.alloc((512,), BF16) for _ in range(2)]
        R_sgm = [Res("sgm0"), Res("sgm1")]
        bcnt = [0]

        def nbank():
            n = bcnt[0] % 8
            bcnt[0] += 1
            return n // 2, n % 2

        def moe_expert(ex):
            b_ = ex % 2
            g_, u_, d_ = wg[b_], wu[b_], wd[0]
            rg_, ru_, rd_ = R_wg[b_], R_wu[b_], R_wd[0]
            P.dma("pool", g_, w_gate[ex].rearrange("(c p) n -> p c n", p=128), writes=[rg_])
            P.dma("pool", u_, w_up[ex].rearrange("(c p) n -> p c n", p=128), writes=[ru_])
            P.dma("pool", d_, w_down[ex].rearrange("(c p) n -> p c n", p=128), writes=[rd_])
            scnt = 0
            for f in range(4):
                for tb in range(2):
                    gi, gk_ = nbank()
                    ui, uk_ = nbank()
                    for kc in range(NKC):
                        P.op("pe", lambda e, kc=kc, f=f, tb=tb, gi=gi, gk_=gk_: e.matmul(
                            pbank(gi, gk_), g_[:, kc, f * 128:(f + 1) * 128], h2T[:, kc, tb * 512:(tb + 1) * 512],
                            start=(kc == 0), stop=(kc == NKC - 1)), [rg_] + R_h2T[tb * 4:(tb + 1) * 4], [PR[gi][gk_]])
                    for kc in range(NKC):
                        P.op("pe", lambda e, kc=kc, f=f, tb=tb, ui=ui, uk_=uk_: e.matmul(
                            pbank(ui, uk_), u_[:, kc, f * 128:(f + 1) * 128], h2T[:, kc, tb * 512:(tb + 1) * 512],
                            start=(kc == 0), stop=(kc == NKC - 1)), [ru_] + R_h2T[tb * 4:(tb + 1) * 4], [PR[ui][uk_]])
                    sg_ = sgm[scnt % 2]
                    rs_ = R_sgm[scnt % 2]
                    scnt += 1
                    P.op("act", lambda e, sg_=sg_, gi=gi, gk_=gk_: e.activation(sg_, pbank(gi, gk_), AF.Silu), [PR[gi][gk_]], [rs_])
                    P.op("dve", lambda e, sg_=sg_, ui=ui, uk_=uk_, f=f, tb=tb: e.tensor_tensor(
                        actT[:, f, tb * 512:(tb + 1) * 512], pbank(ui, uk_), sg_, ALU.mult), [PR[ui][uk_], rs_], [R_act[f][tb]])
            for tt in range(8):
                for db in range(4):
                    di, dk_ = nbank()
                    for f in range(4):
                        P.op("pe", lambda e, f=f, tt=tt, db=db, di=di, dk_=dk_: e.matmul(
                            pbank(di, dk_), actT[:, f, tt * 128:(tt + 1) * 128], d_[:, f, db * 512:(db + 1) * 512],
                            start=(f == 0), stop=(f == 3)), [R_act[f][tt // 4], rd_], [PR[di][dk_]])
                    P.op("dve", lambda e, tt=tt, db=db, di=di, dk_=dk_: e.scalar_tensor_tensor(
                        acc[:, tt, db * 512:(db + 1) * 512], pbank(di, dk_), Wc[:, tt, ex:ex + 1], acc[:, tt, db * 512:(db + 1) * 512],
                        ALU.mult, ALU.add), [PR[di][dk_], R_Wc], [R_acc[tt]])

        for ex in range(NE):
            moe_expert(ex)
        P.barrier()
        A.release(moe_mark)
        acc2 = A.alloc((8, D))
        gf = A.alloc((D,))
        tmpf = A.alloc((D,))
        R_gf = Res("gf")
        P.dma("sp", gf, mod_scr[0:1, 5 * D:6 * D].partition_broadcast(128), reads=[R_modscr], writes=[R_gf])
        P.dma("sp", tmpf, g_post2.partition_broadcast(128), writes=[R_gf])
        P.op("dve", lambda e: e.tensor_tensor(gf, gf, tmpf, ALU.mult), [], [R_gf])
        x1l = [A.alloc((D,)) for _ in range(2)]
        R_x1l = [Res("x1l0"), Res("x1l1")]
        junk = A.alloc((D,))
        R_junk = Res("junk")
        rsf = A.alloc((8, 2))
        R_rsf = Res("rsf")
        P.op("pool", lambda e: e.memset(rsf, 0.0), [], [R_rsf])
        for tt in range(8):
            tsl = slice(tt * 128, (tt + 1) * 128)
            xl = x1l[tt % 2]
            rxl = R_x1l[tt % 2]
            P.dma("sp", xl, x1_scr[tsl, :], reads=[R_x1scr], writes=[rxl])
            at = acc2[:, tt, :]
            P.op("act", lambda e, at=at, tt=tt: e.activation(junk, at, AF.Square, accum_out=rsf[:, tt, 0:1]), [R_acc[tt]], [R_junk, R_rsf])
            P.op("act", lambda e, tt=tt: e.activation(rsf[:, tt, 1:2], rsf[:, tt, 0:1], AF.Sqrt, bias=epsc[:, 0:1], scale=1.0 / D), [R_c2], [R_rsf])
            P.op("dve", lambda e, tt=tt: e.reciprocal(rsf[:, tt, 0:1], rsf[:, tt, 1:2]), [], [R_rsf])
            P.op("dve", lambda e, at=at, tt=tt: e.scalar_tensor_tensor(at, at, rsf[:, tt, 0:1], gf, ALU.mult, ALU.mult), [R_rsf, R_gf], [R_acc[tt]])
            P.op("dve", lambda e, at=at, xl=xl: e.tensor_tensor(xl, at, xl, ALU.add), [R_acc[tt]], [rxl])
            P.dma("sp", out[tsl, :], xl, reads=[rxl], final=True)
        P.run()
    return nc, dbg_out


W_IN_ORDER = None


def _w_in_perm():
    sp = np.cumsum([0, 1024, 1024, 1024, 1024, 8, 8, 512, 512, 1024, 1024])
    seg = lambda i: np.arange(sp[i], sp[i + 1])
    return np.concatenate([seg(0), seg(1), seg(2), seg(3), seg(6), seg(7), seg(8), seg(9), seg(4), seg(5)])


def host_inputs(inputs, core, light=False):
    b, j = core // 4, core % 4
    f = lambda a: np.ascontiguousarray(a, dtype=np.float32)
    fm = lambda v: f(np.asarray(v).reshape(NKC, 128).T)
    m = {}
    x = np.asarray(inputs["x"])
    m["xb"] = f(x[b])
    m["xo"] = f(x[b, j * OWN:(j + 1) * OWN])
    m["cT"] = fm(inputs["c"][b])
    m["pos"] = np.ascontiguousarray(np.asarray(inputs["positions"])[b].reshape(NT, 128).T.astype(np.int32))
    m["w_ada"] = f(inputs["w_ada"][0])
    m["b_ada"] = f(inputs["b_ada"][0]).reshape(1, -1)
    m["g_pre1"] = fm(inputs["pre_norm_mix"][0])
    m["g_pre2"] = fm(inputs["pre_norm_ffn"][0])
    m["g_post1"] = f(inputs["post_norm_mix"][0]).reshape(1, -1)
    m["g_post2"] = f(inputs["post_norm_ffn"][0]).reshape(1, -1)
    m["w_in"] = f(np.asarray(inputs["w_in"][0])[:, _w_in_perm()])
    cw = np.asarray(inputs["conv_w"][0])
    m["conv_w"] = f(cw.T.reshape(24, 128, 4).transpose(1, 0, 2))
    m["a_log"] = f(inputs["a_log"][0]).reshape(1, 8)
    m["dt_bias"] = f(inputs["dt_bias"][0]).reshape(1, 8)
    m["dn_norm_w"] = f(inputs["dn_norm_w"][0]).reshape(1, 128)
    m["rt_norm_w"] = f(inputs["rt_norm_w"][0]).reshape(1, 1024)
    m["w_out"] = f(inputs["w_out"][0])
    m["w_router"] = f(inputs["w_router"][0])
    m["router_bias"] = f(inputs["router_bias"][0]).reshape(1, 64)
    if not light:
      m["w_gate"] = f(np.concatenate([np.asarray(inputs["w_gate_exp"][0]), np.asarray(inputs["w_gate_sh"])], 0))
      m["w_up"] = f(np.concatenate([np.asarray(inputs["w_up_exp"][0]), np.asarray(inputs["w_up_sh"])], 0))
      m["w_down"] = f(np.concatenate([np.asarray(inputs["w_down_exp"][0]), np.asarray(inputs["w_down_sh"])], 0))
    m["own_idx"] = np.ascontiguousarray((j * OWN + np.arange(OWN)).reshape(8, 128).T.astype(np.int32))
    i = np.arange(128)
    m["c_ident"] = f(np.eye(128))
    m["c_utri"] = f(i[:, None] <= i[None, :])
    m["c_maskS"] = f(i[None, :] < i[:, None])
    m["c_maskT"] = f(i[None, :] >= i[:, None])
    oh = np.zeros((128, 8, 128), np.float32)
    for h in range(8):
        oh[h, h, :] = 1.0
    m["c_onehot8"] = oh
    lg = np.log(1.0 - 2.0 ** (-5.0 - np.arange(8, dtype=np.float64)))
    diff = (i[None, :] - i[:, None]).astype(np.float64)
    rtm = np.where(diff[:, None, :] >= 0, np.exp(np.maximum(diff[:, None, :], 0) * lg[None, :, None]), 0.0) * 0.125
    m["c_rtmask"] = f(rtm)
    m["c_gq"] = f(np.broadcast_to(np.exp((i[None, None, :] + 1.0) * lg[None, :, None]), (64, 8, 128)))
    m["c_gk"] = f(np.exp((127.0 - i[:, None]) * lg[None, :]) * 0.125)
    m["c_gC"] = f(np.broadcast_to(np.exp(128.0 * lg)[None, :], (64, 8)))
    mBDh = ((i[:, None] // 8) == (i[None, :] // 8)) & (i[None, :] < i[:, None])
    m["c_mBD"] = f(np.stack([mBDh, mBDh.T]))
    mcs = []
    for bsz in (8, 16, 32, 64):
        same = (i[:, None] // (2 * bsz)) == (i[None, :] // (2 * bsz))
        mcs.append(same & ((i[:, None] % (2 * bsz)) >= bsz) & ((i[None, :] % (2 * bsz)) < bsz))
    m["c_mC"] = f(np.stack(mcs + [x.T for x in mcs]))
    m["c_theta"] = f(1.0 / (10000.0 ** np.linspace(0.0, 1.0, 32, dtype=np.float32))).reshape(1, 32)
    return m


def kernel(**inputs):
    nc, _ = build_program(DBG)
    cores = list(range(8)) if DBG_CORES is None else DBG_CORES
    in_maps = [host_inputs(inputs, c, light=DBG not in (None, "moe")) for c in cores]
    res = run_bass_kernel_spmd(nc, in_maps, core_ids=list(range(len(cores))))
    if DBG is not None:
        return res
    outp = np.zeros((2, S, D), np.float32)
    for k, c in enumerate(cores):
        b, j = c // 4, c % 4
        outp[b, j * OWN:(j + 1) * OWN] = res.results[k]["out"]
    return outp
```

```python
import numpy as np
import concourse.bass as bass
import concourse.mybir as mybir
from concourse.bass_utils import run_bass_kernel_spmd
from contextlib import ExitStack

F32 = mybir.dt.float32
BF16 = mybir.dt.bfloat16
I32 = mybir.dt.int32
ALU = mybir.AluOpType
AF = mybir.ActivationFunctionType
AX = mybir.AxisListType

D = 2048
S = 4096
NT = 32
OWN = 1024
NKC = 16
EPS = 1e-6
DBG = None
DBG_CORES = None
DN_TILES = NT
DN_DUMP = False
DN_STAGE = 6
DN_SUB = 0
MOE_EXPERTS = None
DN_GROUPS = 2


class Res:
    __slots__ = ("name", "w", "r", "excl")

    def __init__(self, name="", excl=False):
        self.name = name
        self.w = None
        self.r = []
        self.excl = excl


class Prog:
    COMPUTE = ("pe", "act", "dve", "pool")
    ENG = ("pe", "act", "dve", "pool", "sp")
    ND = 48

    def __init__(self, nc, es):
        self.nc = nc
        self.es = es
        self.ops = {e: [] for e in self.ENG}
        self.cnt = {e: 0 for e in self.COMPUTE}
        self.sems = {}
        for e in self.COMPUTE:
            self.sems[e] = es.enter_context(nc.semaphore("cs_" + e))
        for i in range(self.ND):
            self.sems[("d", i)] = es.enter_context(nc.semaphore("ds%d" % i))
        self.duse = [0] * self.ND
        self.dnext = 0
        self.waited = {e: {} for e in self.ENG}
        self.final = []
        self.nops = 0

    def _wait(self, eng, tok):
        key, val = tok
        if key == eng and eng == "pe":
            return
        if self.waited[eng].get(key, 0) >= val:
            return
        self.waited[eng][key] = val
        self.ops[eng].append(("w", key, val))

    def op(self, eng, fn, reads=(), writes=(), dma=False, final=False):
        for r in reads:
            if r.w is not None:
                self._wait(eng, r.w)
            if r.excl:
                for t in r.r:
                    if t[0] != eng:
                        self._wait(eng, t)
        for w in writes:
            if w.w is not None:
                self._wait(eng, w.w)
            for t in w.r:
                self._wait(eng, t)
        if dma:
            s = self.dnext % self.ND
            self.dnext += 1
            u = self.duse[s]
            if u > 0:
                self._wait(eng, (("d", s), 16 * u))
            self.duse[s] = u + 1
            tok = (("d", s), 16 * (u + 1))
            self.ops[eng].append(("i", fn, ("d", s), 16))
        else:
            self.cnt[eng] += 1
            tok = (eng, self.cnt[eng])
            self.ops[eng].append(("i", fn, eng, 1))
        for r in reads:
            r.r.append(tok)
            if len(r.r) > 24:
                r.r = r.r[-24:] if False else r.r
        for w in writes:
            w.w = tok
            w.r = []
        if final:
            self.final.append(tok)
        self.nops += 1
        return tok

    def dma(self, eng, out, in_, reads=(), writes=(), final=False, **kw):
        return self.op(eng, lambda e: e.dma_start(out=out, in_=in_, **kw), reads, writes, dma=True, final=final)

    def barrier(self):
        toks = [(e, self.cnt[e]) for e in self.COMPUTE if self.cnt[e] > 0]
        toks += [(("d", s), 16 * self.duse[s]) for s in range(self.ND) if self.duse[s] > 0]
        for e in self.ENG:
            for t in toks:
                self._wait(e, t)

    def run(self):
        for t in self.final:
            self._wait("sp", t)
        nc = self.nc
        sems = self.sems

        def replay(eng_name):
            def body(e):
                for o in self.ops[eng_name]:
                    if o[0] == "w":
                        e.wait_ge(sems[o[1]], o[2])
                    else:
                        o[1](e).then_inc(sems[o[2]], o[3])
            return body

        with nc.Block() as block:
            block.tensor(replay("pe"))
            block.scalar(replay("act"))
            block.vector(replay("dve"))
            block.gpsimd(replay("pool"))
            block.sync(replay("sp"))


class Arena:
    def __init__(self, nc, es, words):
        self.t = es.enter_context(nc.sbuf_tensor("arena", [128, words], F32))
        self.words = words
        self.off = 0

    def alloc(self, free, dtype=F32, parts=128):
        n = 1
        for f in free:
            n *= f
        w = n if dtype != BF16 else (n + 1) // 2
        w = (w + 7) // 8 * 8
        assert self.off + w <= self.words, ("arena overflow", self.off, w, self.words)
        ap = self.t[0:parts, self.off:self.off + w]
        self.off += w
        if dtype == BF16:
            ap = ap.bitcast(BF16)
        elif dtype == I32:
            ap = ap.bitcast(I32)
        ap = ap[:, 0:n]
        if len(free) == 2:
            ap = ap.rearrange("p (a b) -> p a b", a=free[0], b=free[1])
        elif len(free) == 3:
            ap = ap.rearrange("p (a b c) -> p a b c", a=free[0], b=free[1], c=free[2])
        return ap

    def mark(self):
        return self.off

    def release(self, m):
        self.off = m


def bc(ap, shape):
    return ap.to_broadcast(list(shape))


def build_program(dbg=None):
    nc = bass.Bass("TRN2", target_bir_lowering=False)
    dbg_out = {}

    def din(name, shape, dt=F32):
        return nc.dram_tensor(name, list(shape), dt, kind="ExternalInput").ap()

    def scratch(name, shape, dt=F32, dump=False):
        kind = "ExternalOutput" if (dbg is not None and dump) else "Internal"
        t = nc.dram_tensor(name, list(shape), dt, kind=kind).ap()
        if kind == "ExternalOutput":
            dbg_out[name] = t
        return t

    xb = din("xb", [S, D])
    xo = din("xo", [OWN, D])
    cT = din("cT", [128, NKC])
    pos = din("pos", [128, NT], I32)
    w_ada = din("w_ada", [D, 6 * D])
    b_ada = din("b_ada", [1, 6 * D])
    g_pre1 = din("g_pre1", [128, NKC])
    g_pre2 = din("g_pre2", [128, NKC])
    g_post1 = din("g_post1", [1, D])
    g_post2 = din("g_post2", [1, D])
    w_in = din("w_in", [D, 7184])
    conv_w = din("conv_w", [128, 24, 4])
    a_log = din("a_log", [1, 8])
    dt_bias = din("dt_bias", [1, 8])
    dn_norm_w = din("dn_norm_w", [1, 128])
    rt_norm_w = din("rt_norm_w", [1, 1024])
    w_out = din("w_out", [D, D])
    w_router = din("w_router", [D, 64])
    router_bias = din("router_bias", [1, 64])
    if dbg in (None, "moe"):
        w_gate = din("w_gate", [65, D, 512])
        w_up = din("w_up", [65, D, 512])
        w_down = din("w_down", [65, 512, D])
    own_idx = din("own_idx", [128, 8], I32)
    c_ident = din("c_ident", [128, 128])
    c_utri = din("c_utri", [128, 128])
    c_maskS = din("c_maskS", [128, 128])
    c_maskT = din("c_maskT", [128, 128])
    c_onehot8 = din("c_onehot8", [128, 8, 128])
    c_rtmask = din("c_rtmask", [128, 8, 128])
    c_gq = din("c_gq", [64, 8, 128])
    c_gk = din("c_gk", [128, 8])
    c_gC = din("c_gC", [64, 8])
    c_theta = din("c_theta", [1, 32])
    c_mBD = din("c_mBD", [2, 128, 128])
    c_mC = din("c_mC", [8, 128, 128])

    out = nc.dram_tensor("out", [OWN, D], F32, kind="ExternalOutput").ap()

    mod_scr = scratch("mod_scr", [1, 6 * D], dump=True)
    pFM = scratch("pFM", [24, 128, S], dump=(dbg == "a1"))
    pTM = scratch("pTM", [S, 4112], dump=(dbg == "a1"))
    dn_qT = scratch("dn_qT", [8, 128, S], BF16)
    dn_kT = scratch("dn_kT", [8, 128, S], BF16)
    dn_ktm = scratch("dn_ktm", [S, 8, 128], BF16)
    dn_vtm = scratch("dn_vtm", [S, 8, 128], BF16)
    o_scr = scratch("o_scr", [S, D], F32, dump=(dbg in ("rt", "dn")))
    x1_scr = scratch("x1_scr", [OWN, D], dump=(dbg == "b"))
    dbg_a = scratch("dbg_a", [128, 2048], dump=True)

    with ExitStack() as es:
        P = Prog(nc, es)
        A = Arena(nc, es, 50 * 1024)
        psum = [es.enter_context(nc.psum_tensor("ps%d" % i, [128, 1024], F32)) for i in range(4)]
        PR = [[Res("ps%d_%d" % (i, k), excl=True) for k in range(2)] for i in range(4)]

        def pbank(i, k):
            return psum[i][:, k * 512:(k + 1) * 512]

        ident_f = A.alloc((128,))
        ident_b = A.alloc((128,), BF16)
        utri = A.alloc((128,))
        ones_f = A.alloc((128,))
        ones_b = A.alloc((128,), BF16)
        R_const = Res("const")
        P.dma("sp", ident_f, c_ident[:, :], writes=[R_const])
        P.dma("sp", utri, c_utri[:, :], writes=[R_const])
        R_c2 = Res("c2")
        P.op("act", lambda e: e.copy(ident_b, ident_f), [R_const], [R_c2])
        P.op("pool", lambda e: e.memset(ones_f, 1.0), [], [R_c2])
        P.op("pool", lambda e: e.memset(ones_b, 1.0), [], [R_c2])
        epsc = A.alloc((8,))
        P.op("pool", lambda e: e.memset(epsc, EPS), [], [R_c2])

        base_mark = A.mark()

        cTt = A.alloc((NKC,))
        cTb = A.alloc((NKC,), BF16)
        R_c = Res("c")
        P.dma("sp", cTt, cT[:, :], writes=[R_c])
        P.op("act", lambda e: e.activation(cTb, cTt, AF.Silu), [R_c], [R_c])
        wab = [A.alloc((NKC, 512), BF16) for _ in range(2)]
        R_wab = [Res("wab0"), Res("wab1")]
        modrow = A.alloc((6 * D,), parts=1)
        badar = A.alloc((6 * D,), parts=1)
        R_mod = Res("modrow")
        R_bada = Res("bada")
        P.dma("sp", badar, b_ada[:, :], writes=[R_bada])
        w_ada_v = w_ada.rearrange("(c p) n -> p c n", p=128)
        for jb in range(24):
            wb = wab[jb % 2]
            rw = R_wab[jb % 2]
            P.dma("pool", wb, w_ada_v[:, :, jb * 512:(jb + 1) * 512], writes=[rw])
            pi, pk = (jb % 4) // 2, jb % 2
            for kc in range(NKC):
                P.op("pe", lambda e, kc=kc, wb=wb, pi=pi, pk=pk: e.matmul(
                    pbank(pi, pk)[0:1, :], cTb[:, kc:kc + 1], wb[:, kc, :], start=(kc == 0), stop=(kc == NKC - 1)),
                    [R_c, rw], [PR[pi][pk]])
            P.op("dve", lambda e, jb=jb, pi=pi, pk=pk: e.tensor_tensor(
                modrow[:, jb * 512:(jb + 1) * 512], pbank(pi, pk)[0:1, :], badar[:, jb * 512:(jb + 1) * 512], ALU.add),
                [PR[pi][pk], R_bada], [R_mod])
        R_modscr = Res("modscr")
        P.dma("sp", mod_scr[:, :], modrow, reads=[R_mod], writes=[R_modscr])
        P.barrier()
        A.release(base_mark)

        modfm = A.alloc((96,))
        R_modfm = Res("modfm")
        P.dma("sp", modfm, mod_scr.rearrange("o (c p) -> p (o c)", p=128), reads=[R_modscr], writes=[R_modfm],
              allow_slow_non_contiguous=True)
        gp1 = A.alloc((NKC,))
        gp2 = A.alloc((NKC,))
        P.dma("sp", gp1, g_pre1[:, :], writes=[R_modfm])
        P.dma("sp", gp2, g_pre2[:, :], writes=[R_modfm])
        a1 = A.alloc((NKC,))
        a2 = A.alloc((NKC,))
        R_a = Res("a12")
        P.op("dve", lambda e: e.scalar_tensor_tensor(a1, modfm[:, 16:32], 1.0, gp1, ALU.add, ALU.mult), [R_modfm], [R_a])
        P.op("dve", lambda e: e.scalar_tensor_tensor(a2, modfm[:, 64:80], 1.0, gp2, ALU.add, ALU.mult), [R_modfm], [R_a])
        s1 = modfm[:, 0:16]
        s2 = modfm[:, 48:64]
        const_mark = A.mark()

        hT = A.alloc((NKC, S), BF16)
        R_hT = [Res("hT%d" % t) for t in range(NT)]
        hT_mark = A.mark()
        xt = [A.alloc((D,)) for _ in range(2)]
        R_xt = [Res("xt0"), Res("xt1")]
        sqj = A.alloc((D,), BF16)
        R_sqj = Res("sqj")
        xn = [A.alloc((D,), BF16) for _ in range(2)]
        R_xn = [Res("xn0"), Res("xn1")]
        ss = A.alloc((2, 2))
        R_ss = [Res("ss0"), Res("ss1")]
        tmpT = A.alloc((D,))
        R_tmpT = Res("tmpT")

        def rms_rstd(eng_sq, src, rss, ssap, reads):
            P.op("act", lambda e: e.activation(sqj, src, AF.Square, accum_out=ssap[:, 0:1]), reads, [R_sqj, rss])
            P.op("act", lambda e: e.activation(ssap[:, 1:2], ssap[:, 0:1], AF.Sqrt, bias=epsc[:, 0:1], scale=1.0 / D), [rss, R_c2], [rss])
            P.op("dve", lambda e: e.reciprocal(ssap[:, 0:1], ssap[:, 1:2]), [rss], [rss])

        for t in range(NT):
            x_ = xt[t % 2]
            rx = R_xt[t % 2]
            ssap = ss[:, t % 2, :]
            P.dma("sp", x_, xb[t * 128:(t + 1) * 128, :], writes=[rx])
            P.op("pool", lambda e, ssap=ssap: e.memset(ssap, 0.0), [], [R_ss[t % 2]])
            rms_rstd("act", x_, R_ss[t % 2], ssap, [rx])
            xn_ = xn[t % 2]
            P.op("dve", lambda e, x_=x_, xn_=xn_, ssap=ssap: e.tensor_scalar(xn_, x_, ssap[:, 0:1], None, ALU.mult),
                 [rx, R_ss[t % 2]], [R_xn[t % 2]])
            for half in range(2):
                pv = psum[half][:, :].bitcast(BF16)
                for q in range(8):
                    kc = half * 8 + q
                    P.op("pe", lambda e, pv=pv, q=q, kc=kc, xn_=xn_: e.transpose(
                        pv[:, q * 128:(q + 1) * 128], xn_[:, kc * 128:(kc + 1) * 128], ident_b),
                        [R_xn[t % 2], R_c2], [PR[half][0]])
                pvv = pv[:, 0:1024].rearrange("p (c n) -> p c n", c=8)
                tv = tmpT[:, half * 1024:(half + 1) * 1024].rearrange("p (c n) -> p c n", c=8)
                P.op("dve", lambda e, pvv=pvv, tv=tv, half=half: e.tensor_tensor(
                    tv, pvv, bc(a1[:, half * 8:(half + 1) * 8].unsqueeze(2), (128, 8, 128)), ALU.mult),
                    [PR[half][0], R_a], [R_tmpT])
                P.op("pool", lambda e, tv=tv, half=half, t=t: e.tensor_tensor(
                    hT[:, half * 8:(half + 1) * 8, t * 128:(t + 1) * 128], tv,
                    bc(s1[:, half * 8:(half + 1) * 8].unsqueeze(2), (128, 8, 128)), ALU.add),
                    [R_tmpT, R_modfm], [R_hT[t]])

        P.barrier()
        A.release(hT_mark)
        wfm = [A.alloc((NKC, 128), BF16) for _ in range(2)]
        R_wfm = [Res("wfm0"), Res("wfm1")]
        stg = [A.alloc((1024,)) for _ in range(3)]
        R_stg = [Res("stg%d" % i) for i in range(3)]
        gcnt = 0
        w_in_v = w_in.rearrange("(c p) n -> p c n", p=128)
        R_pFM = [Res("pFM%d" % c) for c in range(24)]
        pcnt = 0
        for cc in range(24):
            wt = wfm[cc % 2]
            rw = R_wfm[cc % 2]
            P.dma("pool", wt, w_in_v[:, :, cc * 128:(cc + 1) * 128], writes=[rw])
            for tb in range(8):
                if tb % 2 == 0:
                    st = stg[gcnt % 3]
                    rs = R_stg[gcnt % 3]
                    gcnt += 1
                pi, pk = (pcnt % 4) // 2 + 2, pcnt % 2
                pcnt += 1
                for kc in range(NKC):
                    P.op("pe", lambda e, kc=kc, wt=wt, tb=tb, pi=pi, pk=pk: e.matmul(
                        pbank(pi, pk), wt[:, kc, :], hT[:, kc, tb * 512:(tb + 1) * 512], start=(kc == 0), stop=(kc == NKC - 1)),
                        [rw] + R_hT[tb * 4:(tb + 1) * 4], [PR[pi][pk]])
                P.op("act", lambda e, st=st, tb=tb, pi=pi, pk=pk: e.copy(st[:, (tb % 2) * 512:(tb % 2 + 1) * 512], pbank(pi, pk)),
                     [PR[pi][pk]], [rs])
                if tb % 2 == 1:
                    P.dma("sp", pFM[cc][:, (tb - 1) * 512:(tb + 1) * 512], st, reads=[rs], writes=[R_pFM[cc]])
        P.barrier()
        A.release(hT_mark)
        wtm = [A.alloc((NKC, 512), BF16) for _ in range(2)]
        R_wtm = [Res("wtm0"), Res("wtm1")]
        R_pTM = Res("pTM")
        stq = [A.alloc((512,)) for _ in range(4)]
        R_stq = [Res("stq%d" % i) for i in range(4)]
        scnt = 0
        for cb in range(9):
            ncol = 512 if cb < 8 else 16
            c0 = 3072 + cb * 512
            wt = wtm[cb % 2]
            rw = R_wtm[cb % 2]
            P.dma("pool", wt[:, :, 0:ncol], w_in_v[:, :, c0:c0 + ncol], writes=[rw])
            for t in range(NT):
                pi, pk = (pcnt % 4) // 2 + 2, pcnt % 2
                pcnt += 1
                for kc in range(NKC):
                    P.op("pe", lambda e, kc=kc, wt=wt, t=t, pi=pi, pk=pk, ncol=ncol: e.matmul(
                        pbank(pi, pk)[:, 0:ncol], hT[:, kc, t * 128:(t + 1) * 128], wt[:, kc, 0:ncol],
                        start=(kc == 0), stop=(kc == NKC - 1)),
                        [rw, R_hT[t]], [PR[pi][pk]])
                sq_ = stq[scnt % 4]
                rq = R_stq[scnt % 4]
                scnt += 1
                P.op("act" if t % 2 == 0 else "dve", lambda e, sq_=sq_, pi=pi, pk=pk, ncol=ncol, t=t: (
                    e.copy(sq_[:, 0:ncol], pbank(pi, pk)[:, 0:ncol]) if t % 2 == 0 else
                    e.tensor_copy(sq_[:, 0:ncol], pbank(pi, pk)[:, 0:ncol])),
                    [PR[pi][pk]], [rq])
                P.dma("sp", pTM[t * 128:(t + 1) * 128, cb * 512:cb * 512 + ncol], sq_[:, 0:ncol], reads=[rq], writes=[R_pTM])
        P.barrier()
        A.release(const_mark)
        if dbg == "a1":
            P.dma("sp", out[0:128, 0:1024], hT[:, 0, 0:2048].bitcast(F32), final=True)
            P.run()
            return nc, dbg_out


        R_oscr = Res("oscr")
        rt_mark = A.mark()
        theta_bc = A.alloc((32,))
        posi = A.alloc((NT,), I32)
        posf = A.alloc((NT,))
        ang = A.alloc((NT, 32))
        tmpa = A.alloc((NT, 32))
        sinT = A.alloc((NT, 32))
        cosT = A.alloc((NT, 32))
        R_tab = Res("rt_tab")
        P.dma("sp", theta_bc, c_theta.partition_broadcast(128), writes=[R_tab])
        P.dma("sp", posi, pos[:, :], writes=[R_tab])
        P.op("dve", lambda e: e.tensor_copy(posf, posi), [R_tab], [R_tab])
        P.op("dve", lambda e: e.tensor_tensor(ang, bc(posf.unsqueeze(2), (128, NT, 32)),
                                              bc(theta_bc.unsqueeze(1), (128, NT, 32)), ALU.mult), [R_tab], [R_tab])
        TWO_PI = 2.0 * np.pi
        pic = A.alloc((8,))
        P.op("pool", lambda e: e.memset(pic, -np.pi), [], [R_tab])
        ki = A.alloc((NT, 32), I32)
        kf = A.alloc((NT, 32))

        def sin_table(dst, shift):
            P.op("dve", lambda e: e.tensor_scalar(tmpa, ang, shift, 1.0 / TWO_PI, ALU.add, ALU.mult), [R_tab], [R_tab])
            P.op("dve", lambda e: e.tensor_copy(ki, tmpa), [R_tab], [R_tab])
            P.op("dve", lambda e: e.tensor_copy(kf, ki), [R_tab], [R_tab])
            P.op("dve", lambda e: e.tensor_scalar(tmpa, ang, shift, None, ALU.add), [R_tab], [R_tab])
            P.op("dve", lambda e: e.scalar_tensor_tensor(tmpa, kf, -TWO_PI, tmpa, ALU.mult, ALU.add), [R_tab], [R_tab])
            P.op("dve", lambda e: e.tensor_scalar(kf, tmpa, np.pi, TWO_PI, ALU.is_gt, ALU.mult), [R_tab], [R_tab])
            P.op("dve", lambda e: e.tensor_tensor(tmpa, tmpa, kf, ALU.subtract), [R_tab], [R_tab])
            P.op("dve", lambda e: e.tensor_scalar(kf, tmpa, -np.pi, TWO_PI, ALU.is_lt, ALU.mult), [R_tab], [R_tab])
            P.op("dve", lambda e: e.tensor_tensor(tmpa, tmpa, kf, ALU.add), [R_tab], [R_tab])
            P.op("act", lambda e: e.activation(dst, tmpa, AF.Sin), [R_tab], [R_tab])

        sin_table(sinT, 0.0)
        sin_table(cosT, 0.5 * np.pi)
        rtmask = A.alloc((8, 128))
        gq = A.alloc((8, 128), parts=64)
        gk = A.alloc((8,))
        gC = A.alloc((8,), parts=64)
        rtw = A.alloc((1024,))
        P.dma("sp", rtmask, c_rtmask[:, :, :], writes=[R_tab])
        P.dma("sp", gq, c_gq[:, :, :], writes=[R_tab])
        P.dma("sp", gk, c_gk[:, :], writes=[R_tab])
        P.dma("sp", gC, c_gC[:, :], writes=[R_tab])
        P.dma("sp", rtw, rt_norm_w.partition_broadcast(128), writes=[R_tab])
        Sst = A.alloc((8, 128), parts=64)
        Sbf = A.alloc((8, 128), BF16, parts=64)
        R_S = Res("S")
        R_Sbf = Res("Sbf")
        P.op("pool", lambda e: e.memset(Sst, 0.0), [], [R_S])
        P.op("pool", lambda e: e.memset(Sbf, 0.0), [], [R_Sbf])
        ld = [A.alloc((3072,)) for _ in range(2)]
        R_ld = [Res("ld0"), Res("ld1")]
        qr = A.alloc((8, 64), BF16)
        kr = A.alloc((8, 64), BF16)
        kd = A.alloc((8, 64), BF16)
        vb = A.alloc((8, 128), BF16)
        sgt = A.alloc((1024,))
        ta = A.alloc((8, 32))
        tb_ = A.alloc((8, 32))
        qT = A.alloc((8, 128), BF16, parts=64)
        qdT = A.alloc((8, 128), BF16, parts=64)
        kT = A.alloc((8, 128), BF16, parts=64)
        PT = A.alloc((8, 128), BF16)
        osb = A.alloc((8, 128))
        sqb = A.alloc((8, 128))
        st8 = A.alloc((6, 8))
        R_qr, R_kr, R_kd, R_vb, R_sg, R_ta, R_tb, R_qT, R_qdT, R_kT, R_PT, R_osb, R_sqb, R_st8 = [Res() for _ in range(14)]

        def rotary(src, dst, rdst, t, rl):
            x1 = src[:, :, 0:32]
            x2 = src[:, :, 32:64]
            cs = bc(cosT[:, t, :].unsqueeze(1), (128, 8, 32))
            sn = bc(sinT[:, t, :].unsqueeze(1), (128, 8, 32))
            P.op("dve", lambda e: e.tensor_tensor(ta, x1, cs, ALU.mult), [rl, R_tab], [R_ta])
            P.op("pool", lambda e: e.tensor_tensor(tb_, x2, sn, ALU.mult), [rl, R_tab], [R_tb])
            P.op("dve", lambda e: e.tensor_tensor(dst[:, :, 0:32], ta, tb_, ALU.subtract), [R_ta, R_tb], [rdst])
            P.op("dve", lambda e: e.tensor_tensor(ta, x2, cs, ALU.mult), [rl, R_tab], [R_ta])
            P.op("pool", lambda e: e.tensor_tensor(tb_, x1, sn, ALU.mult), [rl, R_tab], [R_tb])
            P.op("dve", lambda e: e.tensor_tensor(dst[:, :, 32:64], ta, tb_, ALU.add), [R_ta, R_tb], [rdst])

        for t in range(NT):
            l_ = ld[t % 2]
            rl = R_ld[t % 2]
            P.dma("sp", l_, pTM[t * 128:(t + 1) * 128, 1024:4096], reads=[R_pTM], writes=[rl])
            qv = l_[:, 0:512].rearrange("p (h d) -> p h d", h=8)
            kv = l_[:, 512:1024].rearrange("p (h d) -> p h d", h=8)
            vv = l_[:, 1024:2048].rearrange("p (h d) -> p h d", h=8)
            gv = l_[:, 2048:3072]
            rotary(qv, qr, R_qr, t, rl)
            rotary(kv, kr, R_kr, t, rl)
            P.op("pool", lambda e: e.tensor_tensor(kd, kr, bc(gk.unsqueeze(2), (128, 8, 64)), ALU.mult), [R_kr, R_tab], [R_kd])
            P.op("act", lambda e, vv=vv: e.copy(vb, vv), [rl], [R_vb])
            P.op("act", lambda e, gv=gv: e.activation(sgt, gv, AF.Silu), [rl], [R_sg])
            pq = psum[0][:, 0:512].bitcast(BF16)
            pk_ = psum[0][:, 512:1024].bitcast(BF16)
            for h in range(8):
                P.op("pe", lambda e, h=h: e.transpose(pq[0:64, h * 128:(h + 1) * 128], qr[:, h, :], ident_b), [R_qr, R_c2], [PR[0][0]])
            for h in range(8):
                P.op("pe", lambda e, h=h: e.transpose(pk_[0:64, h * 128:(h + 1) * 128], kr[:, h, :], ident_b), [R_kr, R_c2], [PR[0][1]])
            pq3 = pq[0:64, :].rearrange("p (h n) -> p h n", h=8)
            pk3 = pk_[0:64, :].rearrange("p (h n) -> p h n", h=8)
            P.op("act", lambda e: e.copy(qT, pq3), [PR[0][0]], [R_qT])
            P.op("dve", lambda e: e.tensor_tensor(qdT, pq3, gq, ALU.mult), [PR[0][0], R_tab], [R_qdT])
            P.op("act", lambda e: e.copy(kT, pk3), [PR[0][1]], [R_kT])
            for h in range(8):
                P.op("pe", lambda e, h=h: e.matmul(psum[1][:, h * 128:(h + 1) * 128], kT[:, h, :], qT[:, h, :], start=True, stop=True),
                     [R_kT, R_qT], [PR[1][h // 4]])
            for k in range(2):
                P.op("dve", lambda e, k=k: e.tensor_tensor(
                    PT[:, k * 4:(k + 1) * 4, :], psum[1][:, k * 512:(k + 1) * 512].rearrange("p (h n) -> p h n", h=4),
                    rtmask[:, k * 4:(k + 1) * 4, :], ALU.mult), [PR[1][k], R_tab], [R_PT])
            for h in range(8):
                P.op("pe", lambda e, h=h: e.matmul(psum[2][:, h * 128:(h + 1) * 128], PT[:, h, :], vb[:, h, :], start=True, stop=False),
                     [R_PT, R_vb], [PR[2][h // 4]])
                P.op("pe", lambda e, h=h: e.matmul(psum[2][:, h * 128:(h + 1) * 128], qdT[:, h, :], Sbf[:, h, :], start=False, stop=True),
                     [R_qdT, R_Sbf], [PR[2][h // 4]])
            for h in range(8):
                P.op("pe", lambda e, h=h: e.matmul(psum[3][0:64, h * 128:(h + 1) * 128], kd[:, h, :], vb[:, h, :], start=True, stop=True),
                     [R_kd, R_vb], [PR[3][h // 4]])
            P.op("dve", lambda e: e.tensor_tensor(Sst, Sst, bc(gC.unsqueeze(2), (64, 8, 128)), ALU.mult), [R_tab], [R_S])
            for k in range(2):
                P.op("dve", lambda e, k=k: e.tensor_tensor(
                    Sst[:, k * 4:(k + 1) * 4, :], Sst[:, k * 4:(k + 1) * 4, :],
                    psum[3][0:64, k * 512:(k + 1) * 512].rearrange("p (h n) -> p h n", h=4), ALU.add), [PR[3][k]], [R_S])
            P.op("act", lambda e: e.copy(Sbf, Sst), [R_S], [R_Sbf])
            for k in range(2):
                P.op("act", lambda e, k=k: e.copy(osb[:, k * 4:(k + 1) * 4, :],
                                                  psum[2][:, k * 512:(k + 1) * 512].rearrange("p (h n) -> p h n", h=4)),
                     [PR[2][k]], [R_osb])
            P.op("dve", lambda e: e.tensor_reduce(st8[:, 0, :], osb, AX.X, ALU.add), [R_osb], [R_st8])
            P.op("pool", lambda e: e.tensor_tensor(sqb, osb, osb, ALU.mult), [R_osb], [R_sqb])
            P.op("dve", lambda e: e.tensor_reduce(st8[:, 1, :], sqb, AX.X, ALU.add), [R_sqb], [R_st8])
            P.op("dve", lambda e: e.tensor_scalar(st8[:, 2, :], st8[:, 0, :], 1.0 / 128, None, ALU.mult), [R_st8], [R_st8])
            P.op("dve", lambda e: e.tensor_tensor(st8[:, 3, :], st8[:, 2, :], st8[:, 2, :], ALU.mult), [R_st8], [R_st8])
            P.op("dve", lambda e: e.scalar_tensor_tensor(st8[:, 4, :], st8[:, 1, :], 1.0 / 128, st8[:, 3, :], ALU.mult, ALU.subtract),
                 [R_st8], [R_st8])
            P.op("act", lambda e: e.activation(st8[:, 5, :], st8[:, 4, :], AF.Sqrt, bias=epsc[:, 0:1], scale=1.0), [R_st8, R_c2], [R_st8])
            P.op("dve", lambda e: e.reciprocal(st8[:, 4, :], st8[:, 5, :]), [R_st8], [R_st8])
            P.op("dve", lambda e: e.tensor_tensor(osb, osb, bc(st8[:, 2, :].unsqueeze(2), (128, 8, 128)), ALU.subtract), [R_st8], [R_osb])
            P.op("dve", lambda e: e.tensor_tensor(osb, osb, bc(st8[:, 4, :].unsqueeze(2), (128, 8, 128)), ALU.mult), [R_st8], [R_osb])
            osf = osb.rearrange("p h n -> p (h n)")
            P.op("pool", lambda e, osf=osf: e.tensor_tensor(osf, osf, rtw, ALU.mult), [R_tab], [R_osb])
            P.op("pool", lambda e, osf=osf: e.tensor_tensor(sqb.rearrange("p h n -> p (h n)"), osf, sgt, ALU.mult), [R_osb, R_sg], [R_sqb])
            P.dma("sp", o_scr[t * 128:(t + 1) * 128, 1024:2048], sqb.rearrange("p h n -> p (h n)"), reads=[R_sqb], writes=[R_oscr])
        P.barrier()
        A.release(rt_mark)
        if dbg == "rt":
            P.dma("sp", out[0:128, 0:1024], rtw, final=True)
            P.run()
            return nc, dbg_out


        dn_mark = A.mark()
        R_dt = Res("dn_tab")
        ab = A.alloc((NT, 16))
        for q4 in range(4):
            P.dma("sp", ab[:, q4 * 8:(q4 + 1) * 8, :], pTM[q4 * 1024:(q4 + 1) * 1024, 4096:4112].rearrange("(t p) c -> p t c", p=128),
                  reads=[R_pTM], writes=[R_dt])
        dtb = A.alloc((8,))
        alg = A.alloc((8,))
        P.dma("sp", dtb, dt_bias.partition_broadcast(128), writes=[R_dt])
        P.dma("sp", alg, a_log.partition_broadcast(128), writes=[R_dt])
        maskS = A.alloc((128,))
        maskT = A.alloc((128,))
        oh8 = A.alloc((8, 128))
        dnw = A.alloc((128,))
        mBD = A.alloc((128,))
        mBDT = A.alloc((128,))
        mCs = [A.alloc((128,)) for _ in range(4)]
        mCTs = [A.alloc((128,)) for _ in range(4)]
        P.dma("sp", mBD, c_mBD[0], writes=[R_dt])
        P.dma("sp", mBDT, c_mBD[1], writes=[R_dt])
        for bi in range(4):
            P.dma("sp", mCs[bi], c_mC[bi], writes=[R_dt])
            P.dma("sp", mCTs[bi], c_mC[4 + bi], writes=[R_dt])
        P.dma("sp", maskS, c_maskS[:, :], writes=[R_dt])
        P.dma("sp", maskT, c_maskT[:, :], writes=[R_dt])
        P.dma("sp", oh8, c_onehot8[:, :, :], writes=[R_dt])
        P.dma("sp", dnw, dn_norm_w.partition_broadcast(128), writes=[R_dt])
        xg = A.alloc((NT, 8))
        t1_ = A.alloc((NT, 8))
        t2_ = A.alloc((NT, 8))
        gg = A.alloc((NT, 8))
        beta = A.alloc((NT, 8))
        negb = A.alloc((NT, 8))
        gc_ = A.alloc((NT, 8))
        glb = A.alloc((NT, 8))
        egc = A.alloc((NT, 8))
        kds = A.alloc((NT, 8))
        ecd = A.alloc((NT, 8))
        bw = A.alloc((NT, 8))
        nA = A.alloc((8,))
        gcT = A.alloc((S,))
        P.op("pool", lambda e: e.memset(gcT, 0.0), [], [R_dt])
        av = ab[:, :, 0:8]
        bv = ab[:, :, 8:16]
        D_ = lambda fn, rd=(), wr=(): P.op("dve", fn, list(rd) + [R_dt], list(wr) + [R_dt])
        A_ = lambda fn, rd=(), wr=(): P.op("act", fn, list(rd) + [R_dt], list(wr) + [R_dt])
        D_(lambda e: e.tensor_tensor(xg, av, bc(dtb.unsqueeze(1), (128, NT, 8)), ALU.add))
        A_(lambda e: e.activation(t1_, xg, AF.Abs))
        A_(lambda e: e.activation(t2_, t1_, AF.Exp, scale=-1.0))
        A_(lambda e: e.activation(t1_, t2_, AF.Ln, bias=ones_f[:, 0:1], scale=1.0))
        D_(lambda e: e.scalar_tensor_tensor(t2_, xg, 0.0, t1_, ALU.max, ALU.add))
        A_(lambda e: e.activation(nA, alg, AF.Exp))
        D_(lambda e: e.tensor_scalar(nA, nA, -1.0, None, ALU.mult))
        D_(lambda e: e.tensor_tensor(gg, t2_, bc(nA.unsqueeze(1), (128, NT, 8)), ALU.mult))
        A_(lambda e: e.activation(beta, bv, AF.Sigmoid))
        D_(lambda e: e.tensor_scalar(negb, beta, -1.0, None, ALU.mult))
        gflat = gg.rearrange("p t h -> p (t h)")
        P.op("pe", lambda e: e.matmul(psum[0][:, 0:256], utri, gflat, start=True, stop=True), [R_dt, R_const], [PR[0][0]])
        P.op("pe", lambda e: e.matmul(psum[0][:, 512:768], ones_f, gflat, start=True, stop=True), [R_dt, R_c2], [PR[0][1]])
        A_(lambda e: e.copy(gc_.rearrange("p t h -> p (t h)"), psum[0][:, 0:256]), [PR[0][0]])
        A_(lambda e: e.copy(glb.rearrange("p t h -> p (t h)"), psum[0][:, 512:768]), [PR[0][1]])
        A_(lambda e: e.activation(egc, gc_, AF.Exp))
        A_(lambda e: e.activation(ecd, glb, AF.Exp))
        D_(lambda e: e.tensor_tensor(t1_, glb, gc_, ALU.subtract))
        A_(lambda e: e.activation(kds, t1_, AF.Exp))
        D_(lambda e: e.tensor_tensor(bw, beta, egc, ALU.mult))
        for grp in range(4):
            pi = 1 + grp % 2
            for q in range(8):
                t = grp * 8 + q
                P.op("pe", lambda e, t=t, q=q, pi=pi: e.matmul(psum[pi][0:8, q * 128:(q + 1) * 128], gg[:, t, :], utri, start=True, stop=True),
                     [R_dt, R_const], [PR[pi][q // 4]])
            A_(lambda e, grp=grp, pi=pi: e.copy(gcT[0:8, grp * 1024:grp * 1024 + 512], psum[pi][0:8, 0:512]), [PR[pi][0]])
            A_(lambda e, grp=grp, pi=pi: e.copy(gcT[0:8, grp * 1024 + 512:(grp + 1) * 1024], psum[pi][0:8, 512:1024]), [PR[pi][1]])

        p1_mark = A.mark()
        cwt = A.alloc((24, 4))
        P.dma("sp", cwt, conv_w[:, :, :], writes=[R_dt])
        raw = A.alloc((3, S + 3))
        cv = A.alloc((3, S))
        qkn = A.alloc((2, S), BF16)
        vb16 = A.alloc((S,), BF16)
        sq5 = A.alloc((512,), BF16)
        rs5 = A.alloc((512,))
        tmst = A.alloc((2, NT, 128), BF16)
        R_raw, R_cv, R_qkn, R_vb16, R_sq5, R_rs5, R_tmst = [Res() for _ in range(7)]
        R_dnq, R_dnk, R_ktm, R_vtm = Res(), Res(), Res(), Res()
        P.op("pool", lambda e: e.memset(raw[:, :, 0:3], 0.0), [], [R_raw])
        for h in range(8):
            for i in range(3):
                P.dma("sp", raw[:, i, 3:S + 3], pFM[i * 8 + h], reads=[R_pFM[i * 8 + h]], writes=[R_raw])
            for i in range(3):
                cc = i * 8 + h
                eng = "dve"
                P.op(eng, lambda e, i=i, cc=cc: e.tensor_scalar(cv[:, i, :], raw[:, i, 0:S], cwt[:, cc, 0:1], None, ALU.mult), [R_raw, R_dt], [R_cv])
                for jj in range(1, 4):
                    P.op(eng, lambda e, i=i, cc=cc, jj=jj: e.scalar_tensor_tensor(
                        cv[:, i, :], raw[:, i, jj:S + jj], cwt[:, cc, jj:jj + 1], cv[:, i, :], ALU.mult, ALU.add), [R_raw, R_dt], [R_cv])
                P.op("act", lambda e, i=i: e.activation(cv[:, i, :], cv[:, i, :], AF.Silu), [], [R_cv])
            for i in range(2):
                for blk in range(8):
                    sl = slice(blk * 512, (blk + 1) * 512)
                    pi, pk = 3, blk % 2
                    P.op("act", lambda e, i=i, sl=sl: e.activation(sq5, cv[:, i, sl], AF.Square), [R_cv], [R_sq5])
                    P.op("pe", lambda e, pi=pi, pk=pk: e.matmul(pbank(pi, pk), ones_b, sq5, start=True, stop=True), [R_sq5, R_c2], [PR[pi][pk]])
                    P.op("act", lambda e, pi=pi, pk=pk: e.activation(rs5, pbank(pi, pk), AF.Sqrt, bias=epsc[:, 0:1], scale=1.0), [PR[pi][pk], R_c2], [R_rs5])
                    P.op("dve", lambda e: e.reciprocal(rs5, rs5), [], [R_rs5])
                    scl = (128.0 ** -0.5) if i == 0 else 1.0
                    P.op("dve", lambda e, i=i, sl=sl, scl=scl: e.scalar_tensor_tensor(qkn[:, i, sl], cv[:, i, sl], scl, rs5, ALU.mult, ALU.mult),
                         [R_cv, R_rs5], [R_qkn])
            P.op("pool", lambda e: e.tensor_copy(vb16, cv[:, 2, :]), [R_cv], [R_vb16])
            P.dma("sp", dn_qT[h], qkn[:, 0, :], reads=[R_qkn], writes=[R_dnq])
            P.dma("sp", dn_kT[h], qkn[:, 1, :], reads=[R_qkn], writes=[R_dnk])
            for which, src, rsrc in ((0, qkn[:, 1, :], R_qkn), (1, vb16, R_vb16)):
                for g4 in range(4):
                    pi, pk = g4 % 2, g4 // 2
                    pv = psum[pi][:, pk * 512:(pk + 1) * 512].bitcast(BF16)
                    for q in range(8):
                        t = g4 * 8 + q
                        P.op("pe", lambda e, pv=pv, q=q, t=t, src=src: e.transpose(pv[:, q * 128:(q + 1) * 128], src[:, t * 128:(t + 1) * 128], ident_b),
                             [rsrc, R_c2], [PR[pi][pk]])
                    P.op("act" if g4 % 2 == 0 else "dve", lambda e, pv=pv, g4=g4, which=which: (
                        e.copy if g4 % 2 == 0 else e.tensor_copy)(tmst[:, which, g4 * 8:(g4 + 1) * 8, :], pv.rearrange("p (t d) -> p t d", t=8)),
                        [PR[pi][pk]], [R_tmst])
            for q4 in range(4):
                P.dma("sp", dn_ktm[q4 * 1024:(q4 + 1) * 1024].rearrange("(t p) h d -> p t h d", p=128)[:, :, h, :],
                      tmst[:, 0, q4 * 8:(q4 + 1) * 8, :], reads=[R_tmst], writes=[R_ktm])
                P.dma("sp", dn_vtm[q4 * 1024:(q4 + 1) * 1024].rearrange("(t p) h d -> p t h d", p=128)[:, :, h, :],
                      tmst[:, 1, q4 * 8:(q4 + 1) * 8, :], reads=[R_tmst], writes=[R_vtm])
        P.barrier()
        A.release(p1_mark)

        NG = DN_GROUPS
        HG = 8 // NG
        qTt = [A.alloc((8, 128), BF16) for _ in range(2)]
        kTt = [A.alloc((8, 128), BF16) for _ in range(2)]
        ktm = [A.alloc((8, 128), BF16) for _ in range(2)]
        vtm = [A.alloc((8, 128), BF16) for _ in range(2)]
        zt = [A.alloc((1024,)) for _ in range(2)]
        R_in = [Res("dnin0"), Res("dnin1")]
        bf_names = ("Bm", "attnT", "qgT", "Xw", "Xu", "kdec", "Pa", "Pb", "Ma", "Mb", "Mta", "Mtb", "WT", "vnew",
                    "Da", "Db", "Bd", "Bdt", "Cb", "Cbt", "G", "G2", "Mtc")
        f_names = ("dd", "dS_", "GS", "GT", "KKg", "egb", "U", "osb2", "sq2", "szl")
        T_ = {}
        RR = [dict() for _ in range(NG)]
        for nme in bf_names + f_names:
            T_[nme] = A.alloc((8, 128), BF16 if nme in bf_names else F32)
            for g in range(NG):
                RR[g][nme] = Res(nme + str(g))
        st9 = A.alloc((4, 8))
        R_st9 = [Res("st9_%d" % g) for g in range(NG)]
        Sd = A.alloc((8, 128))
        Sdb = A.alloc((8, 128), BF16)
        R_Sd = [Res("Sd%d" % g) for g in range(NG)]
        R_Sdb = [Res("Sdb%d" % g) for g in range(NG)]
        P.op("pool", lambda e: e.memset(Sd, 0.0), [], R_Sd)
        P.op("pool", lambda e: e.memset(Sdb, 0.0), [], R_Sdb)
        bkc = [0]

        def nbk():
            n = bkc[0] % 8
            bkc[0] += 1
            return n

        def pb_(n):
            return psum[n // 2][:, (n % 2) * 512:(n % 2 + 1) * 512]

        def prb(n):
            return PR[n // 2][n % 2]

        def dn_loads(t):
            b_ = t % 2
            rin = R_in[b_]
            tsl = slice(t * 128, (t + 1) * 128)
            P.dma("sp", qTt[b_], dn_qT[:, :, tsl].rearrange("h d n -> d h n"), reads=[R_dnq], writes=[rin])
            P.dma("sp", kTt[b_], dn_kT[:, :, tsl].rearrange("h d n -> d h n"), reads=[R_dnk], writes=[rin])
            P.dma("sp", ktm[b_], dn_ktm[tsl], reads=[R_ktm], writes=[rin])
            P.dma("sp", vtm[b_], dn_vtm[tsl], reads=[R_vtm], writes=[rin])
            P.dma("sp", zt[b_], pTM[tsl, 0:1024], reads=[R_pTM], writes=[rin])

        def dn_gen(t, g):
            b_ = t % 2
            rin = R_in[b_]
            tsl = slice(t * 128, (t + 1) * 128)
            hsl = slice(g * HG, (g + 1) * HG)
            rr = RR[g]
            q_, k_, km_, vm_ = qTt[b_], kTt[b_], ktm[b_], vtm[b_]
            Tg = lambda nme: T_[nme][:, hsl, :]
            pvh = lambda n: pb_(n)[:, 0:HG * 128].rearrange("p (h n) -> p h n", h=HG)
            bch = lambda ap2: bc(ap2[:, hsl].unsqueeze(2), (128, HG, 128))
            bcm = lambda m: bc(m.unsqueeze(1), (128, HG, 128))
            I8 = bc(ident_b.unsqueeze(1), (128, HG, 128))

            def mmg(n, lhs, rhs, reads):
                for hl in range(HG):
                    h = g * HG + hl
                    l_ap, r_ap = lhs(h), rhs(h)
                    P.op("pe", lambda e, hl=hl, l_ap=l_ap, r_ap=r_ap: e.matmul(pb_(n)[:, hl * 128:(hl + 1) * 128], l_ap, r_ap, start=True, stop=True),
                         reads, [prb(n)])

            def ew(eng, fn, reads, writes):
                P.op(eng, fn, reads, writes)

            nKK, nQK, nBC = nbk(), nbk(), nbk()
            mmg(nKK, lambda h: k_[:, h, :], lambda h: k_[:, h, :], [rin])
            mmg(nQK, lambda h: k_[:, h, :], lambda h: q_[:, h, :], [rin])
            mmg(nBC, lambda h: oh8[:, h, :], lambda h: gcT[:, tsl], [R_dt])
            yield
            ew("dve", lambda e: e.tensor_tensor(Tg("dd"), pvh(nBC), bch(gc_[:, t, :]), ALU.subtract), [prb(nBC), R_dt], [rr["dd"]])
            ew("act", lambda e: e.activation(Tg("egb"), pvh(nBC), AF.Exp), [prb(nBC)], [rr["egb"]])
            ew("dve", lambda e: e.tensor_scalar(Tg("dS_"), Tg("dd"), 0.0, None, ALU.max), [rr["dd"]], [rr["dS_"]])
            ew("act", lambda e: e.activation(Tg("GS"), Tg("dS_"), AF.Exp, scale=-1.0), [rr["dS_"]], [rr["GS"]])
            ew("dve", lambda e: e.tensor_scalar(Tg("dS_"), Tg("dd"), 0.0, None, ALU.min), [rr["dd"], rr["GS"]], [rr["dS_"]])
            ew("act", lambda e: e.activation(Tg("GT"), Tg("dS_"), AF.Exp), [rr["dS_"]], [rr["GT"]])
            ew("pool", lambda e: e.tensor_tensor(Tg("GS"), Tg("GS"), bcm(maskS), ALU.mult), [R_dt], [rr["GS"]])
            ew("pool", lambda e: e.tensor_tensor(Tg("GT"), Tg("GT"), bcm(maskT), ALU.mult), [R_dt], [rr["GT"]])
            ew("dve", lambda e: e.tensor_tensor(Tg("KKg"), pvh(nKK), Tg("GS"), ALU.mult), [prb(nKK), rr["GS"]], [rr["KKg"]])
            ew("pool", lambda e: e.tensor_tensor(Tg("Bm"), Tg("KKg"), bch(negb[:, t, :]), ALU.mult), [rr["KKg"], R_dt], [rr["Bm"]])
            ew("dve", lambda e: e.tensor_tensor(Tg("attnT"), pvh(nQK), Tg("GT"), ALU.mult), [prb(nQK), rr["GT"]], [rr["attnT"]])
            ew("pool", lambda e: e.tensor_tensor(Tg("qgT"), q_[:, hsl, :], Tg("egb"), ALU.mult), [rin, rr["egb"]], [rr["qgT"]])
            ew("pool", lambda e: e.tensor_tensor(Tg("Xw"), km_[:, hsl, :], bch(bw[:, t, :]), ALU.mult), [rin, R_dt], [rr["Xw"]])
            ew("pool", lambda e: e.tensor_tensor(Tg("Xu"), vm_[:, hsl, :], bch(beta[:, t, :]), ALU.mult), [rin, R_dt], [rr["Xu"]])
            ew("pool", lambda e: e.tensor_tensor(Tg("kdec"), km_[:, hsl, :], bch(kds[:, t, :]), ALU.mult), [rin, R_dt], [rr["kdec"]])
            yield
            nTr = nbk()
            ptv = pb_(nTr).bitcast(BF16)
            for hl in range(HG):
                h = g * HG + hl
                P.op("pe", lambda e, hl=hl, h=h: e.transpose(ptv[:, hl * 128:(hl + 1) * 128], T_["Bm"][:, h, :], ident_b), [rr["Bm"], R_c2], [prb(nTr)])
            ew("act", lambda e: e.copy(Tg("Mta"), ptv[:, 0:HG * 128].rearrange("p (h n) -> p h n", h=HG)), [prb(nTr)], [rr["Mta"]])
            ew("dve", lambda e: e.tensor_tensor(Tg("Bd"), Tg("Bm"), bcm(mBD), ALU.mult), [rr["Bm"], R_dt], [rr["Bd"]])
            ew("pool", lambda e: e.tensor_tensor(Tg("Bdt"), Tg("Mta"), bcm(mBDT), ALU.mult), [rr["Mta"], R_dt], [rr["Bdt"]])
            ew("dve", lambda e: e.tensor_tensor(Tg("Pa"), Tg("Bdt"), I8, ALU.add), [rr["Bdt"], R_c2], [rr["Pa"]])
            ew("pool", lambda e: e.tensor_tensor(Tg("Da"), Tg("Bd"), I8, ALU.add), [rr["Bd"], R_c2], [rr["Da"]])
            yield
            Mc, Mtc, Ec, Dc = "Bd", "Bdt", "Pa", "Da"
            for lev in range(2):
                Mn = "Ma" if Mc != "Ma" else "Mb"
                Mtn = "Mtb" if Mtc != "Mtb" else "Mtc"
                En = "Pb" if Ec != "Pb" else "Pa"
                Dn = "Db" if Dc != "Db" else "Da"
                nM, nMt = nbk(), nbk()
                mmg(nM, lambda h, Mtc=Mtc: T_[Mtc][:, h, :], lambda h, Mc=Mc: T_[Mc][:, h, :], [rr[Mtc], rr[Mc]])
                mmg(nMt, lambda h, Mc=Mc: T_[Mc][:, h, :], lambda h, Mtc=Mtc: T_[Mtc][:, h, :], [rr[Mtc], rr[Mc]])
                ew("act", lambda e, Mn=Mn, nM=nM: e.copy(Tg(Mn), pvh(nM)), [prb(nM)], [rr[Mn]])
                ew("dve", lambda e, Mtn=Mtn, nMt=nMt: e.tensor_copy(Tg(Mtn), pvh(nMt)), [prb(nMt)], [rr[Mtn]])
                yield
                nE, nD = nbk(), nbk()
                mmg(nE, lambda h, Mn=Mn: T_[Mn][:, h, :], lambda h, Ec=Ec: T_[Ec][:, h, :], [rr[Mn], rr[Ec]])
                mmg(nD, lambda h, Mtn=Mtn: T_[Mtn][:, h, :], lambda h, Dc=Dc: T_[Dc][:, h, :], [rr[Mtn], rr[Dc]])
                ew("dve", lambda e, En=En, Ec=Ec, nE=nE: e.tensor_tensor(Tg(En), pvh(nE), Tg(Ec), ALU.add), [prb(nE), rr[Ec]], [rr[En]])
                ew("dve", lambda e, Dn=Dn, Dc=Dc, nD=nD: e.tensor_tensor(Tg(Dn), pvh(nD), Tg(Dc), ALU.add), [prb(nD), rr[Dc]], [rr[Dn]])
                yield
                Mc, Mtc, Ec, Dc = Mn, Mtn, En, Dn
            for bi in range(4):
                En = "Pb" if Ec != "Pb" else "Pa"
                Dn = "Db" if Dc != "Db" else "Da"
                ew("dve", lambda e, bi=bi: e.tensor_tensor(Tg("Cb"), Tg("Bm"), bcm(mCs[bi]), ALU.mult), [rr["Bm"], R_dt], [rr["Cb"]])
                nG = nbk()
                mmg(nG, lambda h: T_["Cb"][:, h, :], lambda h, Ec=Ec: T_[Ec][:, h, :], [rr["Cb"], rr[Ec]])
                ew("act", lambda e, nG=nG: e.copy(Tg("G"), pvh(nG)), [prb(nG)], [rr["G"]])
                if bi < 3:
                    ew("pool", lambda e, bi=bi: e.tensor_tensor(Tg("Cbt"), Tg("Mta"), bcm(mCTs[bi]), ALU.mult), [rr["Mta"], R_dt], [rr["Cbt"]])
                    nG2 = nbk()
                    mmg(nG2, lambda h: T_["Cbt"][:, h, :], lambda h, Dc=Dc: T_[Dc][:, h, :], [rr["Cbt"], rr[Dc]])
                    ew("act", lambda e, nG2=nG2: e.copy(Tg("G2"), pvh(nG2)), [prb(nG2)], [rr["G2"]])
                yield
                nH = nbk()
                mmg(nH, lambda h, Dc=Dc: T_[Dc][:, h, :], lambda h: T_["G"][:, h, :], [rr[Dc], rr["G"]])
                ew("dve", lambda e, En=En, Ec=Ec, nH=nH: e.tensor_tensor(Tg(En), pvh(nH), Tg(Ec), ALU.add), [prb(nH), rr[Ec]], [rr[En]])
                if bi < 3:
                    nH2 = nbk()
                    mmg(nH2, lambda h, Ec=Ec: T_[Ec][:, h, :], lambda h: T_["G2"][:, h, :], [rr[Ec], rr["G2"]])
                    ew("dve", lambda e, Dn=Dn, Dc=Dc, nH2=nH2: e.tensor_tensor(Tg(Dn), pvh(nH2), Tg(Dc), ALU.add), [prb(nH2), rr[Dc]], [rr[Dn]])
                    Dc = Dn
                Ec = En
                yield
            Pc = Ec
            nW, nU = nbk(), nbk()
            mmg(nW, lambda h: T_["Xw"][:, h, :], lambda h: T_[Pc][:, h, :], [rr["Xw"], rr[Pc]])
            mmg(nU, lambda h: T_[Pc][:, h, :], lambda h: T_["Xu"][:, h, :], [rr["Xu"], rr[Pc]])
            ew("act", lambda e: e.copy(Tg("WT"), pvh(nW)), [prb(nW)], [rr["WT"]])
            ew("act", lambda e: e.copy(Tg("U"), pvh(nU)), [prb(nU)], [rr["U"]])
            yield
            nWS = nbk()
            mmg(nWS, lambda h: T_["WT"][:, h, :], lambda h: Sdb[:, h, :], [rr["WT"], R_Sdb[g]])
            ew("dve", lambda e: e.tensor_tensor(Tg("vnew"), Tg("U"), pvh(nWS), ALU.subtract), [prb(nWS), rr["U"]], [rr["vnew"]])
            yield
            nO, nDS = nbk(), nbk()
            for hl in range(HG):
                h = g * HG + hl
                P.op("pe", lambda e, hl=hl, h=h: e.matmul(pb_(nO)[:, hl * 128:(hl + 1) * 128], T_["attnT"][:, h, :], T_["vnew"][:, h, :], start=True, stop=False),
                     [rr["attnT"], rr["vnew"]], [prb(nO)])
                P.op("pe", lambda e, hl=hl, h=h: e.matmul(pb_(nO)[:, hl * 128:(hl + 1) * 128], T_["qgT"][:, h, :], Sdb[:, h, :], start=False, stop=True),
                     [rr["qgT"], R_Sdb[g]], [prb(nO)])
            mmg(nDS, lambda h: T_["kdec"][:, h, :], lambda h: T_["vnew"][:, h, :], [rr["kdec"], rr["vnew"]])
            ew("dve", lambda e: e.tensor_tensor(Sd[:, hsl, :], Sd[:, hsl, :], bch(ecd[:, t, :]), ALU.mult), [R_dt], [R_Sd[g]])
            ew("dve", lambda e: e.tensor_tensor(Sd[:, hsl, :], Sd[:, hsl, :], pvh(nDS), ALU.add), [prb(nDS)], [R_Sd[g]])
            ew("act", lambda e: e.copy(Sdb[:, hsl, :], Sd[:, hsl, :]), [R_Sd[g]], [R_Sdb[g]])
            yield
            ew("act", lambda e: e.copy(Tg("osb2"), pvh(nO)), [prb(nO)], [rr["osb2"]])
            ew("pool", lambda e: e.tensor_tensor(Tg("sq2"), Tg("osb2"), Tg("osb2"), ALU.mult), [rr["osb2"]], [rr["sq2"]])
            ew("dve", lambda e: e.tensor_reduce(st9[:, 0, hsl], Tg("sq2"), AX.X, ALU.add), [rr["sq2"]], [R_st9[g]])
            ew("act", lambda e: e.activation(st9[:, 1, hsl], st9[:, 0, hsl], AF.Sqrt, bias=epsc[:, 0:1], scale=1.0 / 128), [R_c2], [R_st9[g]])
            ew("dve", lambda e: e.reciprocal(st9[:, 2, hsl], st9[:, 1, hsl]), [], [R_st9[g]])
            ew("dve", lambda e: e.tensor_tensor(Tg("osb2"), Tg("osb2"), bch(st9[:, 2, :]), ALU.mult), [R_st9[g]], [rr["osb2"]])
            ew("pool", lambda e: e.tensor_tensor(Tg("osb2"), Tg("osb2"), bcm(dnw), ALU.mult), [R_dt], [rr["osb2"]])
            zsl = slice(g * HG * 128, (g + 1) * HG * 128)
            ew("act", lambda e: e.activation(Tg("szl").rearrange("p h n -> p (h n)"), zt[b_][:, zsl], AF.Silu), [rin], [rr["szl"]])
            ew("pool", lambda e: e.tensor_tensor(Tg("sq2"), Tg("osb2"), Tg("szl"), ALU.mult), [rr["osb2"], rr["szl"]], [rr["sq2"]])
            P.dma("sp", o_scr[tsl, zsl], Tg("sq2"), reads=[rr["sq2"]], writes=[R_oscr])

        dn_loads(0)
        for t in range(NT):
            if t + 1 < NT:
                dn_loads(t + 1)
            gens = [dn_gen(t, g) for g in range(NG)]
            while gens:
                for gg_ in list(gens):
                    try:
                        next(gg_)
                    except StopIteration:
                        gens.remove(gg_)
        P.barrier()
        A.release(dn_mark)
        if dbg == "dn":
            P.dma("sp", out[0:128, 0:128], dnw, final=True)
            P.run()
            return nc, dbg_out


        b_mark = A.mark()
        R_bc = Res("b_const")
        idxt = A.alloc((8,), I32)
        P.dma("sp", idxt, own_idx[:, :], writes=[R_bc])
        wr = A.alloc((NKC, 64))
        P.dma("sp", wr, w_router.rearrange("(c p) n -> p c n", p=128), writes=[R_bc])
        rb = A.alloc((64,))
        P.dma("sp", rb, router_bias.partition_broadcast(128), writes=[R_bc])
        Wc = A.alloc((8, 65))
        R_Wc = Res("Wc")
        P.op("pool", lambda e: e.memset(Wc, 1.0), [], [R_Wc])
        h2T = A.alloc((NKC, OWN), BF16)
        R_h2T = [Res("h2T%d" % i) for i in range(8)]
        moe_mark = A.mark()
        gm = A.alloc((D,))
        tmpr = A.alloc((D,))
        P.dma("sp", gm, mod_scr[0:1, 2 * D:3 * D].partition_broadcast(128), reads=[R_modscr], writes=[R_bc])
        P.dma("sp", tmpr, g_post1.partition_broadcast(128), writes=[R_bc])
        P.op("dve", lambda e: e.tensor_tensor(gm, gm, tmpr, ALU.mult), [], [R_bc])
        wo = A.alloc((NKC, D), BF16)
        R_wo = Res("wo")
        w_out_v = w_out.rearrange("(c p) n -> p c n", p=128)
        for q4 in range(4):
            P.dma("pool", wo[:, :, q4 * 512:(q4 + 1) * 512], w_out_v[:, :, q4 * 512:(q4 + 1) * 512], writes=[R_wo])
        og = A.alloc((D,))
        ogb = A.alloc((D,), BF16)
        oT = A.alloc((NKC, 128), BF16)
        ysb = A.alloc((D,))
        xot = A.alloc((D,))
        x1t = A.alloc((D,))
        xn2 = A.alloc((D,))
        hTf = A.alloc((NKC, 128))
        rsb = A.alloc((4,))
        rt_ = A.alloc((16, 64))
        R_og, R_ogb, R_oT, R_ysb, R_xot, R_x1t, R_xn2, R_hTf, R_rsb, R_rt = [Res() for _ in range(10)]
        R_x1scr = Res("x1scr")
        sc_, sel_, eq_, sel2_, selm_, ch_ = [rt_[:, i, :] for i in range(6)]
        m1_ = rt_[:, 6, 0:8]
        m2_ = rt_[:, 6, 8:16]
        gs_ = rt_[:, 6, 16:24]
        keep_ = rt_[:, 6, 24:32]
        kt_ = rt_[:, 6, 32:40]
        top8 = rt_[:, 7, 0:8]
        top8b = rt_[:, 7, 8:16]
        den_ = rt_[:, 7, 16:17]
        rden_ = rt_[:, 7, 17:18]

        def rms_rstd2(src, rsrc, col):
            P.op("pool", lambda e: e.memset(rsb[:, col:col + 2], 0.0), [], [R_rsb])
            P.op("act", lambda e: e.activation(xn2, src, AF.Square, accum_out=rsb[:, col:col + 1]), [rsrc], [R_xn2, R_rsb])
            P.op("act", lambda e: e.activation(rsb[:, col + 1:col + 2], rsb[:, col:col + 1], AF.Sqrt, bias=epsc[:, 0:1], scale=1.0 / D), [R_c2], [R_rsb])
            P.op("dve", lambda e: e.reciprocal(rsb[:, col:col + 1], rsb[:, col + 1:col + 2]), [], [R_rsb])

        def phase_b_tile(tt):
            tsl = slice(tt * 128, (tt + 1) * 128)
            P.op("pool", lambda e: e.indirect_dma_start(out=og, out_offset=None, in_=o_scr[:, :],
                                                        in_offset=bass.IndirectOffsetOnAxis(ap=idxt[:, tt:tt + 1], axis=0)),
                 [R_oscr, R_bc], [R_og], dma=True)
            P.dma("sp", xot, xo[tsl, :], writes=[R_xot])
            P.op("act", lambda e: e.copy(ogb, og), [R_og], [R_ogb])
            for half in range(2):
                pv = psum[0][:, half * 512:(half + 1) * 512].bitcast(BF16)
                for q in range(8):
                    kc = half * 8 + q
                    P.op("pe", lambda e, pv=pv, q=q, kc=kc: e.transpose(pv[:, q * 128:(q + 1) * 128], ogb[:, kc * 128:(kc + 1) * 128], ident_b),
                         [R_ogb, R_c2], [PR[0][half]])
                P.op("act" if half == 0 else "dve", lambda e, pv=pv, half=half: (e.copy if half == 0 else e.tensor_copy)(
                    oT[:, half * 8:(half + 1) * 8, :], pv.rearrange("p (c n) -> p c n", c=8)), [PR[0][half]], [R_oT])
            for db in range(4):
                pi, pk = 1 + db // 2, db % 2
                for kc in range(NKC):
                    P.op("pe", lambda e, kc=kc, db=db, pi=pi, pk=pk: e.matmul(pbank(pi, pk), oT[:, kc, :], wo[:, kc, db * 512:(db + 1) * 512],
                                                                            start=(kc == 0), stop=(kc == NKC - 1)), [R_oT, R_wo], [PR[pi][pk]])
                P.op("act", lambda e, db=db, pi=pi, pk=pk: e.copy(ysb[:, db * 512:(db + 1) * 512], pbank(pi, pk)), [PR[pi][pk]], [R_ysb])
            rms_rstd2(ysb, R_ysb, 0)
            P.op("dve", lambda e: e.scalar_tensor_tensor(ysb, ysb, rsb[:, 0:1], gm, ALU.mult, ALU.mult), [R_rsb, R_bc], [R_ysb])
            P.op("dve", lambda e: e.tensor_tensor(x1t, ysb, xot, ALU.add), [R_ysb, R_xot], [R_x1t])
            P.dma("sp", x1_scr[tsl, :], x1t, reads=[R_x1t], writes=[R_x1scr])
            rms_rstd2(x1t, R_x1t, 2)
            P.op("dve", lambda e: e.tensor_scalar(xn2, x1t, rsb[:, 2:3], None, ALU.mult), [R_x1t, R_rsb], [R_xn2])
            for g4 in range(4):
                pi, pk = (g4 % 2), (g4 // 2)
                for q in range(4):
                    kc = g4 * 4 + q
                    P.op("pe", lambda e, pi=pi, pk=pk, q=q, kc=kc: e.transpose(pbank(pi, pk)[:, q * 128:(q + 1) * 128], xn2[:, kc * 128:(kc + 1) * 128], ident_f),
                         [R_xn2, R_const], [PR[pi][pk]])
                sl4 = slice(g4 * 4, (g4 + 1) * 4)
                P.op("dve", lambda e, pi=pi, pk=pk, sl4=sl4: e.tensor_tensor(
                    hTf[:, sl4, :], pbank(pi, pk).rearrange("p (c n) -> p c n", c=4), bc(a2[:, sl4].unsqueeze(2), (128, 4, 128)), ALU.mult),
                    [PR[pi][pk], R_a], [R_hTf])
            P.op("pool", lambda e: e.tensor_tensor(hTf, hTf, bc(s2.unsqueeze(2), (128, NKC, 128)), ALU.add), [R_modfm], [R_hTf])
            P.op("act", lambda e: e.copy(h2T[:, :, tsl], hTf), [R_hTf], [R_h2T[tt]])
            for kc in range(NKC):
                P.op("pe", lambda e, kc=kc: e.matmul(pbank(2, 0)[:, 0:64], hTf[:, kc, :], wr[:, kc, :], start=(kc == 0), stop=(kc == NKC - 1)),
                     [R_hTf, R_bc], [PR[2][0]])
            V = lambda fn, rd=(): P.op("dve", fn, list(rd) + [R_rt, R_bc], [R_rt])
            P.op("act", lambda e: e.activation(sc_, pbank(2, 0)[:, 0:64], AF.Sigmoid), [PR[2][0]], [R_rt])
            V(lambda e: e.tensor_tensor(sel_, sc_, rb, ALU.add))
            g3 = lambda ap: ap.rearrange("p (g k) -> p g k", g=8)
            V(lambda e: e.tensor_reduce(m1_, g3(sel_), AX.X, ALU.max))
            V(lambda e: e.tensor_tensor(g3(eq_), g3(sel_), bc(m1_.unsqueeze(2), (128, 8, 8)), ALU.is_equal))
            V(lambda e: e.scalar_tensor_tensor(sel2_, eq_, -1.0e9, sel_, ALU.mult, ALU.add))
            V(lambda e: e.tensor_reduce(m2_, g3(sel2_), AX.X, ALU.max))
            V(lambda e: e.tensor_tensor(gs_, m1_, m2_, ALU.add))
            V(lambda e: e.max(top8, gs_))
            V(lambda e: e.tensor_scalar(keep_, gs_, top8[:, 3:4], None, ALU.is_ge))
            V(lambda e: e.tensor_scalar(kt_, keep_, 1.0e3, -1.0e3, ALU.mult, ALU.add))
            V(lambda e: e.tensor_tensor(g3(selm_), g3(sel_), bc(keep_.unsqueeze(2), (128, 8, 8)), ALU.mult))
            V(lambda e: e.tensor_tensor(g3(selm_), g3(selm_), bc(kt_.unsqueeze(2), (128, 8, 8)), ALU.add))
            V(lambda e: e.max(top8b, selm_))
            V(lambda e: e.tensor_scalar(ch_, selm_, top8b[:, 7:8], None, ALU.is_ge))
            V(lambda e: e.tensor_tensor(ch_, ch_, sc_, ALU.mult))
            V(lambda e: e.tensor_reduce(den_, ch_, AX.X, ALU.add))
            V(lambda e: e.reciprocal(rden_, den_))
            P.op("dve", lambda e: e.tensor_scalar(Wc[:, tt, 0:64], ch_, rden_, 2.5, ALU.mult, ALU.mult), [R_rt], [R_Wc])

        for tt in range(8):
            phase_b_tile(tt)
        if dbg == "b":
            P.dma("sp", dbg_a[:, 0:520], Wc.rearrange("p t e -> p (t e)"), reads=[R_Wc], final=True)
            P.dma("sp", dbg_a[:, 1024:1536], h2T[:, 0, :].bitcast(F32), reads=R_h2T, final=True)
            P.dma("sp", out[0:128, :], x1t, reads=[R_x1t], final=True)
            P.run()
            return nc, dbg_out
        P.barrier()
        A.release(moe_mark)


        NE = 65 if MOE_EXPERTS is None else MOE_EXPERTS
        acc = A.alloc((8, D))
        R_acc = [Res("acc%d" % i) for i in range(8)]
        P.op("pool", lambda e: e.memset(acc, 0.0), [], R_acc)
        wg = [A.alloc((NKC, 512), BF16) for _ in range(2)]
        wu = [A.alloc((NKC, 512), BF16) for _ in range(2)]
        wd = [A.alloc((4, D), BF16) for _ in range(1)]
        R_wg = [Res("wg0"), Res("wg1")]
        R_wu = [Res("wu0"), Res("wu1")]
        R_wd = [Res("wd0")]
        actT = A.alloc((4, OWN), BF16)
        R_act = [[Res("act%d_%d" % (f, tb)) for tb in range(2)] for f in range(4)]
        sgm = [A.alloc((512,), BF16) for _ in range(2)]
        R_sgm = [Res("sgm0"), Res("sgm1")]
        bcnt = [0]

        def nbank():
            n = bcnt[0] % 8
            bcnt[0] += 1
            return n // 2, n % 2

        def moe_expert(ex):
            b_ = ex % 2
            g_, u_, d_ = wg[b_], wu[b_], wd[0]
            rg_, ru_, rd_ = R_wg[b_], R_wu[b_], R_wd[0]
            P.dma("pool", g_, w_gate[ex].rearrange("(c p) n -> p c n", p=128), writes=[rg_])
            P.dma("pool", u_, w_up[ex].rearrange("(c p) n -> p c n", p=128), writes=[ru_])
            P.dma("pool", d_, w_down[ex].rearrange("(c p) n -> p c n", p=128), writes=[rd_])
            scnt = 0
            for f in range(4):
                for tb in range(2):
                    gi, gk_ = nbank()
                    ui, uk_ = nbank()
                    for kc in range(NKC):
                        P.op("pe", lambda e, kc=kc, f=f, tb=tb, gi=gi, gk_=gk_: e.matmul(
                            pbank(gi, gk_), g_[:, kc, f * 128:(f + 1) * 128], h2T[:, kc, tb * 512:(tb + 1) * 512],
                            start=(kc == 0), stop=(kc == NKC - 1)), [rg_] + R_h2T[tb * 4:(tb + 1) * 4], [PR[gi][gk_]])
                    for kc in range(NKC):
                        P.op("pe", lambda e, kc=kc, f=f, tb=tb, ui=ui, uk_=uk_: e.matmul(
                            pbank(ui, uk_), u_[:, kc, f * 128:(f + 1) * 128], h2T[:, kc, tb * 512:(tb + 1) * 512],
                            start=(kc == 0), stop=(kc == NKC - 1)), [ru_] + R_h2T[tb * 4:(tb + 1) * 4], [PR[ui][uk_]])
                    sg_ = sgm[scnt % 2]
                    rs_ = R_sgm[scnt % 2]
                    scnt += 1
                    P.op("act", lambda e, sg_=sg_, gi=gi, gk_=gk_: e.activation(sg_, pbank(gi, gk_), AF.Silu), [PR[gi][gk_]], [rs_])
                    P.op("dve", lambda e, sg_=sg_, ui=ui, uk_=uk_, f=f, tb=tb: e.tensor_tensor(
                        actT[:, f, tb * 512:(tb + 1) * 512], pbank(ui, uk_), sg_, ALU.mult), [PR[ui][uk_], rs_], [R_act[f][tb]])
            for tt in range(8):
                for db in range(4):
                    di, dk_ = nbank()
                    for f in range(4):
                        P.op("pe", lambda e, f=f, tt=tt, db=db, di=di, dk_=dk_: e.matmul(
                            pbank(di, dk_), actT[:, f, tt * 128:(tt + 1) * 128], d_[:, f, db * 512:(db + 1) * 512],
                            start=(f == 0), stop=(f == 3)), [R_act[f][tt // 4], rd_], [PR[di][dk_]])
                    P.op("dve", lambda e, tt=tt, db=db, di=di, dk_=dk_: e.scalar_tensor_tensor(
                        acc[:, tt, db * 512:(db + 1) * 512], pbank(di, dk_), Wc[:, tt, ex:ex + 1], acc[:, tt, db * 512:(db + 1) * 512],
                        ALU.mult, ALU.add), [PR[di][dk_], R_Wc], [R_acc[tt]])

        for ex in range(NE):
            moe_expert(ex)
        P.barrier()
        A.release(moe_mark)
        acc2 = A.alloc((8, D))
        gf = A.alloc((D,))
        tmpf = A.alloc((D,))
        R_gf = Res("gf")
        P.dma("sp", gf, mod_scr[0:1, 5 * D:6 * D].partition_broadcast(128), reads=[R_modscr], writes=[R_gf])
        P.dma("sp", tmpf, g_post2.partition_broadcast(128), writes=[R_gf])
        P.op("dve", lambda e: e.tensor_tensor(gf, gf, tmpf, ALU.mult), [], [R_gf])
        x1l = [A.alloc((D,)) for _ in range(2)]
        R_x1l = [Res("x1l0"), Res("x1l1")]
        junk = A.alloc((D,))
        R_junk = Res("junk")
        rsf = A.alloc((8, 2))
        R_rsf = Res("rsf")
        P.op("pool", lambda e: e.memset(rsf, 0.0), [], [R_rsf])
        for tt in range(8):
            tsl = slice(tt * 128, (tt + 1) * 128)
            xl = x1l[tt % 2]
            rxl = R_x1l[tt % 2]
            P.dma("sp", xl, x1_scr[tsl, :], reads=[R_x1scr], writes=[rxl])
            at = acc2[:, tt, :]
            P.op("act", lambda e, at=at, tt=tt: e.activation(junk, at, AF.Square, accum_out=rsf[:, tt, 0:1]), [R_acc[tt]], [R_junk, R_rsf])
            P.op("act", lambda e, tt=tt: e.activation(rsf[:, tt, 1:2], rsf[:, tt, 0:1], AF.Sqrt, bias=epsc[:, 0:1], scale=1.0 / D), [R_c2], [R_rsf])
            P.op("dve", lambda e, tt=tt: e.reciprocal(rsf[:, tt, 0:1], rsf[:, tt, 1:2]), [], [R_rsf])
            P.op("dve", lambda e, at=at, tt=tt: e.scalar_tensor_tensor(at, at, rsf[:, tt, 0:1], gf, ALU.mult, ALU.mult), [R_rsf, R_gf], [R_acc[tt]])
            P.op("dve", lambda e, at=at, xl=xl: e.tensor_tensor(xl, at, xl, ALU.add), [R_acc[tt]], [rxl])
            P.dma("sp", out[tsl, :], xl, reads=[rxl], final=True)
        P.run()
    return nc, dbg_out


W_IN_ORDER = None


def _w_in_perm():
    sp = np.cumsum([0, 1024, 1024, 1024, 1024, 8, 8, 512, 512, 1024, 1024])
    seg = lambda i: np.arange(sp[i], sp[i + 1])
    return np.concatenate([seg(0), seg(1), seg(2), seg(3), seg(6), seg(7), seg(8), seg(9), seg(4), seg(5)])


def host_inputs(inputs, core, light=False):
    b, j = core // 4, core % 4
    f = lambda a: np.ascontiguousarray(a, dtype=np.float32)
    fm = lambda v: f(np.asarray(v).reshape(NKC, 128).T)
    m = {}
    x = np.asarray(inputs["x"])
    m["xb"] = f(x[b])
    m["xo"] = f(x[b, j * OWN:(j + 1) * OWN])
    m["cT"] = fm(inputs["c"][b])
    m["pos"] = np.ascontiguousarray(np.asarray(inputs["positions"])[b].reshape(NT, 128).T.astype(np.int32))
    m["w_ada"] = f(inputs["w_ada"][0])
    m["b_ada"] = f(inputs["b_ada"][0]).reshape(1, -1)
    m["g_pre1"] = fm(inputs["pre_norm_mix"][0])
    m["g_pre2"] = fm(inputs["pre_norm_ffn"][0])
    m["g_post1"] = f(inputs["post_norm_mix"][0]).reshape(1, -1)
    m["g_post2"] = f(inputs["post_norm_ffn"][0]).reshape(1, -1)
    m["w_in"] = f(np.asarray(inputs["w_in"][0])[:, _w_in_perm()])
    cw = np.asarray(inputs["conv_w"][0])
    m["conv_w"] = f(cw.T.reshape(24, 128, 4).transpose(1, 0, 2))
    m["a_log"] = f(inputs["a_log"][0]).reshape(1, 8)
    m["dt_bias"] = f(inputs["dt_bias"][0]).reshape(1, 8)
    m["dn_norm_w"] = f(inputs["dn_norm_w"][0]).reshape(1, 128)
    m["rt_norm_w"] = f(inputs["rt_norm_w"][0]).reshape(1, 1024)
    m["w_out"] = f(inputs["w_out"][0])
    m["w_router"] = f(inputs["w_router"][0])
    m["router_bias"] = f(inputs["router_bias"][0]).reshape(1, 64)
    if not light:
      m["w_gate"] = f(np.concatenate([np.asarray(inputs["w_gate_exp"][0]), np.asarray(inputs["w_gate_sh"])], 0))
      m["w_up"] = f(np.concatenate([np.asarray(inputs["w_up_exp"][0]), np.asarray(inputs["w_up_sh"])], 0))
      m["w_down"] = f(np.concatenate([np.asarray(inputs["w_down_exp"][0]), np.asarray(inputs["w_down_sh"])], 0))
    m["own_idx"] = np.ascontiguousarray((j * OWN + np.arange(OWN)).reshape(8, 128).T.astype(np.int32))
    i = np.arange(128)
    m["c_ident"] = f(np.eye(128))
    m["c_utri"] = f(i[:, None] <= i[None, :])
    m["c_maskS"] = f(i[None, :] < i[:, None])
    m["c_maskT"] = f(i[None, :] >= i[:, None])
    oh = np.zeros((128, 8, 128), np.float32)
    for h in range(8):
        oh[h, h, :] = 1.0
    m["c_onehot8"] = oh
    lg = np.log(1.0 - 2.0 ** (-5.0 - np.arange(8, dtype=np.float64)))
    diff = (i[None, :] - i[:, None]).astype(np.float64)
    rtm = np.where(diff[:, None, :] >= 0, np.exp(np.maximum(diff[:, None, :], 0) * lg[None, :, None]), 0.0) * 0.125
    m["c_rtmask"] = f(rtm)
    m["c_gq"] = f(np.broadcast_to(np.exp((i[None, None, :] + 1.0) * lg[None, :, None]), (64, 8, 128)))
    m["c_gk"] = f(np.exp((127.0 - i[:, None]) * lg[None, :]) * 0.125)
    m["c_gC"] = f(np.broadcast_to(np.exp(128.0 * lg)[None, :], (64, 8)))
    mBDh = ((i[:, None] // 8) == (i[None, :] // 8)) & (i[None, :] < i[:, None])
    m["c_mBD"] = f(np.stack([mBDh, mBDh.T]))
    mcs = []
    for bsz in (8, 16, 32, 64):
        same = (i[:, None] // (2 * bsz)) == (i[None, :] // (2 * bsz))
        mcs.append(same & ((i[:, None] % (2 * bsz)) >= bsz) & ((i[None, :] % (2 * bsz)) < bsz))
    m["c_mC"] = f(np.stack(mcs + [x.T for x in mcs]))
    m["c_theta"] = f(1.0 / (10000.0 ** np.linspace(0.0, 1.0, 32, dtype=np.float32))).reshape(1, 32)
    return m


def kernel(**inputs):
    nc, _ = build_program(DBG)
    cores = list(range(8)) if DBG_CORES is None else DBG_CORES
    in_maps = [host_inputs(inputs, c, light=DBG not in (None, "moe")) for c in cores]
    res = run_bass_kernel_spmd(nc, in_maps, core_ids=list(range(len(cores))))
    if DBG is not None:
        return res
    outp = np.zeros((2, S, D), np.float32)
    for k, c in enumerate(cores):
        b, j = c // 4, c % 4
        outp[b, j * OWN:(j + 1) * OWN] = res.results[k]["out"]
    return outp
```

```python
import numpy as np
import concourse.bass as bass
import concourse.mybir as mybir
from concourse.bass_utils import run_bass_kernel_spmd
from contextlib import ExitStack

F32 = mybir.dt.float32
BF16 = mybir.dt.bfloat16
I32 = mybir.dt.int32
ALU = mybir.AluOpType
AF = mybir.ActivationFunctionType
AX = mybir.AxisListType

D = 2048
S = 4096
NT = 32
OWN = 1024
NKC = 16
EPS = 1e-6
DBG = None
DBG_CORES = None
DN_TILES = NT
DN_DUMP = False
DN_STAGE = 6
DN_SUB = 0
MOE_EXPERTS = None
DN_GROUPS = 2


class Res:
    __slots__ = ("name", "w", "r", "excl")

    def __init__(self, name="", excl=False):
        self.name = name
        self.w = None
        self.r = []
        self.excl = excl


class Prog:
    COMPUTE = ("pe", "act", "dve", "pool")
    ENG = ("pe", "act", "dve", "pool", "sp")
    ND = 48

    def __init__(self, nc, es):
        self.nc = nc
        self.es = es
        self.ops = {e: [] for e in self.ENG}
        self.cnt = {e: 0 for e in self.COMPUTE}
        self.sems = {}
        for e in self.COMPUTE:
            self.sems[e] = es.enter_context(nc.semaphore("cs_" + e))
        for i in range(self.ND):
            self.sems[("d", i)] = es.enter_context(nc.semaphore("ds%d" % i))
        self.duse = [0] * self.ND
        self.dnext = 0
        self.waited = {e: {} for e in self.ENG}
        self.final = []
        self.nops = 0

    def _wait(self, eng, tok):
        key, val = tok
        if key == eng and eng == "pe":
            return
        if self.waited[eng].get(key, 0) >= val:
            return
        self.waited[eng][key] = val
        self.ops[eng].append(("w", key, val))

    def op(self, eng, fn, reads=(), writes=(), dma=False, final=False):
        for r in reads:
            if r.w is not None:
                self._wait(eng, r.w)
            if r.excl:
                for t in r.r:
                    if t[0] != eng:
                        self._wait(eng, t)
        for w in writes:
            if w.w is not None:
                self._wait(eng, w.w)
            for t in w.r:
                self._wait(eng, t)
        if dma:
            s = self.dnext % self.ND
            self.dnext += 1
            u = self.duse[s]
            if u > 0:
                self._wait(eng, (("d", s), 16 * u))
            self.duse[s] = u + 1
            tok = (("d", s), 16 * (u + 1))
            self.ops[eng].append(("i", fn, ("d", s), 16))
        else:
            self.cnt[eng] += 1
            tok = (eng, self.cnt[eng])
            self.ops[eng].append(("i", fn, eng, 1))
        for r in reads:
            r.r.append(tok)
            if len(r.r) > 24:
                r.r = r.r[-24:] if False else r.r
        for w in writes:
            w.w = tok
            w.r = []
        if final:
            self.final.append(tok)
        self.nops += 1
        return tok

    def dma(self, eng, out, in_, reads=(), writes=(), final=False, **kw):
        return self.op(eng, lambda e: e.dma_start(out=out, in_=in_, **kw), reads, writes, dma=True, final=final)

    def barrier(self):
        toks = [(e, self.cnt[e]) for e in self.COMPUTE if self.cnt[e] > 0]
        toks += [(("d", s), 16 * self.duse[s]) for s in range(self.ND) if self.duse[s] > 0]
        for e in self.ENG:
            for t in toks:
                self._wait(e, t)

    def run(self):
        for t in self.final:
            self._wait("sp", t)
        nc = self.nc
        sems = self.sems

        def replay(eng_name):
            def body(e):
                for o in self.ops[eng_name]:
                    if o[0] == "w":
                        e.wait_ge(sems[o[1]], o[2])
                    else:
                        o[1](e).then_inc(sems[o[2]], o[3])
            return body

        with nc.Block() as block:
            block.tensor(replay("pe"))
            block.scalar(replay("act"))
            block.vector(replay("dve"))
            block.gpsimd(replay("pool"))
            block.sync(replay("sp"))


class Arena:
    def __init__(self, nc, es, words):
        self.t = es.enter_context(nc.sbuf_tensor("arena", [128, words], F32))
        self.words = words
        self.off = 0

    def alloc(self, free, dtype=F32, parts=128):
        n = 1
        for f in free:
            n *= f
        w = n if dtype != BF16 else (n + 1) // 2
        w = (w + 7) // 8 * 8
        assert self.off + w <= self.words, ("arena overflow", self.off, w, self.words)
        ap = self.t[0:parts, self.off:self.off + w]
        self.off += w
        if dtype == BF16:
            ap = ap.bitcast(BF16)
        elif dtype == I32:
            ap = ap.bitcast(I32)
        ap = ap[:, 0:n]
        if len(free) == 2:
            ap = ap.rearrange("p (a b) -> p a b", a=free[0], b=free[1])
        elif len(free) == 3:
            ap = ap.rearrange("p (a b c) -> p a b c", a=free[0], b=free[1], c=free[2])
        return ap

    def mark(self):
        return self.off

    def release(self, m):
        self.off = m


def bc(ap, shape):
    return ap.to_broadcast(list(shape))


def build_program(dbg=None):
    nc = bass.Bass("TRN2", target_bir_lowering=False)
    dbg_out = {}

    def din(name, shape, dt=F32):
        return nc.dram_tensor(name, list(shape), dt, kind="ExternalInput").ap()

    def scratch(name, shape, dt=F32, dump=False):
        kind = "ExternalOutput" if (dbg is not None and dump) else "Internal"
        t = nc.dram_tensor(name, list(shape), dt, kind=kind).ap()
        if kind == "ExternalOutput":
            dbg_out[name] = t
        return t

    xb = din("xb", [S, D])
    xo = din("xo", [OWN, D])
    cT = din("cT", [128, NKC])
    pos = din("pos", [128, NT], I32)
    w_ada = din("w_ada", [D, 6 * D])
    b_ada = din("b_ada", [1, 6 * D])
    g_pre1 = din("g_pre1", [128, NKC])
    g_pre2 = din("g_pre2", [128, NKC])
    g_post1 = din("g_post1", [1, D])
    g_post2 = din("g_post2", [1, D])
    w_in = din("w_in", [D, 7184])
    conv_w = din("conv_w", [128, 24, 4])
    a_log = din("a_log", [1, 8])
    dt_bias = din("dt_bias", [1, 8])
    dn_norm_w = din("dn_norm_w", [1, 128])
    rt_norm_w = din("rt_norm_w", [1, 1024])
    w_out = din("w_out", [D, D])
    w_router = din("w_router", [D, 64])
    router_bias = din("router_bias", [1, 64])
    if dbg in (None, "moe"):
        w_gate = din("w_gate", [65, D, 512])
        w_up = din("w_up", [65, D, 512])
        w_down = din("w_down", [65, 512, D])
    own_idx = din("own_idx", [128, 8], I32)
    c_ident = din("c_ident", [128, 128])
    c_utri = din("c_utri", [128, 128])
    c_maskS = din("c_maskS", [128, 128])
    c_maskT = din("c_maskT", [128, 128])
    c_onehot8 = din("c_onehot8", [128, 8, 128])
    c_rtmask = din("c_rtmask", [128, 8, 128])
    c_gq = din("c_gq", [64, 8, 128])
    c_gk = din("c_gk", [128, 8])
    c_gC = din("c_gC", [64, 8])
    c_theta = din("c_theta", [1, 32])
    c_mBD = din("c_mBD", [2, 128, 128])
    c_mC = din("c_mC", [8, 128, 128])

    out = nc.dram_tensor("out", [OWN, D], F32, kind="ExternalOutput").ap()

    mod_scr = scratch("mod_scr", [1, 6 * D], dump=True)
    pFM = scratch("pFM", [24, 128, S], dump=(dbg == "a1"))
    pTM = scratch("pTM", [S, 4112], dump=(dbg == "a1"))
    dn_qT = scratch("dn_qT", [8, 128, S], BF16)
    dn_kT = scratch("dn_kT", [8, 128, S], BF16)
    dn_ktm = scratch("dn_ktm", [S, 8, 128], BF16)
    dn_vtm = scratch("dn_vtm", [S, 8, 128], BF16)
    o_scr = scratch("o_scr", [S, D], F32, dump=(dbg in ("rt", "dn")))
    x1_scr = scratch("x1_scr", [OWN, D], dump=(dbg == "b"))
    dbg_a = scratch("dbg_a", [128, 2048], dump=True)

    with ExitStack() as es:
        P = Prog(nc, es)
        A = Arena(nc, es, 53000)
        psum = [es.enter_context(nc.psum_tensor("ps%d" % i, [128, 1024], F32)) for i in range(4)]
        PR = [[Res("ps%d_%d" % (i, k), excl=True) for k in range(2)] for i in range(4)]

        def pbank(i, k):
            return psum[i][:, k * 512:(k + 1) * 512]

        ident_f = A.alloc((128,))
        ident_b = A.alloc((128,), BF16)
        utri = A.alloc((128,))
        ones_f = A.alloc((128,))
        ones_b = A.alloc((128,), BF16)
        R_const = Res("const")
        P.dma("sp", ident_f, c_ident[:, :], writes=[R_const])
        P.dma("sp", utri, c_utri[:, :], writes=[R_const])
        R_c2 = Res("c2")
        P.op("act", lambda e: e.copy(ident_b, ident_f), [R_const], [R_c2])
        P.op("pool", lambda e: e.memset(ones_f, 1.0), [], [R_c2])
        P.op("pool", lambda e: e.memset(ones_b, 1.0), [], [R_c2])
        epsc = A.alloc((8,))
        P.op("pool", lambda e: e.memset(epsc, EPS), [], [R_c2])

        base_mark = A.mark()

        cTt = A.alloc((NKC,))
        cTb = A.alloc((NKC,), BF16)
        R_c = Res("c")
        P.dma("sp", cTt, cT[:, :], writes=[R_c])
        P.op("act", lambda e: e.activation(cTb, cTt, AF.Silu), [R_c], [R_c])
        wab = [A.alloc((NKC, 512), BF16) for _ in range(2)]
        R_wab = [Res("wab0"), Res("wab1")]
        modrow = A.alloc((6 * D,), parts=1)
        badar = A.alloc((6 * D,), parts=1)
        R_mod = Res("modrow")
        R_bada = Res("bada")
        P.dma("sp", badar, b_ada[:, :], writes=[R_bada])
        w_ada_v = w_ada.rearrange("(c p) n -> p c n", p=128)
        for jb in range(24):
            wb = wab[jb % 2]
            rw = R_wab[jb % 2]
            P.dma("pool", wb, w_ada_v[:, :, jb * 512:(jb + 1) * 512], writes=[rw])
            pi, pk = (jb % 4) // 2, jb % 2
            for kc in range(NKC):
                P.op("pe", lambda e, kc=kc, wb=wb, pi=pi, pk=pk: e.matmul(
                    pbank(pi, pk)[0:1, :], cTb[:, kc:kc + 1], wb[:, kc, :], start=(kc == 0), stop=(kc == NKC - 1)),
                    [R_c, rw], [PR[pi][pk]])
            P.op("dve", lambda e, jb=jb, pi=pi, pk=pk: e.tensor_tensor(
                modrow[:, jb * 512:(jb + 1) * 512], pbank(pi, pk)[0:1, :], badar[:, jb * 512:(jb + 1) * 512], ALU.add),
                [PR[pi][pk], R_bada], [R_mod])
        R_modscr = Res("modscr")
        P.dma("sp", mod_scr[:, :], modrow, reads=[R_mod], writes=[R_modscr])
        P.barrier()
        A.release(base_mark)

        modfm = A.alloc((96,))
        R_modfm = Res("modfm")
        P.dma("sp", modfm, mod_scr.rearrange("o (c p) -> p (o c)", p=128), reads=[R_modscr], writes=[R_modfm],
              allow_slow_non_contiguous=True)
        gp1 = A.alloc((NKC,))
        gp2 = A.alloc((NKC,))
        P.dma("sp", gp1, g_pre1[:, :], writes=[R_modfm])
        P.dma("sp", gp2, g_pre2[:, :], writes=[R_modfm])
        a1 = A.alloc((NKC,))
        a2 = A.alloc((NKC,))
        R_a = Res("a12")
        P.op("dve", lambda e: e.scalar_tensor_tensor(a1, modfm[:, 16:32], 1.0, gp1, ALU.add, ALU.mult), [R_modfm], [R_a])
        P.op("dve", lambda e: e.scalar_tensor_tensor(a2, modfm[:, 64:80], 1.0, gp2, ALU.add, ALU.mult), [R_modfm], [R_a])
        s1 = modfm[:, 0:16]
        s2 = modfm[:, 48:64]
        const_mark = A.mark()

        hT = A.alloc((NKC, S), BF16)
        R_hT = [Res("hT%d" % t) for t in range(NT)]
        hT_mark = A.mark()
        xt = [A.alloc((D,)) for _ in range(2)]
        R_xt = [Res("xt0"), Res("xt1")]
        sqj = A.alloc((D,), BF16)
        R_sqj = Res("sqj")
        xn = [A.alloc((D,), BF16) for _ in range(2)]
        R_xn = [Res("xn0"), Res("xn1")]
        ss = A.alloc((2, 2))
        R_ss = [Res("ss0"), Res("ss1")]
        tmpT = A.alloc((D,))
        R_tmpT = Res("tmpT")

        def rms_rstd(eng_sq, src, rss, ssap, reads):
            P.op("act", lambda e: e.activation(sqj, src, AF.Square, accum_out=ssap[:, 0:1]), reads, [R_sqj, rss])
            P.op("act", lambda e: e.activation(ssap[:, 1:2], ssap[:, 0:1], AF.Sqrt, bias=epsc[:, 0:1], scale=1.0 / D), [rss, R_c2], [rss])
            P.op("dve", lambda e: e.reciprocal(ssap[:, 0:1], ssap[:, 1:2]), [rss], [rss])

        for t in range(NT):
            x_ = xt[t % 2]
            rx = R_xt[t % 2]
            ssap = ss[:, t % 2, :]
            P.dma("sp", x_, xb[t * 128:(t + 1) * 128, :], writes=[rx])
            P.op("pool", lambda e, ssap=ssap: e.memset(ssap, 0.0), [], [R_ss[t % 2]])
            rms_rstd("act", x_, R_ss[t % 2], ssap, [rx])
            xn_ = xn[t % 2]
            P.op("dve", lambda e, x_=x_, xn_=xn_, ssap=ssap: e.tensor_scalar(xn_, x_, ssap[:, 0:1], None, ALU.mult),
                 [rx, R_ss[t % 2]], [R_xn[t % 2]])
            for half in range(2):
                pv = psum[half][:, :].bitcast(BF16)
                for q in range(8):
                    kc = half * 8 + q
                    P.op("pe", lambda e, pv=pv, q=q, kc=kc, xn_=xn_: e.transpose(
                        pv[:, q * 128:(q + 1) * 128], xn_[:, kc * 128:(kc + 1) * 128], ident_b),
                        [R_xn[t % 2], R_c2], [PR[half][0]])
                pvv = pv[:, 0:1024].rearrange("p (c n) -> p c n", c=8)
                tv = tmpT[:, half * 1024:(half + 1) * 1024].rearrange("p (c n) -> p c n", c=8)
                P.op("dve", lambda e, pvv=pvv, tv=tv, half=half: e.tensor_tensor(
                    tv, pvv, bc(a1[:, half * 8:(half + 1) * 8].unsqueeze(2), (128, 8, 128)), ALU.mult),
                    [PR[half][0], R_a], [R_tmpT])
                P.op("pool", lambda e, tv=tv, half=half, t=t: e.tensor_tensor(
                    hT[:, half * 8:(half + 1) * 8, t * 128:(t + 1) * 128], tv,
                    bc(s1[:, half * 8:(half + 1) * 8].unsqueeze(2), (128, 8, 128)), ALU.add),
                    [R_tmpT, R_modfm], [R_hT[t]])

        P.barrier()
        A.release(hT_mark)
        wfm = [A.alloc((NKC, 128), BF16) for _ in range(2)]
        R_wfm = [Res("wfm0"), Res("wfm1")]
        stg = [A.alloc((1024,)) for _ in range(3)]
        R_stg = [Res("stg%d" % i) for i in range(3)]
        gcnt = 0
        w_in_v = w_in.rearrange("(c p) n -> p c n", p=128)
        R_pFM = [Res("pFM%d" % c) for c in range(24)]
        pcnt = 0
        for cc in range(24):
            wt = wfm[cc % 2]
            rw = R_wfm[cc % 2]
            P.dma("pool", wt, w_in_v[:, :, cc * 128:(cc + 1) * 128], writes=[rw])
            for tb in range(8):
                if tb % 2 == 0:
                    st = stg[gcnt % 3]
                    rs = R_stg[gcnt % 3]
                    gcnt += 1
                pi, pk = (pcnt % 4) // 2 + 2, pcnt % 2
                pcnt += 1
                for kc in range(NKC):
                    P.op("pe", lambda e, kc=kc, wt=wt, tb=tb, pi=pi, pk=pk: e.matmul(
                        pbank(pi, pk), wt[:, kc, :], hT[:, kc, tb * 512:(tb + 1) * 512], start=(kc == 0), stop=(kc == NKC - 1)),
                        [rw] + R_hT[tb * 4:(tb + 1) * 4], [PR[pi][pk]])
                P.op("act", lambda e, st=st, tb=tb, pi=pi, pk=pk: e.copy(st[:, (tb % 2) * 512:(tb % 2 + 1) * 512], pbank(pi, pk)),
                     [PR[pi][pk]], [rs])
                if tb % 2 == 1:
                    P.dma("sp", pFM[cc][:, (tb - 1) * 512:(tb + 1) * 512], st, reads=[rs], writes=[R_pFM[cc]])
        P.barrier()
        A.release(hT_mark)
        wtm = [A.alloc((NKC, 512), BF16) for _ in range(2)]
        R_wtm = [Res("wtm0"), Res("wtm1")]
        R_pTM = Res("pTM")
        stq = [A.alloc((512,)) for _ in range(4)]
        R_stq = [Res("stq%d" % i) for i in range(4)]
        scnt = 0
        for cb in range(9):
            ncol = 512 if cb < 8 else 16
            c0 = 3072 + cb * 512
            wt = wtm[cb % 2]
            rw = R_wtm[cb % 2]
            P.dma("pool", wt[:, :, 0:ncol], w_in_v[:, :, c0:c0 + ncol], writes=[rw])
            for t in range(NT):
                pi, pk = (pcnt % 4) // 2 + 2, pcnt % 2
                pcnt += 1
                for kc in range(NKC):
                    P.op("pe", lambda e, kc=kc, wt=wt, t=t, pi=pi, pk=pk, ncol=ncol: e.matmul(
                        pbank(pi, pk)[:, 0:ncol], hT[:, kc, t * 128:(t + 1) * 128], wt[:, kc, 0:ncol],
                        start=(kc == 0), stop=(kc == NKC - 1)),
                        [rw, R_hT[t]], [PR[pi][pk]])
                sq_ = stq[scnt % 4]
                rq = R_stq[scnt % 4]
                scnt += 1
                P.op("act" if t % 2 == 0 else "dve", lambda e, sq_=sq_, pi=pi, pk=pk, ncol=ncol, t=t: (
                    e.copy(sq_[:, 0:ncol], pbank(pi, pk)[:, 0:ncol]) if t % 2 == 0 else
                    e.tensor_copy(sq_[:, 0:ncol], pbank(pi, pk)[:, 0:ncol])),
                    [PR[pi][pk]], [rq])
                P.dma("sp", pTM[t * 128:(t + 1) * 128, cb * 512:cb * 512 + ncol], sq_[:, 0:ncol], reads=[rq], writes=[R_pTM])
        P.barrier()
        A.release(const_mark)
        if dbg == "a1":
            P.dma("sp", out[0:128, 0:1024], hT[:, 0, 0:2048].bitcast(F32), final=True)
            P.run()
            return nc, dbg_out


        R_oscr = Res("oscr")
        dn_mark = A.mark()
        R_dt = Res("dn_tab")
        ab = A.alloc((NT, 16))
        for q4 in range(4):
            P.dma("sp", ab[:, q4 * 8:(q4 + 1) * 8, :], pTM[q4 * 1024:(q4 + 1) * 1024, 4096:4112].rearrange("(t p) c -> p t c", p=128),
                  reads=[R_pTM], writes=[R_dt])
        dtb = A.alloc((8,))
        alg = A.alloc((8,))
        P.dma("sp", dtb, dt_bias.partition_broadcast(128), writes=[R_dt])
        P.dma("sp", alg, a_log.partition_broadcast(128), writes=[R_dt])
        maskS = A.alloc((128,))
        maskT = A.alloc((128,))
        oh8 = A.alloc((8, 128))
        dnw = A.alloc((128,))
        mBD = A.alloc((128,))
        mBDT = A.alloc((128,))
        mCs = [A.alloc((128,)) for _ in range(4)]
        mCTs = [A.alloc((128,)) for _ in range(4)]
        P.dma("sp", mBD, c_mBD[0], writes=[R_dt])
        P.dma("sp", mBDT, c_mBD[1], writes=[R_dt])
        for bi in range(4):
            P.dma("sp", mCs[bi], c_mC[bi], writes=[R_dt])
            P.dma("sp", mCTs[bi], c_mC[4 + bi], writes=[R_dt])
        P.dma("sp", maskS, c_maskS[:, :], writes=[R_dt])
        P.dma("sp", maskT, c_maskT[:, :], writes=[R_dt])
        P.dma("sp", oh8, c_onehot8[:, :, :], writes=[R_dt])
        P.dma("sp", dnw, dn_norm_w.partition_broadcast(128), writes=[R_dt])
        xg = A.alloc((NT, 8))
        t1_ = A.alloc((NT, 8))
        t2_ = A.alloc((NT, 8))
        gg = A.alloc((NT, 8))
        beta = A.alloc((NT, 8))
        negb = A.alloc((NT, 8))
        gc_ = A.alloc((NT, 8))
        glb = A.alloc((NT, 8))
        egc = A.alloc((NT, 8))
        kds = A.alloc((NT, 8))
        ecd = A.alloc((NT, 8))
        bw = A.alloc((NT, 8))
        nA = A.alloc((8,))
        gcTt = [A.alloc((128,)) for _ in range(2)]
        R_gct = [Res("gct0"), Res("gct1")]
        for i_ in range(2):
            P.op("pool", lambda e, i_=i_: e.memset(gcTt[i_], 0.0), [], [R_gct[i_]])
        av = ab[:, :, 0:8]
        bv = ab[:, :, 8:16]
        D_ = lambda fn, rd=(), wr=(): P.op("dve", fn, list(rd) + [R_dt], list(wr) + [R_dt])
        A_ = lambda fn, rd=(), wr=(): P.op("act", fn, list(rd) + [R_dt], list(wr) + [R_dt])
        D_(lambda e: e.tensor_tensor(xg, av, bc(dtb.unsqueeze(1), (128, NT, 8)), ALU.add))
        A_(lambda e: e.activation(t1_, xg, AF.Abs))
        A_(lambda e: e.activation(t2_, t1_, AF.Exp, scale=-1.0))
        A_(lambda e: e.activation(t1_, t2_, AF.Ln, bias=ones_f[:, 0:1], scale=1.0))
        D_(lambda e: e.scalar_tensor_tensor(t2_, xg, 0.0, t1_, ALU.max, ALU.add))
        A_(lambda e: e.activation(nA, alg, AF.Exp))
        D_(lambda e: e.tensor_scalar(nA, nA, -1.0, None, ALU.mult))
        D_(lambda e: e.tensor_tensor(gg, t2_, bc(nA.unsqueeze(1), (128, NT, 8)), ALU.mult))
        A_(lambda e: e.activation(beta, bv, AF.Sigmoid))
        D_(lambda e: e.tensor_scalar(negb, beta, -1.0, None, ALU.mult))
        gflat = gg.rearrange("p t h -> p (t h)")
        P.op("pe", lambda e: e.matmul(psum[0][:, 0:256], utri, gflat, start=True, stop=True), [R_dt, R_const], [PR[0][0]])
        P.op("pe", lambda e: e.matmul(psum[0][:, 512:768], ones_f, gflat, start=True, stop=True), [R_dt, R_c2], [PR[0][1]])
        A_(lambda e: e.copy(gc_.rearrange("p t h -> p (t h)"), psum[0][:, 0:256]), [PR[0][0]])
        A_(lambda e: e.copy(glb.rearrange("p t h -> p (t h)"), psum[0][:, 512:768]), [PR[0][1]])
        A_(lambda e: e.activation(egc, gc_, AF.Exp))
        A_(lambda e: e.activation(ecd, glb, AF.Exp))
        D_(lambda e: e.tensor_tensor(t1_, glb, gc_, ALU.subtract))
        A_(lambda e: e.activation(kds, t1_, AF.Exp))
        D_(lambda e: e.tensor_tensor(bw, beta, egc, ALU.mult))
        p1_mark = A.mark()
        cwt = A.alloc((24, 4))
        P.dma("sp", cwt, conv_w[:, :, :], writes=[R_dt])
        raw = A.alloc((3, S + 8), BF16)
        rsq = A.alloc((2, S))
        sqb16 = A.alloc((S,), BF16)
        dg = A.alloc((2, 4, 128), BF16)
        cv = A.alloc((3, S))
        qkn = A.alloc((2, S), BF16)
        vb16 = A.alloc((S,), BF16)
        tmst = A.alloc((2, NT, 128), BF16)
        R_raw, R_cv, R_qkn, R_vb16, R_tmst, R_rsq, R_sqb = [Res() for _ in range(7)]
        R_dg = [Res("dg0"), Res("dg1")]
        R_dnq, R_dnk, R_ktm, R_vtm = Res(), Res(), Res(), Res()
        P.op("pool", lambda e: e.memset(raw[:, :, 0:8], 0.0), [], [R_raw])
        dgc = 0
        for h in range(8):
            for i in range(3):
                for hf in range(2):
                    P.dma("pool", raw[:, i, 8 + hf * 2048:8 + (hf + 1) * 2048], pFM[i * 8 + h][:, hf * 2048:(hf + 1) * 2048],
                          reads=[R_pFM[i * 8 + h]], writes=[R_raw])
            for i in range(3):
                cc = i * 8 + h
                d_ = dg[:, dgc % 2, :, :]
                rd_ = R_dg[dgc % 2]
                dgc += 1
                for jj in range(4):
                    P.op("dve", lambda e, d_=d_, cc=cc, jj=jj: e.tensor_scalar(d_[:, jj, :], ident_b, cwt[:, cc, jj:jj + 1], None, ALU.mult),
                         [R_c2, R_dt], [rd_])
                for blk in range(8):
                    pi, pk = (blk % 8) // 2, blk % 2
                    for jj in range(4):
                        P.op("pe", lambda e, d_=d_, i=i, jj=jj, blk=blk, pi=pi, pk=pk: e.matmul(
                            pbank(pi, pk), d_[:, jj, :], raw[:, i, 5 + jj + blk * 512:5 + jj + (blk + 1) * 512], start=(jj == 0), stop=(jj == 3)),
                            [rd_, R_raw], [PR[pi][pk]])
                    P.op("act", lambda e, i=i, blk=blk, pi=pi, pk=pk: e.activation(cv[:, i, blk * 512:(blk + 1) * 512], pbank(pi, pk), AF.Silu),
                         [PR[pi][pk]], [R_cv])
            for i in range(2):
                rsv = rsq[:, i, :]
                P.op("act", lambda e, i=i: e.activation(sqb16, cv[:, i, :], AF.Square), [R_cv], [R_sqb])
                for blk in range(8):
                    pi, pk = blk // 2, blk % 2
                    P.op("pe", lambda e, pi=pi, pk=pk, blk=blk: e.matmul(pbank(pi, pk), ones_b, sqb16[:, blk * 512:(blk + 1) * 512], start=True, stop=True),
                         [R_sqb, R_c2], [PR[pi][pk]])
                for blk in range(8):
                    pi, pk = blk // 2, blk % 2
                    P.op("act", lambda e, pi=pi, pk=pk, blk=blk, rsv=rsv: e.activation(rsv[:, blk * 512:(blk + 1) * 512], pbank(pi, pk), AF.Sqrt,
                                                                                        bias=epsc[:, 0:1], scale=1.0), [PR[pi][pk], R_c2], [R_rsq])
                P.op("dve", lambda e, rsv=rsv: e.reciprocal(rsv, rsv), [], [R_rsq])
                scl = (128.0 ** -0.5) if i == 0 else 1.0
                P.op("dve", lambda e, i=i, scl=scl, rsv=rsv: e.scalar_tensor_tensor(qkn[:, i, :], cv[:, i, :], scl, rsv, ALU.mult, ALU.mult),
                     [R_cv, R_rsq], [R_qkn])
            P.op("pool", lambda e: e.tensor_copy(vb16, cv[:, 2, :]), [R_cv], [R_vb16])
            P.dma("sp", dn_qT[h], qkn[:, 0, :], reads=[R_qkn], writes=[R_dnq])
            P.dma("sp", dn_kT[h], qkn[:, 1, :], reads=[R_qkn], writes=[R_dnk])
            for which, src, rsrc in ((0, qkn[:, 1, :], R_qkn), (1, vb16, R_vb16)):
                for g4 in range(4):
                    pi, pk = g4 % 2, g4 // 2
                    pv = psum[pi][:, pk * 512:(pk + 1) * 512].bitcast(BF16)
                    for q in range(8):
                        t = g4 * 8 + q
                        P.op("pe", lambda e, pv=pv, q=q, t=t, src=src: e.transpose(pv[:, q * 128:(q + 1) * 128], src[:, t * 128:(t + 1) * 128], ident_b),
                             [rsrc, R_c2], [PR[pi][pk]])
                    P.op("act" if g4 % 2 == 0 else "dve", lambda e, pv=pv, g4=g4, which=which: (
                        e.copy if g4 % 2 == 0 else e.tensor_copy)(tmst[:, which, g4 * 8:(g4 + 1) * 8, :], pv.rearrange("p (t d) -> p t d", t=8)),
                        [PR[pi][pk]], [R_tmst])
            for q4 in range(4):
                P.dma("sp", dn_ktm[q4 * 1024:(q4 + 1) * 1024].rearrange("(t p) h d -> p t h d", p=128)[:, :, h, :],
                      tmst[:, 0, q4 * 8:(q4 + 1) * 8, :], reads=[R_tmst], writes=[R_ktm])
                P.dma("sp", dn_vtm[q4 * 1024:(q4 + 1) * 1024].rearrange("(t p) h d -> p t h d", p=128)[:, :, h, :],
                      tmst[:, 1, q4 * 8:(q4 + 1) * 8, :], reads=[R_tmst], writes=[R_vtm])
        P.barrier()
        A.release(p1_mark)

        theta_bc = A.alloc((32,))
        posi = A.alloc((NT,), I32)
        posf = A.alloc((NT,))
        sinT = A.alloc((NT, 32))
        cosT = A.alloc((NT, 32))
        rt_tmp_mark = A.mark()
        ang = A.alloc((NT, 32))
        tmpa = A.alloc((NT, 32))
        R_tab = Res("rt_tab")
        P.dma("sp", theta_bc, c_theta.partition_broadcast(128), writes=[R_tab])
        P.dma("sp", posi, pos[:, :], writes=[R_tab])
        P.op("dve", lambda e: e.tensor_copy(posf, posi), [R_tab], [R_tab])
        P.op("dve", lambda e: e.tensor_tensor(ang, bc(posf.unsqueeze(2), (128, NT, 32)),
                                              bc(theta_bc.unsqueeze(1), (128, NT, 32)), ALU.mult), [R_tab], [R_tab])
        TWO_PI = 2.0 * np.pi
        pic = A.alloc((8,))
        P.op("pool", lambda e: e.memset(pic, -np.pi), [], [R_tab])
        ki = A.alloc((NT, 32), I32)
        kf = A.alloc((NT, 32))

        def sin_table(dst, shift):
            P.op("dve", lambda e: e.tensor_scalar(tmpa, ang, shift, 1.0 / TWO_PI, ALU.add, ALU.mult), [R_tab], [R_tab])
            P.op("dve", lambda e: e.tensor_copy(ki, tmpa), [R_tab], [R_tab])
            P.op("dve", lambda e: e.tensor_copy(kf, ki), [R_tab], [R_tab])
            P.op("dve", lambda e: e.tensor_scalar(tmpa, ang, shift, None, ALU.add), [R_tab], [R_tab])
            P.op("dve", lambda e: e.scalar_tensor_tensor(tmpa, kf, -TWO_PI, tmpa, ALU.mult, ALU.add), [R_tab], [R_tab])
            P.op("dve", lambda e: e.tensor_scalar(kf, tmpa, np.pi, TWO_PI, ALU.is_gt, ALU.mult), [R_tab], [R_tab])
            P.op("dve", lambda e: e.tensor_tensor(tmpa, tmpa, kf, ALU.subtract), [R_tab], [R_tab])
            P.op("dve", lambda e: e.tensor_scalar(kf, tmpa, -np.pi, TWO_PI, ALU.is_lt, ALU.mult), [R_tab], [R_tab])
            P.op("dve", lambda e: e.tensor_tensor(tmpa, tmpa, kf, ALU.add), [R_tab], [R_tab])
            P.op("act", lambda e: e.activation(dst, tmpa, AF.Sin), [R_tab], [R_tab])

        sin_table(sinT, 0.0)
        sin_table(cosT, 0.5 * np.pi)
        P.barrier()
        A.release(rt_tmp_mark)
        rtmask = A.alloc((8, 128))
        gq = A.alloc((8, 128), parts=64)
        gk = A.alloc((8,))
        gC = A.alloc((8,), parts=64)
        rtw = A.alloc((1024,))
        P.dma("sp", rtmask, c_rtmask[:, :, :], writes=[R_tab])
        P.dma("sp", gq, c_gq[:, :, :], writes=[R_tab])
        P.dma("sp", gk, c_gk[:, :], writes=[R_tab])
        P.dma("sp", gC, c_gC[:, :], writes=[R_tab])
        P.dma("sp", rtw, rt_norm_w.partition_broadcast(128), writes=[R_tab])
        Sst = A.alloc((8, 128), parts=64)
        Sbf = A.alloc((8, 128), BF16, parts=64)
        R_S = Res("S")
        R_Sbf = Res("Sbf")
        P.op("pool", lambda e: e.memset(Sst, 0.0), [], [R_S])
        P.op("pool", lambda e: e.memset(Sbf, 0.0), [], [R_Sbf])
        ld = [A.alloc((3072,), BF16) for _ in range(2)]
        R_ld = [Res("ld0"), Res("ld1")]
        qr = A.alloc((8, 64), BF16)
        kr = A.alloc((8, 64), BF16)
        kd = A.alloc((8, 64), BF16)
        sgt = A.alloc((1024,))
        ta = A.alloc((8, 32))
        tb_ = A.alloc((8, 32))
        qT = A.alloc((8, 128), BF16, parts=64)
        qdT = A.alloc((8, 128), BF16, parts=64)
        kT = A.alloc((8, 128), BF16, parts=64)
        PT = A.alloc((8, 128), BF16)
        osb = A.alloc((8, 128))
        sqb = A.alloc((8, 128))
        st8 = A.alloc((6, 8))
        R_qr, R_kr, R_kd, R_vb, R_sg, R_ta, R_tb, R_qT, R_qdT, R_kT, R_PT, R_osb, R_sqb, R_st8 = [Res() for _ in range(14)]

        def rotary(src, dst, rdst, t, rl):
            x1 = src[:, :, 0:32]
            x2 = src[:, :, 32:64]
            cs = bc(cosT[:, t, :].unsqueeze(1), (128, 8, 32))
            sn = bc(sinT[:, t, :].unsqueeze(1), (128, 8, 32))
            P.op("dve", lambda e: e.tensor_tensor(ta, x1, cs, ALU.mult), [rl, R_tab], [R_ta])
            P.op("pool", lambda e: e.tensor_tensor(tb_, x2, sn, ALU.mult), [rl, R_tab], [R_tb])
            P.op("dve", lambda e: e.tensor_tensor(dst[:, :, 0:32], ta, tb_, ALU.subtract), [R_ta, R_tb], [rdst])
            P.op("dve", lambda e: e.tensor_tensor(ta, x2, cs, ALU.mult), [rl, R_tab], [R_ta])
            P.op("pool", lambda e: e.tensor_tensor(tb_, x1, sn, ALU.mult), [rl, R_tab], [R_tb])
            P.op("dve", lambda e: e.tensor_tensor(dst[:, :, 32:64], ta, tb_, ALU.add), [R_ta, R_tb], [rdst])


        NG = DN_GROUPS
        HG = 8 // NG
        qTt = [A.alloc((8, 128), BF16) for _ in range(2)]
        kTt = [A.alloc((8, 128), BF16) for _ in range(2)]
        ktm = [A.alloc((8, 128), BF16) for _ in range(2)]
        vtm = [A.alloc((8, 128), BF16) for _ in range(2)]
        zt = [A.alloc((1024,), BF16) for _ in range(2)]
        R_in = [Res("dnin0"), Res("dnin1")]
        bf_names = ("Bm", "attnT", "qgT", "Xw", "Xu", "kdec", "Pa", "Pb", "Ma", "Mb", "Mta", "Mtb", "WT", "vnew",
                    "Da", "Db", "Bd", "Bdt", "Cb", "Cbt", "G", "G2", "Mtc")
        f_names = ("dd", "dS_", "GS", "GT", "KKg", "egb", "U")
        T_ = {}
        RR = [dict() for _ in range(NG)]
        for nme in bf_names + f_names:
            T_[nme] = A.alloc((8, 128), BF16 if nme in bf_names else F32)
            for g in range(NG):
                RR[g][nme] = Res(nme + str(g))
        for nme, al in (("osb2", "dd"), ("sq2", "dS_"), ("szl", "GS")):
            T_[nme] = T_[al]
            for g in range(NG):
                RR[g][nme] = RR[g][al]
        st9 = A.alloc((4, 8))
        R_st9 = [Res("st9_%d" % g) for g in range(NG)]
        Sd = A.alloc((8, 128))
        Sdb = A.alloc((8, 128), BF16)
        R_Sd = [Res("Sd%d" % g) for g in range(NG)]
        R_Sdb = [Res("Sdb%d" % g) for g in range(NG)]
        P.op("pool", lambda e: e.memset(Sd, 0.0), [], R_Sd)
        P.op("pool", lambda e: e.memset(Sdb, 0.0), [], R_Sdb)
        bkc = [0]

        def nbk():
            n = bkc[0] % 8
            bkc[0] += 1
            return n

        def pb_(n):
            return psum[n // 2][:, (n % 2) * 512:(n % 2 + 1) * 512]

        def prb(n):
            return PR[n // 2][n % 2]


        def rt_loads(t):
            for hf in range(2):
                P.dma("pool", ld[t % 2][:, hf * 1536:(hf + 1) * 1536], pTM[t * 128:(t + 1) * 128, 1024 + hf * 1536:1024 + (hf + 1) * 1536],
                      reads=[R_pTM], writes=[R_ld[t % 2]])

        def rt_gen(t):
            l_ = ld[t % 2]
            rl = R_ld[t % 2]
            qv = l_[:, 0:512].rearrange("p (h d) -> p h d", h=8)
            kv = l_[:, 512:1024].rearrange("p (h d) -> p h d", h=8)
            vv = l_[:, 1024:2048].rearrange("p (h d) -> p h d", h=8)
            gv = l_[:, 2048:3072]
            rotary(qv, qr, R_qr, t, rl)
            rotary(kv, kr, R_kr, t, rl)
            P.op("pool", lambda e: e.tensor_tensor(kd, kr, bc(gk.unsqueeze(2), (128, 8, 64)), ALU.mult), [R_kr, R_tab], [R_kd])
            P.op("act", lambda e: e.activation(sgt, gv, AF.Silu), [rl], [R_sg])
            yield
            nq, nk = nbk(), nbk()
            pq = pb_(nq).bitcast(BF16)
            pk_ = pb_(nk).bitcast(BF16)
            for h in range(8):
                P.op("pe", lambda e, h=h: e.transpose(pq[0:64, h * 128:(h + 1) * 128], qr[:, h, :], ident_b), [R_qr, R_c2], [prb(nq)])
            for h in range(8):
                P.op("pe", lambda e, h=h: e.transpose(pk_[0:64, h * 128:(h + 1) * 128], kr[:, h, :], ident_b), [R_kr, R_c2], [prb(nk)])
            pq3 = pq[0:64, :].rearrange("p (h n) -> p h n", h=8)
            pk3 = pk_[0:64, :].rearrange("p (h n) -> p h n", h=8)
            P.op("act", lambda e: e.copy(qT, pq3), [prb(nq)], [R_qT])
            P.op("dve", lambda e: e.tensor_tensor(qdT, pq3, gq, ALU.mult), [prb(nq), R_tab], [R_qdT])
            P.op("act", lambda e: e.copy(kT, pk3), [prb(nk)], [R_kT])
            yield
            ns = [nbk(), nbk()]
            for h in range(8):
                P.op("pe", lambda e, h=h: e.matmul(pb_(ns[h // 4])[:, (h % 4) * 128:(h % 4 + 1) * 128], kT[:, h, :], qT[:, h, :], start=True, stop=True),
                     [R_kT, R_qT], [prb(ns[h // 4])])
            for k in range(2):
                P.op("dve", lambda e, k=k: e.tensor_tensor(
                    PT[:, k * 4:(k + 1) * 4, :], pb_(ns[k]).rearrange("p (h n) -> p h n", h=4),
                    rtmask[:, k * 4:(k + 1) * 4, :], ALU.mult), [prb(ns[k]), R_tab], [R_PT])
            yield
            no = [nbk(), nbk()]
            nd = [nbk(), nbk()]
            for h in range(8):
                P.op("pe", lambda e, h=h: e.matmul(pb_(no[h // 4])[:, (h % 4) * 128:(h % 4 + 1) * 128], PT[:, h, :], vv[:, h, :], start=True, stop=False),
                     [R_PT, rl], [prb(no[h // 4])])
                P.op("pe", lambda e, h=h: e.matmul(pb_(no[h // 4])[:, (h % 4) * 128:(h % 4 + 1) * 128], qdT[:, h, :], Sbf[:, h, :], start=False, stop=True),
                     [R_qdT, R_Sbf], [prb(no[h // 4])])
            for h in range(8):
                P.op("pe", lambda e, h=h: e.matmul(pb_(nd[h // 4])[0:64, (h % 4) * 128:(h % 4 + 1) * 128], kd[:, h, :], vv[:, h, :], start=True, stop=True),
                     [R_kd, rl], [prb(nd[h // 4])])
            for k in range(2):
                P.op("act", lambda e, k=k: e.copy(osb[:, k * 4:(k + 1) * 4, :], pb_(no[k]).rearrange("p (h n) -> p h n", h=4)),
                     [prb(no[k])], [R_osb])
            P.op("dve", lambda e: e.tensor_tensor(Sst, Sst, bc(gC.unsqueeze(2), (64, 8, 128)), ALU.mult), [R_tab], [R_S])
            for k in range(2):
                P.op("dve", lambda e, k=k: e.tensor_tensor(
                    Sst[:, k * 4:(k + 1) * 4, :], Sst[:, k * 4:(k + 1) * 4, :],
                    pb_(nd[k])[0:64, :].rearrange("p (h n) -> p h n", h=4), ALU.add), [prb(nd[k])], [R_S])
            P.op("act", lambda e: e.copy(Sbf, Sst), [R_S], [R_Sbf])
            yield
            P.op("dve", lambda e: e.tensor_reduce(st8[:, 0, :], osb, AX.X, ALU.add), [R_osb], [R_st8])
            P.op("pool", lambda e: e.tensor_tensor(sqb, osb, osb, ALU.mult), [R_osb], [R_sqb])
            P.op("dve", lambda e: e.tensor_reduce(st8[:, 1, :], sqb, AX.X, ALU.add), [R_sqb], [R_st8])
            P.op("dve", lambda e: e.tensor_scalar(st8[:, 2, :], st8[:, 0, :], 1.0 / 128, None, ALU.mult), [R_st8], [R_st8])
            P.op("dve", lambda e: e.tensor_tensor(st8[:, 3, :], st8[:, 2, :], st8[:, 2, :], ALU.mult), [R_st8], [R_st8])
            P.op("dve", lambda e: e.scalar_tensor_tensor(st8[:, 4, :], st8[:, 1, :], 1.0 / 128, st8[:, 3, :], ALU.mult, ALU.subtract),
                 [R_st8], [R_st8])
            P.op("act", lambda e: e.activation(st8[:, 5, :], st8[:, 4, :], AF.Sqrt, bias=epsc[:, 0:1], scale=1.0), [R_st8, R_c2], [R_st8])
            P.op("dve", lambda e: e.reciprocal(st8[:, 4, :], st8[:, 5, :]), [R_st8], [R_st8])
            yield
            P.op("dve", lambda e: e.tensor_tensor(osb, osb, bc(st8[:, 2, :].unsqueeze(2), (128, 8, 128)), ALU.subtract), [R_st8], [R_osb])
            P.op("dve", lambda e: e.tensor_tensor(osb, osb, bc(st8[:, 4, :].unsqueeze(2), (128, 8, 128)), ALU.mult), [R_st8], [R_osb])
            osf = osb.rearrange("p h n -> p (h n)")
            P.op("pool", lambda e: e.tensor_tensor(osf, osf, rtw, ALU.mult), [R_tab], [R_osb])
            P.op("pool", lambda e: e.tensor_tensor(sqb.rearrange("p h n -> p (h n)"), osf, sgt, ALU.mult), [R_osb, R_sg], [R_sqb])
            P.dma("sp", o_scr[t * 128:(t + 1) * 128, 1024:2048], sqb.rearrange("p h n -> p (h n)"), reads=[R_sqb], writes=[R_oscr])

        def gct_prep(t):
            nb = nbk()
            P.op("pe", lambda e: e.matmul(pb_(nb)[0:8, 0:128], gg[:, t, :], utri, start=True, stop=True), [R_dt, R_const], [prb(nb)])
            P.op("act", lambda e: e.copy(gcTt[t % 2][0:8, :], pb_(nb)[0:8, 0:128]), [prb(nb)], [R_gct[t % 2]])

        def dn_loads(t):
            b_ = t % 2
            rin = R_in[b_]
            tsl = slice(t * 128, (t + 1) * 128)
            P.dma("sp", qTt[b_], dn_qT[:, :, tsl].rearrange("h d n -> d h n"), reads=[R_dnq], writes=[rin])
            P.dma("sp", kTt[b_], dn_kT[:, :, tsl].rearrange("h d n -> d h n"), reads=[R_dnk], writes=[rin])
            P.dma("sp", ktm[b_], dn_ktm[tsl], reads=[R_ktm], writes=[rin])
            P.dma("sp", vtm[b_], dn_vtm[tsl], reads=[R_vtm], writes=[rin])
            P.dma("pool", zt[b_], pTM[tsl, 0:1024], reads=[R_pTM], writes=[rin])

        def dn_gen(t, g):
            b_ = t % 2
            rin = R_in[b_]
            tsl = slice(t * 128, (t + 1) * 128)
            hsl = slice(g * HG, (g + 1) * HG)
            rr = RR[g]
            q_, k_, km_, vm_ = qTt[b_], kTt[b_], ktm[b_], vtm[b_]
            Tg = lambda nme: T_[nme][:, hsl, :]
            pvh = lambda n: pb_(n)[:, 0:HG * 128].rearrange("p (h n) -> p h n", h=HG)
            bch = lambda ap2: bc(ap2[:, hsl].unsqueeze(2), (128, HG, 128))
            bcm = lambda m: bc(m.unsqueeze(1), (128, HG, 128))
            I8 = bc(ident_b.unsqueeze(1), (128, HG, 128))

            def mmg(n, lhs, rhs, reads):
                for hl in range(HG):
                    h = g * HG + hl
                    l_ap, r_ap = lhs(h), rhs(h)
                    P.op("pe", lambda e, hl=hl, l_ap=l_ap, r_ap=r_ap: e.matmul(pb_(n)[:, hl * 128:(hl + 1) * 128], l_ap, r_ap, start=True, stop=True),
                         reads, [prb(n)])

            def ew(eng, fn, reads, writes):
                P.op(eng, fn, reads, writes)

            nKK, nQK, nBC = nbk(), nbk(), nbk()
            mmg(nKK, lambda h: k_[:, h, :], lambda h: k_[:, h, :], [rin])
            mmg(nQK, lambda h: k_[:, h, :], lambda h: q_[:, h, :], [rin])
            mmg(nBC, lambda h: oh8[:, h, :], lambda h: gcTt[b_][:, :], [R_dt, R_gct[b_]])
            yield
            ew("dve", lambda e: e.tensor_tensor(Tg("dd"), pvh(nBC), bch(gc_[:, t, :]), ALU.subtract), [prb(nBC), R_dt], [rr["dd"]])
            ew("act", lambda e: e.activation(Tg("egb"), pvh(nBC), AF.Exp), [prb(nBC)], [rr["egb"]])
            ew("dve", lambda e: e.tensor_scalar(Tg("dS_"), Tg("dd"), 0.0, None, ALU.max), [rr["dd"]], [rr["dS_"]])
            ew("act", lambda e: e.activation(Tg("GS"), Tg("dS_"), AF.Exp, scale=-1.0), [rr["dS_"]], [rr["GS"]])
            ew("dve", lambda e: e.tensor_scalar(Tg("dS_"), Tg("dd"), 0.0, None, ALU.min), [rr["dd"], rr["GS"]], [rr["dS_"]])
            ew("act", lambda e: e.activation(Tg("GT"), Tg("dS_"), AF.Exp), [rr["dS_"]], [rr["GT"]])
            ew("pool", lambda e: e.tensor_tensor(Tg("GS"), Tg("GS"), bcm(maskS), ALU.mult), [R_dt], [rr["GS"]])
            ew("pool", lambda e: e.tensor_tensor(Tg("GT"), Tg("GT"), bcm(maskT), ALU.mult), [R_dt], [rr["GT"]])
            ew("dve", lambda e: e.tensor_tensor(Tg("KKg"), pvh(nKK), Tg("GS"), ALU.mult), [prb(nKK), rr["GS"]], [rr["KKg"]])
            ew("pool", lambda e: e.tensor_tensor(Tg("Bm"), Tg("KKg"), bch(negb[:, t, :]), ALU.mult), [rr["KKg"], R_dt], [rr["Bm"]])
            ew("dve", lambda e: e.tensor_tensor(Tg("attnT"), pvh(nQK), Tg("GT"), ALU.mult), [prb(nQK), rr["GT"]], [rr["attnT"]])
            ew("pool", lambda e: e.tensor_tensor(Tg("qgT"), q_[:, hsl, :], Tg("egb"), ALU.mult), [rin, rr["egb"]], [rr["qgT"]])
            ew("pool", lambda e: e.tensor_tensor(Tg("Xw"), km_[:, hsl, :], bch(bw[:, t, :]), ALU.mult), [rin, R_dt], [rr["Xw"]])
            ew("pool", lambda e: e.tensor_tensor(Tg("Xu"), vm_[:, hsl, :], bch(beta[:, t, :]), ALU.mult), [rin, R_dt], [rr["Xu"]])
            ew("pool", lambda e: e.tensor_tensor(Tg("kdec"), km_[:, hsl, :], bch(kds[:, t, :]), ALU.mult), [rin, R_dt], [rr["kdec"]])
            yield
            nTr = nbk()
            ptv = pb_(nTr).bitcast(BF16)
            for hl in range(HG):
                h = g * HG + hl
                P.op("pe", lambda e, hl=hl, h=h: e.transpose(ptv[:, hl * 128:(hl + 1) * 128], T_["Bm"][:, h, :], ident_b), [rr["Bm"], R_c2], [prb(nTr)])
            ew("act", lambda e: e.copy(Tg("Mta"), ptv[:, 0:HG * 128].rearrange("p (h n) -> p h n", h=HG)), [prb(nTr)], [rr["Mta"]])
            ew("dve", lambda e: e.tensor_tensor(Tg("Bd"), Tg("Bm"), bcm(mBD), ALU.mult), [rr["Bm"], R_dt], [rr["Bd"]])
            ew("pool", lambda e: e.tensor_tensor(Tg("Bdt"), Tg("Mta"), bcm(mBDT), ALU.mult), [rr["Mta"], R_dt], [rr["Bdt"]])
            ew("dve", lambda e: e.tensor_tensor(Tg("Pa"), Tg("Bdt"), I8, ALU.add), [rr["Bdt"], R_c2], [rr["Pa"]])
            ew("pool", lambda e: e.tensor_tensor(Tg("Da"), Tg("Bd"), I8, ALU.add), [rr["Bd"], R_c2], [rr["Da"]])
            yield
            Mc, Mtc, Ec, Dc = "Bd", "Bdt", "Pa", "Da"
            for lev in range(2):
                Mn = "Ma" if Mc != "Ma" else "Mb"
                Mtn = "Mtb" if Mtc != "Mtb" else "Mtc"
                En = "Pb" if Ec != "Pb" else "Pa"
                Dn = "Db" if Dc != "Db" else "Da"
                nM, nMt = nbk(), nbk()
                mmg(nM, lambda h, Mtc=Mtc: T_[Mtc][:, h, :], lambda h, Mc=Mc: T_[Mc][:, h, :], [rr[Mtc], rr[Mc]])
                mmg(nMt, lambda h, Mc=Mc: T_[Mc][:, h, :], lambda h, Mtc=Mtc: T_[Mtc][:, h, :], [rr[Mtc], rr[Mc]])
                ew("act", lambda e, Mn=Mn, nM=nM: e.copy(Tg(Mn), pvh(nM)), [prb(nM)], [rr[Mn]])
                ew("dve", lambda e, Mtn=Mtn, nMt=nMt: e.tensor_copy(Tg(Mtn), pvh(nMt)), [prb(nMt)], [rr[Mtn]])
                yield
                nE, nD = nbk(), nbk()
                mmg(nE, lambda h, Mn=Mn: T_[Mn][:, h, :], lambda h, Ec=Ec: T_[Ec][:, h, :], [rr[Mn], rr[Ec]])
                mmg(nD, lambda h, Mtn=Mtn: T_[Mtn][:, h, :], lambda h, Dc=Dc: T_[Dc][:, h, :], [rr[Mtn], rr[Dc]])
                ew("dve", lambda e, En=En, Ec=Ec, nE=nE: e.tensor_tensor(Tg(En), pvh(nE), Tg(Ec), ALU.add), [prb(nE), rr[Ec]], [rr[En]])
                ew("dve", lambda e, Dn=Dn, Dc=Dc, nD=nD: e.tensor_tensor(Tg(Dn), pvh(nD), Tg(Dc), ALU.add), [prb(nD), rr[Dc]], [rr[Dn]])
                yield
                Mc, Mtc, Ec, Dc = Mn, Mtn, En, Dn
            for bi in range(4):
                En = "Pb" if Ec != "Pb" else "Pa"
                Dn = "Db" if Dc != "Db" else "Da"
                ew("dve", lambda e, bi=bi: e.tensor_tensor(Tg("Cb"), Tg("Bm"), bcm(mCs[bi]), ALU.mult), [rr["Bm"], R_dt], [rr["Cb"]])
                nG = nbk()
                mmg(nG, lambda h: T_["Cb"][:, h, :], lambda h, Ec=Ec: T_[Ec][:, h, :], [rr["Cb"], rr[Ec]])
                ew("act", lambda e, nG=nG: e.copy(Tg("G"), pvh(nG)), [prb(nG)], [rr["G"]])
                if bi < 3:
                    ew("pool", lambda e, bi=bi: e.tensor_tensor(Tg("Cbt"), Tg("Mta"), bcm(mCTs[bi]), ALU.mult), [rr["Mta"], R_dt], [rr["Cbt"]])
                    nG2 = nbk()
                    mmg(nG2, lambda h: T_["Cbt"][:, h, :], lambda h, Dc=Dc: T_[Dc][:, h, :], [rr["Cbt"], rr[Dc]])
                    ew("act", lambda e, nG2=nG2: e.copy(Tg("G2"), pvh(nG2)), [prb(nG2)], [rr["G2"]])
                yield
                nH = nbk()
                mmg(nH, lambda h, Dc=Dc: T_[Dc][:, h, :], lambda h: T_["G"][:, h, :], [rr[Dc], rr["G"]])
                ew("dve", lambda e, En=En, Ec=Ec, nH=nH: e.tensor_tensor(Tg(En), pvh(nH), Tg(Ec), ALU.add), [prb(nH), rr[Ec]], [rr[En]])
                if bi < 3:
                    nH2 = nbk()
                    mmg(nH2, lambda h, Ec=Ec: T_[Ec][:, h, :], lambda h: T_["G2"][:, h, :], [rr[Ec], rr["G2"]])
                    ew("dve", lambda e, Dn=Dn, Dc=Dc, nH2=nH2: e.tensor_tensor(Tg(Dn), pvh(nH2), Tg(Dc), ALU.add), [prb(nH2), rr[Dc]], [rr[Dn]])
                    Dc = Dn
                Ec = En
                yield
            Pc = Ec
            nW, nU = nbk(), nbk()
            mmg(nW, lambda h: T_["Xw"][:, h, :], lambda h: T_[Pc][:, h, :], [rr["Xw"], rr[Pc]])
            mmg(nU, lambda h: T_[Pc][:, h, :], lambda h: T_["Xu"][:, h, :], [rr["Xu"], rr[Pc]])
            ew("act", lambda e: e.copy(Tg("WT"), pvh(nW)), [prb(nW)], [rr["WT"]])
            ew("act", lambda e: e.copy(Tg("U"), pvh(nU)), [prb(nU)], [rr["U"]])
            yield
            nWS = nbk()
            mmg(nWS, lambda h: T_["WT"][:, h, :], lambda h: Sdb[:, h, :], [rr["WT"], R_Sdb[g]])
            ew("dve", lambda e: e.tensor_tensor(Tg("vnew"), Tg("U"), pvh(nWS), ALU.subtract), [prb(nWS), rr["U"]], [rr["vnew"]])
            yield
            nO, nDS = nbk(), nbk()
            for hl in range(HG):
                h = g * HG + hl
                P.op("pe", lambda e, hl=hl, h=h: e.matmul(pb_(nO)[:, hl * 128:(hl + 1) * 128], T_["attnT"][:, h, :], T_["vnew"][:, h, :], start=True, stop=False),
                     [rr["attnT"], rr["vnew"]], [prb(nO)])
                P.op("pe", lambda e, hl=hl, h=h: e.matmul(pb_(nO)[:, hl * 128:(hl + 1) * 128], T_["qgT"][:, h, :], Sdb[:, h, :], start=False, stop=True),
                     [rr["qgT"], R_Sdb[g]], [prb(nO)])
            mmg(nDS, lambda h: T_["kdec"][:, h, :], lambda h: T_["vnew"][:, h, :], [rr["kdec"], rr["vnew"]])
            ew("dve", lambda e: e.tensor_tensor(Sd[:, hsl, :], Sd[:, hsl, :], bch(ecd[:, t, :]), ALU.mult), [R_dt], [R_Sd[g]])
            ew("dve", lambda e: e.tensor_tensor(Sd[:, hsl, :], Sd[:, hsl, :], pvh(nDS), ALU.add), [prb(nDS)], [R_Sd[g]])
            ew("act", lambda e: e.copy(Sdb[:, hsl, :], Sd[:, hsl, :]), [R_Sd[g]], [R_Sdb[g]])
            yield
            ew("act", lambda e: e.copy(Tg("osb2"), pvh(nO)), [prb(nO)], [rr["osb2"]])
            ew("pool", lambda e: e.tensor_tensor(Tg("sq2"), Tg("osb2"), Tg("osb2"), ALU.mult), [rr["osb2"]], [rr["sq2"]])
            ew("dve", lambda e: e.tensor_reduce(st9[:, 0, hsl], Tg("sq2"), AX.X, ALU.add), [rr["sq2"]], [R_st9[g]])
            ew("act", lambda e: e.activation(st9[:, 1, hsl], st9[:, 0, hsl], AF.Sqrt, bias=epsc[:, 0:1], scale=1.0 / 128), [R_c2], [R_st9[g]])
            ew("dve", lambda e: e.reciprocal(st9[:, 2, hsl], st9[:, 1, hsl]), [], [R_st9[g]])
            ew("dve", lambda e: e.tensor_tensor(Tg("osb2"), Tg("osb2"), bch(st9[:, 2, :]), ALU.mult), [R_st9[g]], [rr["osb2"]])
            ew("pool", lambda e: e.tensor_tensor(Tg("osb2"), Tg("osb2"), bcm(dnw), ALU.mult), [R_dt], [rr["osb2"]])
            zsl = slice(g * HG * 128, (g + 1) * HG * 128)
            ew("act", lambda e: e.activation(Tg("szl").rearrange("p h n -> p (h n)"), zt[b_][:, zsl], AF.Silu), [rin], [rr["szl"]])
            ew("pool", lambda e: e.tensor_tensor(Tg("sq2"), Tg("osb2"), Tg("szl"), ALU.mult), [rr["osb2"], rr["szl"]], [rr["sq2"]])
            P.dma("sp", o_scr[tsl, zsl], Tg("sq2"), reads=[rr["sq2"]], writes=[R_oscr])

        dn_loads(0)
        rt_loads(0)
        gct_prep(0)
        for t in range(NT):
            if t + 1 < NT:
                dn_loads(t + 1)
                rt_loads(t + 1)
                gct_prep(t + 1)
            gens = [dn_gen(t, g) for g in range(NG)] + [rt_gen(t)]
            while gens:
                for gg_ in list(gens):
                    try:
                        next(gg_)
                    except StopIteration:
                        gens.remove(gg_)
        P.barrier()
        A.release(dn_mark)
        if dbg == "dn":
            P.dma("sp", out[0:128, 0:128], dnw, final=True)
            P.run()
            return nc, dbg_out


        b_mark = A.mark()
        R_bc = Res("b_const")
        idxt = A.alloc((8,), I32)
        P.dma("sp", idxt, own_idx[:, :], writes=[R_bc])
        wr = A.alloc((NKC, 64))
        P.dma("sp", wr, w_router.rearrange("(c p) n -> p c n", p=128), writes=[R_bc])
        rb = A.alloc((64,))
        P.dma("sp", rb, router_bias.partition_broadcast(128), writes=[R_bc])
        Wc = A.alloc((8, 65))
        R_Wc = Res("Wc")
        P.op("pool", lambda e: e.memset(Wc, 1.0), [], [R_Wc])
        h2T = A.alloc((NKC, OWN), BF16)
        R_h2T = [Res("h2T%d" % i) for i in range(8)]
        moe_mark = A.mark()
        gm = A.alloc((D,))
        tmpr = A.alloc((D,))
        P.dma("sp", gm, mod_scr[0:1, 2 * D:3 * D].partition_broadcast(128), reads=[R_modscr], writes=[R_bc])
        P.dma("sp", tmpr, g_post1.partition_broadcast(128), writes=[R_bc])
        P.op("dve", lambda e: e.tensor_tensor(gm, gm, tmpr, ALU.mult), [], [R_bc])
        wo = A.alloc((NKC, D), BF16)
        R_wo = Res("wo")
        w_out_v = w_out.rearrange("(c p) n -> p c n", p=128)
        for q4 in range(4):
            P.dma("pool", wo[:, :, q4 * 512:(q4 + 1) * 512], w_out_v[:, :, q4 * 512:(q4 + 1) * 512], writes=[R_wo])
        og = A.alloc((D,))
        ogb = A.alloc((D,), BF16)
        oT = A.alloc((NKC, 128), BF16)
        ysb = A.alloc((D,))
        xot = A.alloc((D,))
        x1t = A.alloc((D,))
        xn2 = A.alloc((D,))
        hTf = A.alloc((NKC, 128))
        rsb = A.alloc((4,))
        rt_ = A.alloc((16, 64))
        R_og, R_ogb, R_oT, R_ysb, R_xot, R_x1t, R_xn2, R_hTf, R_rsb, R_rt = [Res() for _ in range(10)]
        R_x1scr = Res("x1scr")
        sc_, sel_, eq_, sel2_, selm_, ch_ = [rt_[:, i, :] for i in range(6)]
        m1_ = rt_[:, 6, 0:8]
        m2_ = rt_[:, 6, 8:16]
        gs_ = rt_[:, 6, 16:24]
        keep_ = rt_[:, 6, 24:32]
        kt_ = rt_[:, 6, 32:40]
        top8 = rt_[:, 7, 0:8]
        top8b = rt_[:, 7, 8:16]
        den_ = rt_[:, 7, 16:17]
        rden_ = rt_[:, 7, 17:18]

        def rms_rstd2(src, rsrc, col):
            P.op("pool", lambda e: e.memset(rsb[:, col:col + 2], 0.0), [], [R_rsb])
            P.op("act", lambda e: e.activation(xn2, src, AF.Square, accum_out=rsb[:, col:col + 1]), [rsrc], [R_xn2, R_rsb])
            P.op("act", lambda e: e.activation(rsb[:, col + 1:col + 2], rsb[:, col:col + 1], AF.Sqrt, bias=epsc[:, 0:1], scale=1.0 / D), [R_c2], [R_rsb])
            P.op("dve", lambda e: e.reciprocal(rsb[:, col:col + 1], rsb[:, col + 1:col + 2]), [], [R_rsb])

        def phase_b_tile(tt):
            tsl = slice(tt * 128, (tt + 1) * 128)
            P.op("pool", lambda e: e.indirect_dma_start(out=og, out_offset=None, in_=o_scr[:, :],
                                                        in_offset=bass.IndirectOffsetOnAxis(ap=idxt[:, tt:tt + 1], axis=0)),
                 [R_oscr, R_bc], [R_og], dma=True)
            P.dma("sp", xot, xo[tsl, :], writes=[R_xot])
            P.op("act", lambda e: e.copy(ogb, og), [R_og], [R_ogb])
            for half in range(2):
                pv = psum[0][:, half * 512:(half + 1) * 512].bitcast(BF16)
                for q in range(8):
                    kc = half * 8 + q
                    P.op("pe", lambda e, pv=pv, q=q, kc=kc: e.transpose(pv[:, q * 128:(q + 1) * 128], ogb[:, kc * 128:(kc + 1) * 128], ident_b),
                         [R_ogb, R_c2], [PR[0][half]])
                P.op("act" if half == 0 else "dve", lambda e, pv=pv, half=half: (e.copy if half == 0 else e.tensor_copy)(
                    oT[:, half * 8:(half + 1) * 8, :], pv.rearrange("p (c n) -> p c n", c=8)), [PR[0][half]], [R_oT])
            for db in range(4):
                pi, pk = 1 + db // 2, db % 2
                for kc in range(NKC):
                    P.op("pe", lambda e, kc=kc, db=db, pi=pi, pk=pk: e.matmul(pbank(pi, pk), oT[:, kc, :], wo[:, kc, db * 512:(db + 1) * 512],
                                                                            start=(kc == 0), stop=(kc == NKC - 1)), [R_oT, R_wo], [PR[pi][pk]])
                P.op("act", lambda e, db=db, pi=pi, pk=pk: e.copy(ysb[:, db * 512:(db + 1) * 512], pbank(pi, pk)), [PR[pi][pk]], [R_ysb])
            rms_rstd2(ysb, R_ysb, 0)
            P.op("dve", lambda e: e.scalar_tensor_tensor(ysb, ysb, rsb[:, 0:1], gm, ALU.mult, ALU.mult), [R_rsb, R_bc], [R_ysb])
            P.op("dve", lambda e: e.tensor_tensor(x1t, ysb, xot, ALU.add), [R_ysb, R_xot], [R_x1t])
            P.dma("sp", x1_scr[tsl, :], x1t, reads=[R_x1t], writes=[R_x1scr])
            rms_rstd2(x1t, R_x1t, 2)
            P.op("dve", lambda e: e.tensor_scalar(xn2, x1t, rsb[:, 2:3], None, ALU.mult), [R_x1t, R_rsb], [R_xn2])
            for g4 in range(4):
                pi, pk = (g4 % 2), (g4 // 2)
                for q in range(4):
                    kc = g4 * 4 + q
                    P.op("pe", lambda e, pi=pi, pk=pk, q=q, kc=kc: e.transpose(pbank(pi, pk)[:, q * 128:(q + 1) * 128], xn2[:, kc * 128:(kc + 1) * 128], ident_f),
                         [R_xn2, R_const], [PR[pi][pk]])
                sl4 = slice(g4 * 4, (g4 + 1) * 4)
                P.op("dve", lambda e, pi=pi, pk=pk, sl4=sl4: e.tensor_tensor(
                    hTf[:, sl4, :], pbank(pi, pk).rearrange("p (c n) -> p c n", c=4), bc(a2[:, sl4].unsqueeze(2), (128, 4, 128)), ALU.mult),
                    [PR[pi][pk], R_a], [R_hTf])
            P.op("pool", lambda e: e.tensor_tensor(hTf, hTf, bc(s2.unsqueeze(2), (128, NKC, 128)), ALU.add), [R_modfm], [R_hTf])
            P.op("act", lambda e: e.copy(h2T[:, :, tsl], hTf), [R_hTf], [R_h2T[tt]])
            for kc in range(NKC):
                P.op("pe", lambda e, kc=kc: e.matmul(pbank(2, 0)[:, 0:64], hTf[:, kc, :], wr[:, kc, :], start=(kc == 0), stop=(kc == NKC - 1)),
                     [R_hTf, R_bc], [PR[2][0]])
            V = lambda fn, rd=(): P.op("dve", fn, list(rd) + [R_rt, R_bc], [R_rt])
            P.op("act", lambda e: e.activation(sc_, pbank(2, 0)[:, 0:64], AF.Sigmoid), [PR[2][0]], [R_rt])
            V(lambda e: e.tensor_tensor(sel_, sc_, rb, ALU.add))
            g3 = lambda ap: ap.rearrange("p (g k) -> p g k", g=8)
            V(lambda e: e.tensor_reduce(m1_, g3(sel_), AX.X, ALU.max))
            V(lambda e: e.tensor_tensor(g3(eq_), g3(sel_), bc(m1_.unsqueeze(2), (128, 8, 8)), ALU.is_equal))
            V(lambda e: e.scalar_tensor_tensor(sel2_, eq_, -1.0e9, sel_, ALU.mult, ALU.add))
            V(lambda e: e.tensor_reduce(m2_, g3(sel2_), AX.X, ALU.max))
            V(lambda e: e.tensor_tensor(gs_, m1_, m2_, ALU.add))
            V(lambda e: e.max(top8, gs_))
            V(lambda e: e.tensor_scalar(keep_, gs_, top8[:, 3:4], None, ALU.is_ge))
            V(lambda e: e.tensor_scalar(kt_, keep_, 1.0e3, -1.0e3, ALU.mult, ALU.add))
            V(lambda e: e.tensor_tensor(g3(selm_), g3(sel_), bc(keep_.unsqueeze(2), (128, 8, 8)), ALU.mult))
            V(lambda e: e.tensor_tensor(g3(selm_), g3(selm_), bc(kt_.unsqueeze(2), (128, 8, 8)), ALU.add))
            V(lambda e: e.max(top8b, selm_))
            V(lambda e: e.tensor_scalar(ch_, selm_, top8b[:, 7:8], None, ALU.is_ge))
            V(lambda e: e.tensor_tensor(ch_, ch_, sc_, ALU.mult))
            V(lambda e: e.tensor_reduce(den_, ch_, AX.X, ALU.add))
            V(lambda e: e.reciprocal(rden_, den_))
            P.op("dve", lambda e: e.tensor_scalar(Wc[:, tt, 0:64], ch_, rden_, 2.5, ALU.mult, ALU.mult), [R_rt], [R_Wc])

        for tt in range(8):
            phase_b_tile(tt)
        if dbg == "b":
            P.dma("sp", dbg_a[:, 0:520], Wc.rearrange("p t e -> p (t e)"), reads=[R_Wc], final=True)
            P.dma("sp", dbg_a[:, 1024:1536], h2T[:, 0, :].bitcast(F32), reads=R_h2T, final=True)
            P.dma("sp", out[0:128, :], x1t, reads=[R_x1t], final=True)
            P.run()
            return nc, dbg_out
        P.barrier()
        A.release(moe_mark)


        NE = 65 if MOE_EXPERTS is None else MOE_EXPERTS
        acc = A.alloc((8, D))
        R_acc = [Res("acc%d" % i) for i in range(8)]
        P.op("pool", lambda e: e.memset(acc, 0.0), [], R_acc)
        wg = [A.alloc((NKC, 512), BF16) for _ in range(2)]
        wu = [A.alloc((NKC, 512), BF16) for _ in range(2)]
        wd = [A.alloc((4, D), BF16) for _ in range(1)]
        R_wg = [Res("wg0"), Res("wg1")]
        R_wu = [Res("wu0"), Res("wu1")]
        R_wd = [Res("wd0")]
        actT = A.alloc((4, OWN), BF16)
        R_act = [[Res("act%d_%d" % (f, tb)) for tb in range(2)] for f in range(4)]
        sgm = [A.alloc((512,), BF16) for _ in range(2)]
        R_sgm = [Res("sgm0"), Res("sgm1")]
        bcnt = [0]

        def nbank():
            n = bcnt[0] % 8
            bcnt[0] += 1
            return n // 2, n % 2

        def moe_expert(ex):
            b_ = ex % 2
            g_, u_, d_ = wg[b_], wu[b_], wd[0]
            rg_, ru_, rd_ = R_wg[b_], R_wu[b_], R_wd[0]
            P.dma("pool", g_, w_gate[ex].rearrange("(c p) n -> p c n", p=128), writes=[rg_])
            P.dma("pool", u_, w_up[ex].rearrange("(c p) n -> p c n", p=128), writes=[ru_])
            P.dma("pool", d_, w_down[ex].rearrange("(c p) n -> p c n", p=128), writes=[rd_])
            scnt = 0
            for f in range(4):
                for tb in range(2):
                    gi, gk_ = nbank()
                    ui, uk_ = nbank()
                    for kc in range(NKC):
                        P.op("pe", lambda e, kc=kc, f=f, tb=tb, gi=gi, gk_=gk_: e.matmul(
                            pbank(gi, gk_), g_[:, kc, f * 128:(f + 1) * 128], h2T[:, kc, tb * 512:(tb + 1) * 512],
                            start=(kc == 0), stop=(kc == NKC - 1)), [rg_] + R_h2T[tb * 4:(tb + 1) * 4], [PR[gi][gk_]])
                    for kc in range(NKC):
                        P.op("pe", lambda e, kc=kc, f=f, tb=tb, ui=ui, uk_=uk_: e.matmul(
                            pbank(ui, uk_), u_[:, kc, f * 128:(f + 1) * 128], h2T[:, kc, tb * 512:(tb + 1) * 512],
                            start=(kc == 0), stop=(kc == NKC - 1)), [ru_] + R_h2T[tb * 4:(tb + 1) * 4], [PR[ui][uk_]])
                    sg_ = sgm[scnt % 2]
                    rs_ = R_sgm[scnt % 2]
                    scnt += 1
                    P.op("act", lambda e, sg_=sg_, gi=gi, gk_=gk_: e.activation(sg_, pbank(gi, gk_), AF.Silu), [PR[gi][gk_]], [rs_])
                    P.op("dve", lambda e, sg_=sg_, ui=ui, uk_=uk_, f=f, tb=tb: e.tensor_tensor(
                        actT[:, f, tb * 512:(tb + 1) * 512], pbank(ui, uk_), sg_, ALU.mult), [PR[ui][uk_], rs_], [R_act[f][tb]])
            for tt in range(8):
                for db in range(4):
                    di, dk_ = nbank()
                    for f in range(4):
                        P.op("pe", lambda e, f=f, tt=tt, db=db, di=di, dk_=dk_: e.matmul(
                            pbank(di, dk_), actT[:, f, tt * 128:(tt + 1) * 128], d_[:, f, db * 512:(db + 1) * 512],
                            start=(f == 0), stop=(f == 3)), [R_act[f][tt // 4], rd_], [PR[di][dk_]])
                    P.op("dve", lambda e, tt=tt, db=db, di=di, dk_=dk_: e.scalar_tensor_tensor(
                        acc[:, tt, db * 512:(db + 1) * 512], pbank(di, dk_), Wc[:, tt, ex:ex + 1], acc[:, tt, db * 512:(db + 1) * 512],
                        ALU.mult, ALU.add), [PR[di][dk_], R_Wc], [R_acc[tt]])

        for ex in range(NE):
            moe_expert(ex)
        P.barrier()
        A.release(moe_mark)
        acc2 = A.alloc((8, D))
        gf = A.alloc((D,))
        tmpf = A.alloc((D,))
        R_gf = Res("gf")
        P.dma("sp", gf, mod_scr[0:1, 5 * D:6 * D].partition_broadcast(128), reads=[R_modscr], writes=[R_gf])
        P.dma("sp", tmpf, g_post2.partition_broadcast(128), writes=[R_gf])
        P.op("dve", lambda e: e.tensor_tensor(gf, gf, tmpf, ALU.mult), [], [R_gf])
        x1l = [A.alloc((D,)) for _ in range(2)]
        R_x1l = [Res("x1l0"), Res("x1l1")]
        junk = A.alloc((D,))
        R_junk = Res("junk")
        rsf = A.alloc((8, 2))
        R_rsf = Res("rsf")
        P.op("pool", lambda e: e.memset(rsf, 0.0), [], [R_rsf])
        for tt in range(8):
            tsl = slice(tt * 128, (tt + 1) * 128)
            xl = x1l[tt % 2]
            rxl = R_x1l[tt % 2]
            P.dma("sp", xl, x1_scr[tsl, :], reads=[R_x1scr], writes=[rxl])
            at = acc2[:, tt, :]
            P.op("act", lambda e, at=at, tt=tt: e.activation(junk, at, AF.Square, accum_out=rsf[:, tt, 0:1]), [R_acc[tt]], [R_junk, R_rsf])
            P.op("act", lambda e, tt=tt: e.activation(rsf[:, tt, 1:2], rsf[:, tt, 0:1], AF.Sqrt, bias=epsc[:, 0:1], scale=1.0 / D), [R_c2], [R_rsf])
            P.op("dve", lambda e, tt=tt: e.reciprocal(rsf[:, tt, 0:1], rsf[:, tt, 1:2]), [], [R_rsf])
            P.op("dve", lambda e, at=at, tt=tt: e.scalar_tensor_tensor(at, at, rsf[:, tt, 0:1], gf, ALU.mult, ALU.mult), [R_rsf, R_gf], [R_acc[tt]])
            P.op("dve", lambda e, at=at, xl=xl: e.tensor_tensor(xl, at, xl, ALU.add), [R_acc[tt]], [rxl])
            P.dma("sp", out[tsl, :], xl, reads=[rxl], final=True)
        P.run()
    return nc, dbg_out


W_IN_ORDER = None


def _w_in_perm():
    sp = np.cumsum([0, 1024, 1024, 1024, 1024, 8, 8, 512, 512, 1024, 1024])
    seg = lambda i: np.arange(sp[i], sp[i + 1])
    return np.concatenate([seg(0), seg(1), seg(2), seg(3), seg(6), seg(7), seg(8), seg(9), seg(4), seg(5)])


def host_inputs(inputs, core, light=False):
    b, j = core // 4, core % 4
    f = lambda a: np.ascontiguousarray(a, dtype=np.float32)
    fm = lambda v: f(np.asarray(v).reshape(NKC, 128).T)
    m = {}
    x = np.asarray(inputs["x"])
    m["xb"] = f(x[b])
    m["xo"] = f(x[b, j * OWN:(j + 1) * OWN])
    m["cT"] = fm(inputs["c"][b])
    m["pos"] = np.ascontiguousarray(np.asarray(inputs["positions"])[b].reshape(NT, 128).T.astype(np.int32))
    m["w_ada"] = f(inputs["w_ada"][0])
    m["b_ada"] = f(inputs["b_ada"][0]).reshape(1, -1)
    m["g_pre1"] = fm(inputs["pre_norm_mix"][0])
    m["g_pre2"] = fm(inputs["pre_norm_ffn"][0])
    m["g_post1"] = f(inputs["post_norm_mix"][0]).reshape(1, -1)
    m["g_post2"] = f(inputs["post_norm_ffn"][0]).reshape(1, -1)
    m["w_in"] = f(np.asarray(inputs["w_in"][0])[:, _w_in_perm()])
    cw = np.asarray(inputs["conv_w"][0])
    m["conv_w"] = f(cw.T.reshape(24, 128, 4).transpose(1, 0, 2))
    m["a_log"] = f(inputs["a_log"][0]).reshape(1, 8)
    m["dt_bias"] = f(inputs["dt_bias"][0]).reshape(1, 8)
    m["dn_norm_w"] = f(inputs["dn_norm_w"][0]).reshape(1, 128)
    m["rt_norm_w"] = f(inputs["rt_norm_w"][0]).reshape(1, 1024)
    m["w_out"] = f(inputs["w_out"][0])
    m["w_router"] = f(inputs["w_router"][0])
    m["router_bias"] = f(inputs["router_bias"][0]).reshape(1, 64)
    if not light:
      m["w_gate"] = f(np.concatenate([np.asarray(inputs["w_gate_exp"][0]), np.asarray(inputs["w_gate_sh"])], 0))
      m["w_up"] = f(np.concatenate([np.asarray(inputs["w_up_exp"][0]), np.asarray(inputs["w_up_sh"])], 0))
      m["w_down"] = f(np.concatenate([np.asarray(inputs["w_down_exp"][0]), np.asarray(inputs["w_down_sh"])], 0))
    m["own_idx"] = np.ascontiguousarray((j * OWN + np.arange(OWN)).reshape(8, 128).T.astype(np.int32))
    i = np.arange(128)
    m["c_ident"] = f(np.eye(128))
    m["c_utri"] = f(i[:, None] <= i[None, :])
    m["c_maskS"] = f(i[None, :] < i[:, None])
    m["c_maskT"] = f(i[None, :] >= i[:, None])
    oh = np.zeros((128, 8, 128), np.float32)
    for h in range(8):
        oh[h, h, :] = 1.0
    m["c_onehot8"] = oh
    lg = np.log(1.0 - 2.0 ** (-5.0 - np.arange(8, dtype=np.float64)))
    diff = (i[None, :] - i[:, None]).astype(np.float64)
    rtm = np.where(diff[:, None, :] >= 0, np.exp(np.maximum(diff[:, None, :], 0) * lg[None, :, None]), 0.0) * 0.125
    m["c_rtmask"] = f(rtm)
    m["c_gq"] = f(np.broadcast_to(np.exp((i[None, None, :] + 1.0) * lg[None, :, None]), (64, 8, 128)))
    m["c_gk"] = f(np.exp((127.0 - i[:, None]) * lg[None, :]) * 0.125)
    m["c_gC"] = f(np.broadcast_to(np.exp(128.0 * lg)[None, :], (64, 8)))
    mBDh = ((i[:, None] // 8) == (i[None, :] // 8)) & (i[None, :] < i[:, None])
    m["c_mBD"] = f(np.stack([mBDh, mBDh.T]))
    mcs = []
    for bsz in (8, 16, 32, 64):
        same = (i[:, None] // (2 * bsz)) == (i[None, :] // (2 * bsz))
        mcs.append(same & ((i[:, None] % (2 * bsz)) >= bsz) & ((i[None, :] % (2 * bsz)) < bsz))
    m["c_mC"] = f(np.stack(mcs + [x.T for x in mcs]))
    m["c_theta"] = f(1.0 / (10000.0 ** np.linspace(0.0, 1.0, 32, dtype=np.float32))).reshape(1, 32)
    return m


def kernel(**inputs):
    nc, _ = build_program(DBG)
    cores = list(range(8)) if DBG_CORES is None else DBG_CORES
    in_maps = [host_inputs(inputs, c, light=DBG not in (None, "moe")) for c in cores]
    res = run_bass_kernel_spmd(nc, in_maps, core_ids=list(range(len(cores))))
    if DBG is not None:
        return res
    outp = np.zeros((2, S, D), np.float32)
    for k, c in enumerate(cores):
        b, j = c // 4, c % 4
        outp[b, j * OWN:(j + 1) * OWN] = res.results[k]["out"]
    return outp
```

```python
import numpy as np
import concourse.bass as bass
import concourse.mybir as mybir
from concourse.bass_utils import run_bass_kernel_spmd
from contextlib import ExitStack

F32 = mybir.dt.float32
BF16 = mybir.dt.bfloat16
I32 = mybir.dt.int32
ALU = mybir.AluOpType
AF = mybir.ActivationFunctionType
AX = mybir.AxisListType

D = 2048
S = 4096
NT = 32
OWN = 1024
NKC = 16
EPS = 1e-6
DBG = None
DBG_CORES = None
DN_TILES = NT
DN_DUMP = False
DN_STAGE = 6
DN_SUB = 0
MOE_EXPERTS = None
DN_GROUPS = 2


class Res:
    __slots__ = ("name", "w", "r", "excl")

    def __init__(self, name="", excl=False):
        self.name = name
        self.w = None
        self.r = []
        self.excl = excl


class Prog:
    COMPUTE = ("pe", "act", "dve", "pool")
    ENG = ("pe", "act", "dve", "pool", "sp")
    ND = 48

    def __init__(self, nc, es):
        self.nc = nc
        self.es = es
        self.ops = {e: [] for e in self.ENG}
        self.cnt = {e: 0 for e in self.COMPUTE}
        self.sems = {}
        for e in self.COMPUTE:
            self.sems[e] = es.enter_context(nc.semaphore("cs_" + e))
        for i in range(self.ND):
            self.sems[("d", i)] = es.enter_context(nc.semaphore("ds%d" % i))
        self.duse = [0] * self.ND
        self.dnext = 0
        self.waited = {e: {} for e in self.ENG}
        self.final = []
        self.nops = 0

    def _wait(self, eng, tok):
        key, val = tok
        if key == eng and eng == "pe":
            return
        if self.waited[eng].get(key, 0) >= val:
            return
        self.waited[eng][key] = val
        self.ops[eng].append(("w", key, val))

    def op(self, eng, fn, reads=(), writes=(), dma=False, final=False):
        for r in reads:
            if r.w is not None:
                self._wait(eng, r.w)
            if r.excl:
                for t in r.r:
                    if t[0] != eng:
                        self._wait(eng, t)
        for w in writes:
            if w.w is not None:
                self._wait(eng, w.w)
            for t in w.r:
                self._wait(eng, t)
        if dma:
            s = self.dnext % self.ND
            self.dnext += 1
            u = self.duse[s]
            if u > 0:
                self._wait(eng, (("d", s), 16 * u))
            self.duse[s] = u + 1
            tok = (("d", s), 16 * (u + 1))
            self.ops[eng].append(("i", fn, ("d", s), 16))
        else:
            self.cnt[eng] += 1
            tok = (eng, self.cnt[eng])
            self.ops[eng].append(("i", fn, eng, 1))
        for r in reads:
            r.r.append(tok)
            if len(r.r) > 24:
                r.r = r.r[-24:] if False else r.r
        for w in writes:
            w.w = tok
            w.r = []
        if final:
            self.final.append(tok)
        self.nops += 1
        return tok

    def dma(self, eng, out, in_, reads=(), writes=(), final=False, **kw):
        return self.op(eng, lambda e: e.dma_start(out=out, in_=in_, **kw), reads, writes, dma=True, final=final)

    def barrier(self):
        toks = [(e, self.cnt[e]) for e in self.COMPUTE if self.cnt[e] > 0]
        toks += [(("d", s), 16 * self.duse[s]) for s in range(self.ND) if self.duse[s] > 0]
        for e in self.ENG:
            for t in toks:
                self._wait(e, t)

    def run(self):
        for t in self.final:
            self._wait("sp", t)
        nc = self.nc
        sems = self.sems

        def replay(eng_name):
            def body(e):
                for o in self.ops[eng_name]:
                    if o[0] == "w":
                        e.wait_ge(sems[o[1]], o[2])
                    else:
                        o[1](e).then_inc(sems[o[2]], o[3])
            return body

        with nc.Block() as block:
            block.tensor(replay("pe"))
            block.scalar(replay("act"))
            block.vector(replay("dve"))
            block.gpsimd(replay("pool"))
            block.sync(replay("sp"))


class Arena:
    def __init__(self, nc, es, words):
        self.t = es.enter_context(nc.sbuf_tensor("arena", [128, words], F32))
        self.words = words
        self.off = 0

    def alloc(self, free, dtype=F32, parts=128):
        n = 1
        for f in free:
            n *= f
        w = n if dtype != BF16 else (n + 1) // 2
        w = (w + 7) // 8 * 8
        assert self.off + w <= self.words, ("arena overflow", self.off, w, self.words)
        ap = self.t[0:parts, self.off:self.off + w]
        self.off += w
        if dtype == BF16:
            ap = ap.bitcast(BF16)
        elif dtype == I32:
            ap = ap.bitcast(I32)
        ap = ap[:, 0:n]
        if len(free) == 2:
            ap = ap.rearrange("p (a b) -> p a b", a=free[0], b=free[1])
        elif len(free) == 3:
            ap = ap.rearrange("p (a b c) -> p a b c", a=free[0], b=free[1], c=free[2])
        return ap

    def mark(self):
        return self.off

    def release(self, m):
        self.off = m


def bc(ap, shape):
    return ap.to_broadcast(list(shape))


def build_program(dbg=None):
    nc = bass.Bass("TRN2", target_bir_lowering=False)
    dbg_out = {}

    def din(name, shape, dt=F32):
        return nc.dram_tensor(name, list(shape), dt, kind="ExternalInput").ap()

    def scratch(name, shape, dt=F32, dump=False):
        kind = "ExternalOutput" if (dbg is not None and dump) else "Internal"
        t = nc.dram_tensor(name, list(shape), dt, kind=kind).ap()
        if kind == "ExternalOutput":
            dbg_out[name] = t
        return t

    xb = din("xb", [S, D])
    xo = din("xo", [OWN, D])
    cT = din("cT", [128, NKC])
    pos = din("pos", [128, NT], I32)
    w_ada = din("w_ada", [D, 6 * D])
    b_ada = din("b_ada", [1, 6 * D])
    g_pre1 = din("g_pre1", [128, NKC])
    g_pre2 = din("g_pre2", [128, NKC])
    g_post1 = din("g_post1", [1, D])
    g_post2 = din("g_post2", [1, D])
    w_in = din("w_in", [D, 7184])
    conv_w = din("conv_w", [128, 24, 4])
    a_log = din("a_log", [1, 8])
    dt_bias = din("dt_bias", [1, 8])
    dn_norm_w = din("dn_norm_w", [1, 128])
    rt_norm_w = din("rt_norm_w", [1, 1024])
    w_out = din("w_out", [D, D])
    w_router = din("w_router", [D, 64])
    router_bias = din("router_bias", [1, 64])
    if dbg in (None, "moe"):
        w_gate = din("w_gate", [65, D, 512])
        w_up = din("w_up", [65, D, 512])
        w_down = din("w_down", [65, 512, D])
    own_idx = din("own_idx", [128, 8], I32)
    c_ident = din("c_ident", [128, 128])
    c_utri = din("c_utri", [128, 128])
    c_maskS = din("c_maskS", [128, 128])
    c_maskT = din("c_maskT", [128, 128])
    c_onehot8 = din("c_onehot8", [128, 8, 128])
    c_rtmask = din("c_rtmask", [128, 8, 128])
    c_gq = din("c_gq", [64, 8, 128])
    c_gk = din("c_gk", [128, 8])
    c_gC = din("c_gC", [64, 8])
    c_theta = din("c_theta", [1, 32])
    c_mBD = din("c_mBD", [2, 128, 128])
    c_mC = din("c_mC", [8, 128, 128])

    out = nc.dram_tensor("out", [OWN, D], F32, kind="ExternalOutput").ap()

    mod_scr = scratch("mod_scr", [1, 6 * D], dump=True)
    pFM = scratch("pFM", [24, 128, S], dump=(dbg == "a1"))
    pTM = scratch("pTM", [S, 4112], dump=(dbg == "a1"))
    dn_qT = scratch("dn_qT", [8, 128, S], BF16)
    dn_kT = scratch("dn_kT", [8, 128, S], BF16)
    dn_ktm = scratch("dn_ktm", [S, 8, 128], BF16)
    dn_vtm = scratch("dn_vtm", [S, 8, 128], BF16)
    o_scr = scratch("o_scr", [S, D], F32, dump=(dbg in ("rt", "dn")))
    x1_scr = scratch("x1_scr", [OWN, D], dump=(dbg == "b"))
    dbg_a = scratch("dbg_a", [128, 2048], dump=True)

    with ExitStack() as es:
        P = Prog(nc, es)
        A = Arena(nc, es, 53000)
        psum = [es.enter_context(nc.psum_tensor("ps%d" % i, [128, 1024], F32)) for i in range(4)]
        PR = [[Res("ps%d_%d" % (i, k), excl=True) for k in range(2)] for i in range(4)]

        def pbank(i, k):
            return psum[i][:, k * 512:(k + 1) * 512]

        ident_f = A.alloc((128,))
        ident_b = A.alloc((128,), BF16)
        utri = A.alloc((128,))
        ones_f = A.alloc((128,))
        ones_b = A.alloc((128,), BF16)
        R_const = Res("const")
        P.dma("sp", ident_f, c_ident[:, :], writes=[R_const])
        P.dma("sp", utri, c_utri[:, :], writes=[R_const])
        R_c2 = Res("c2")
        P.op("act", lambda e: e.copy(ident_b, ident_f), [R_const], [R_c2])
        P.op("pool", lambda e: e.memset(ones_f, 1.0), [], [R_c2])
        P.op("pool", lambda e: e.memset(ones_b, 1.0), [], [R_c2])
        epsc = A.alloc((8,))
        P.op("pool", lambda e: e.memset(epsc, EPS), [], [R_c2])

        base_mark = A.mark()

        cTt = A.alloc((NKC,))
        cTb = A.alloc((NKC,), BF16)
        R_c = Res("c")
        P.dma("sp", cTt, cT[:, :], writes=[R_c])
        P.op("act", lambda e: e.activation(cTb, cTt, AF.Silu), [R_c], [R_c])
        wab = [A.alloc((NKC, 512), BF16) for _ in range(2)]
        R_wab = [Res("wab0"), Res("wab1")]
        modrow = A.alloc((6 * D,), parts=1)
        badar = A.alloc((6 * D,), parts=1)
        R_mod = Res("modrow")
        R_bada = Res("bada")
        P.dma("sp", badar, b_ada[:, :], writes=[R_bada])
        w_ada_v = w_ada.rearrange("(c p) n -> p c n", p=128)
        for jb in range(24):
            wb = wab[jb % 2]
            rw = R_wab[jb % 2]
            P.dma("pool", wb, w_ada_v[:, :, jb * 512:(jb + 1) * 512], writes=[rw])
            pi, pk = (jb % 4) // 2, jb % 2
            for kc in range(NKC):
                P.op("pe", lambda e, kc=kc, wb=wb, pi=pi, pk=pk: e.matmul(
                    pbank(pi, pk)[0:1, :], cTb[:, kc:kc + 1], wb[:, kc, :], start=(kc == 0), stop=(kc == NKC - 1)),
                    [R_c, rw], [PR[pi][pk]])
            P.op("dve", lambda e, jb=jb, pi=pi, pk=pk: e.tensor_tensor(
                modrow[:, jb * 512:(jb + 1) * 512], pbank(pi, pk)[0:1, :], badar[:, jb * 512:(jb + 1) * 512], ALU.add),
                [PR[pi][pk], R_bada], [R_mod])
        R_modscr = Res("modscr")
        P.dma("sp", mod_scr[:, :], modrow, reads=[R_mod], writes=[R_modscr])
        P.barrier()
        A.release(base_mark)

        modfm = A.alloc((96,))
        R_modfm = Res("modfm")
        P.dma("sp", modfm, mod_scr.rearrange("o (c p) -> p (o c)", p=128), reads=[R_modscr], writes=[R_modfm],
              allow_slow_non_contiguous=True)
        gp1 = A.alloc((NKC,))
        gp2 = A.alloc((NKC,))
        P.dma("sp", gp1, g_pre1[:, :], writes=[R_modfm])
        P.dma("sp", gp2, g_pre2[:, :], writes=[R_modfm])
        a1 = A.alloc((NKC,))
        a2 = A.alloc((NKC,))
        R_a = Res("a12")
        P.op("dve", lambda e: e.scalar_tensor_tensor(a1, modfm[:, 16:32], 1.0, gp1, ALU.add, ALU.mult), [R_modfm], [R_a])
        P.op("dve", lambda e: e.scalar_tensor_tensor(a2, modfm[:, 64:80], 1.0, gp2, ALU.add, ALU.mult), [R_modfm], [R_a])
        s1 = modfm[:, 0:16]
        s2 = modfm[:, 48:64]
        const_mark = A.mark()

        hT = A.alloc((NKC, S), BF16)
        R_hT = [Res("hT%d" % t) for t in range(NT)]
        hT_mark = A.mark()
        xt = [A.alloc((D,)) for _ in range(2)]
        R_xt = [Res("xt0"), Res("xt1")]
        sqj = A.alloc((D,), BF16)
        R_sqj = Res("sqj")
        xn = [A.alloc((D,), BF16) for _ in range(2)]
        R_xn = [Res("xn0"), Res("xn1")]
        ss = A.alloc((2, 2))
        R_ss = [Res("ss0"), Res("ss1")]
        tmpT = A.alloc((D,))
        R_tmpT = Res("tmpT")

        def rms_rstd(eng_sq, src, rss, ssap, reads):
            P.op("act", lambda e: e.activation(sqj, src, AF.Square, accum_out=ssap[:, 0:1]), reads, [R_sqj, rss])
            P.op("act", lambda e: e.activation(ssap[:, 1:2], ssap[:, 0:1], AF.Sqrt, bias=epsc[:, 0:1], scale=1.0 / D), [rss, R_c2], [rss])
            P.op("dve", lambda e: e.reciprocal(ssap[:, 0:1], ssap[:, 1:2]), [rss], [rss])

        for t in range(NT):
            x_ = xt[t % 2]
            rx = R_xt[t % 2]
            ssap = ss[:, t % 2, :]
            P.dma("sp", x_, xb[t * 128:(t + 1) * 128, :], writes=[rx])
            P.op("pool", lambda e, ssap=ssap: e.memset(ssap, 0.0), [], [R_ss[t % 2]])
            rms_rstd("act", x_, R_ss[t % 2], ssap, [rx])
            xn_ = xn[t % 2]
            P.op("dve", lambda e, x_=x_, xn_=xn_, ssap=ssap: e.tensor_scalar(xn_, x_, ssap[:, 0:1], None, ALU.mult),
                 [rx, R_ss[t % 2]], [R_xn[t % 2]])
            for half in range(2):
                pv = psum[half][:, :].bitcast(BF16)
                for q in range(8):
                    kc = half * 8 + q
                    P.op("pe", lambda e, pv=pv, q=q, kc=kc, xn_=xn_: e.transpose(
                        pv[:, q * 128:(q + 1) * 128], xn_[:, kc * 128:(kc + 1) * 128], ident_b),
                        [R_xn[t % 2], R_c2], [PR[half][0]])
                pvv = pv[:, 0:1024].rearrange("p (c n) -> p c n", c=8)
                tv = tmpT[:, half * 1024:(half + 1) * 1024].rearrange("p (c n) -> p c n", c=8)
                P.op("dve", lambda e, pvv=pvv, tv=tv, half=half: e.tensor_tensor(
                    tv, pvv, bc(a1[:, half * 8:(half + 1) * 8].unsqueeze(2), (128, 8, 128)), ALU.mult),
                    [PR[half][0], R_a], [R_tmpT])
                P.op("pool", lambda e, tv=tv, half=half, t=t: e.tensor_tensor(
                    hT[:, half * 8:(half + 1) * 8, t * 128:(t + 1) * 128], tv,
                    bc(s1[:, half * 8:(half + 1) * 8].unsqueeze(2), (128, 8, 128)), ALU.add),
                    [R_tmpT, R_modfm], [R_hT[t]])

        P.barrier()
        A.release(hT_mark)
        wfm = [A.alloc((NKC, 128), BF16) for _ in range(2)]
        R_wfm = [Res("wfm0"), Res("wfm1")]
        stg = [A.alloc((1024,)) for _ in range(3)]
        R_stg = [Res("stg%d" % i) for i in range(3)]
        gcnt = 0
        w_in_v = w_in.rearrange("(c p) n -> p c n", p=128)
        R_pFM = [Res("pFM%d" % c) for c in range(24)]
        pcnt = 0
        for cc in range(24):
            wt = wfm[cc % 2]
            rw = R_wfm[cc % 2]
            P.dma("pool", wt, w_in_v[:, :, cc * 128:(cc + 1) * 128], writes=[rw])
            for tb in range(8):
                if tb % 2 == 0:
                    st = stg[gcnt % 3]
                    rs = R_stg[gcnt % 3]
                    gcnt += 1
                pi, pk = (pcnt % 4) // 2 + 2, pcnt % 2
                pcnt += 1
                for kc in range(NKC):
                    P.op("pe", lambda e, kc=kc, wt=wt, tb=tb, pi=pi, pk=pk: e.matmul(
                        pbank(pi, pk), wt[:, kc, :], hT[:, kc, tb * 512:(tb + 1) * 512], start=(kc == 0), stop=(kc == NKC - 1)),
                        [rw] + R_hT[tb * 4:(tb + 1) * 4], [PR[pi][pk]])
                P.op("act", lambda e, st=st, tb=tb, pi=pi, pk=pk: e.copy(st[:, (tb % 2) * 512:(tb % 2 + 1) * 512], pbank(pi, pk)),
                     [PR[pi][pk]], [rs])
                if tb % 2 == 1:
                    P.dma("sp", pFM[cc][:, (tb - 1) * 512:(tb + 1) * 512], st, reads=[rs], writes=[R_pFM[cc]])
        P.barrier()
        A.release(hT_mark)
        wtm = [A.alloc((NKC, 512), BF16) for _ in range(2)]
        R_wtm = [Res("wtm0"), Res("wtm1")]
        R_pTM = Res("pTM")
        stq = [A.alloc((512,)) for _ in range(4)]
        R_stq = [Res("stq%d" % i) for i in range(4)]
        scnt = 0
        for cb in range(9):
            ncol = 512 if cb < 8 else 16
            c0 = 3072 + cb * 512
            wt = wtm[cb % 2]
            rw = R_wtm[cb % 2]
            P.dma("pool", wt[:, :, 0:ncol], w_in_v[:, :, c0:c0 + ncol], writes=[rw])
            for t in range(NT):
                pi, pk = (pcnt % 4) // 2 + 2, pcnt % 2
                pcnt += 1
                for kc in range(NKC):
                    P.op("pe", lambda e, kc=kc, wt=wt, t=t, pi=pi, pk=pk, ncol=ncol: e.matmul(
                        pbank(pi, pk)[:, 0:ncol], hT[:, kc, t * 128:(t + 1) * 128], wt[:, kc, 0:ncol],
                        start=(kc == 0), stop=(kc == NKC - 1)),
                        [rw, R_hT[t]], [PR[pi][pk]])
                sq_ = stq[scnt % 4]
                rq = R_stq[scnt % 4]
                scnt += 1
                P.op("act" if t % 2 == 0 else "dve", lambda e, sq_=sq_, pi=pi, pk=pk, ncol=ncol, t=t: (
                    e.copy(sq_[:, 0:ncol], pbank(pi, pk)[:, 0:ncol]) if t % 2 == 0 else
                    e.tensor_copy(sq_[:, 0:ncol], pbank(pi, pk)[:, 0:ncol])),
                    [PR[pi][pk]], [rq])
                P.dma("sp", pTM[t * 128:(t + 1) * 128, cb * 512:cb * 512 + ncol], sq_[:, 0:ncol], reads=[rq], writes=[R_pTM])
        P.barrier()
        A.release(const_mark)
        if dbg == "a1":
            P.dma("sp", out[0:128, 0:1024], hT[:, 0, 0:2048].bitcast(F32), final=True)
            P.run()
            return nc, dbg_out


        R_oscr = Res("oscr")
        dn_mark = A.mark()
        R_dt = Res("dn_tab")
        ab = A.alloc((NT, 16))
        for q4 in range(4):
            P.dma("sp", ab[:, q4 * 8:(q4 + 1) * 8, :], pTM[q4 * 1024:(q4 + 1) * 1024, 4096:4112].rearrange("(t p) c -> p t c", p=128),
                  reads=[R_pTM], writes=[R_dt])
        dtb = A.alloc((8,))
        alg = A.alloc((8,))
        P.dma("sp", dtb, dt_bias.partition_broadcast(128), writes=[R_dt])
        P.dma("sp", alg, a_log.partition_broadcast(128), writes=[R_dt])
        maskS = A.alloc((128,))
        maskT = A.alloc((128,))
        oh8 = A.alloc((8, 128))
        dnw = A.alloc((128,))
        mBD = A.alloc((128,))
        mBDT = A.alloc((128,))
        mCs = [A.alloc((128,)) for _ in range(4)]
        mCTs = [A.alloc((128,)) for _ in range(4)]
        P.dma("sp", mBD, c_mBD[0], writes=[R_dt])
        P.dma("sp", mBDT, c_mBD[1], writes=[R_dt])
        for bi in range(4):
            P.dma("sp", mCs[bi], c_mC[bi], writes=[R_dt])
            P.dma("sp", mCTs[bi], c_mC[4 + bi], writes=[R_dt])
        P.dma("sp", maskS, c_maskS[:, :], writes=[R_dt])
        P.dma("sp", maskT, c_maskT[:, :], writes=[R_dt])
        P.dma("sp", oh8, c_onehot8[:, :, :], writes=[R_dt])
        P.dma("sp", dnw, dn_norm_w.partition_broadcast(128), writes=[R_dt])
        xg = A.alloc((NT, 8))
        t1_ = A.alloc((NT, 8))
        t2_ = A.alloc((NT, 8))
        gg = A.alloc((NT, 8))
        beta = A.alloc((NT, 8))
        negb = A.alloc((NT, 8))
        gc_ = A.alloc((NT, 8))
        glb = A.alloc((NT, 8))
        egc = A.alloc((NT, 8))
        kds = A.alloc((NT, 8))
        ecd = A.alloc((NT, 8))
        bw = A.alloc((NT, 8))
        nA = A.alloc((8,))
        gcTt = [A.alloc((128,)) for _ in range(2)]
        R_gct = [Res("gct0"), Res("gct1")]
        for i_ in range(2):
            P.op("pool", lambda e, i_=i_: e.memset(gcTt[i_], 0.0), [], [R_gct[i_]])
        av = ab[:, :, 0:8]
        bv = ab[:, :, 8:16]
        D_ = lambda fn, rd=(), wr=(): P.op("dve", fn, list(rd) + [R_dt], list(wr) + [R_dt])
        A_ = lambda fn, rd=(), wr=(): P.op("act", fn, list(rd) + [R_dt], list(wr) + [R_dt])
        D_(lambda e: e.tensor_tensor(xg, av, bc(dtb.unsqueeze(1), (128, NT, 8)), ALU.add))
        A_(lambda e: e.activation(t1_, xg, AF.Abs))
        A_(lambda e: e.activation(t2_, t1_, AF.Exp, scale=-1.0))
        A_(lambda e: e.activation(t1_, t2_, AF.Ln, bias=ones_f[:, 0:1], scale=1.0))
        D_(lambda e: e.scalar_tensor_tensor(t2_, xg, 0.0, t1_, ALU.max, ALU.add))
        A_(lambda e: e.activation(nA, alg, AF.Exp))
        D_(lambda e: e.tensor_scalar(nA, nA, -1.0, None, ALU.mult))
        D_(lambda e: e.tensor_tensor(gg, t2_, bc(nA.unsqueeze(1), (128, NT, 8)), ALU.mult))
        A_(lambda e: e.activation(beta, bv, AF.Sigmoid))
        D_(lambda e: e.tensor_scalar(negb, beta, -1.0, None, ALU.mult))
        gflat = gg.rearrange("p t h -> p (t h)")
        P.op("pe", lambda e: e.matmul(psum[0][:, 0:256], utri, gflat, start=True, stop=True), [R_dt, R_const], [PR[0][0]])
        P.op("pe", lambda e: e.matmul(psum[0][:, 512:768], ones_f, gflat, start=True, stop=True), [R_dt, R_c2], [PR[0][1]])
        A_(lambda e: e.copy(gc_.rearrange("p t h -> p (t h)"), psum[0][:, 0:256]), [PR[0][0]])
        A_(lambda e: e.copy(glb.rearrange("p t h -> p (t h)"), psum[0][:, 512:768]), [PR[0][1]])
        A_(lambda e: e.activation(egc, gc_, AF.Exp))
        A_(lambda e: e.activation(ecd, glb, AF.Exp))
        D_(lambda e: e.tensor_tensor(t1_, glb, gc_, ALU.subtract))
        A_(lambda e: e.activation(kds, t1_, AF.Exp))
        D_(lambda e: e.tensor_tensor(bw, beta, egc, ALU.mult))
        p1_mark = A.mark()
        cwt = A.alloc((24, 4))
        P.dma("sp", cwt, conv_w[:, :, :], writes=[R_dt])
        raw = A.alloc((3, S + 8), BF16)
        rsq = A.alloc((2, S))
        sqb16 = A.alloc((S,), BF16)
        dg = A.alloc((2, 4, 128), BF16)
        cv = A.alloc((3, S))
        qkn = A.alloc((2, S), BF16)
        vb16 = A.alloc((S,), BF16)
        tmst = A.alloc((2, NT, 128), BF16)
        R_raw, R_cv, R_qkn, R_vb16, R_tmst, R_rsq, R_sqb = [Res() for _ in range(7)]
        R_dg = [Res("dg0"), Res("dg1")]
        R_dnq, R_dnk, R_ktm, R_vtm = Res(), Res(), Res(), Res()
        P.op("pool", lambda e: e.memset(raw[:, :, 0:8], 0.0), [], [R_raw])
        dgc = 0
        for h in range(8):
            for i in range(3):
                for hf in range(2):
                    P.dma("pool", raw[:, i, 8 + hf * 2048:8 + (hf + 1) * 2048], pFM[i * 8 + h][:, hf * 2048:(hf + 1) * 2048],
                          reads=[R_pFM[i * 8 + h]], writes=[R_raw])
            for i in range(3):
                cc = i * 8 + h
                d_ = dg[:, dgc % 2, :, :]
                rd_ = R_dg[dgc % 2]
                dgc += 1
                for jj in range(4):
                    P.op("dve", lambda e, d_=d_, cc=cc, jj=jj: e.tensor_scalar(d_[:, jj, :], ident_b, cwt[:, cc, jj:jj + 1], None, ALU.mult),
                         [R_c2, R_dt], [rd_])
                for blk in range(8):
                    pi, pk = (blk % 8) // 2, blk % 2
                    for jj in range(4):
                        P.op("pe", lambda e, d_=d_, i=i, jj=jj, blk=blk, pi=pi, pk=pk: e.matmul(
                            pbank(pi, pk), d_[:, jj, :], raw[:, i, 5 + jj + blk * 512:5 + jj + (blk + 1) * 512], start=(jj == 0), stop=(jj == 3)),
                            [rd_, R_raw], [PR[pi][pk]])
                    P.op("act", lambda e, i=i, blk=blk, pi=pi, pk=pk: e.activation(cv[:, i, blk * 512:(blk + 1) * 512], pbank(pi, pk), AF.Silu),
                         [PR[pi][pk]], [R_cv])
            for i in range(2):
                rsv = rsq[:, i, :]
                P.op("act", lambda e, i=i: e.activation(sqb16, cv[:, i, :], AF.Square), [R_cv], [R_sqb])
                for blk in range(8):
                    pi, pk = blk // 2, blk % 2
                    P.op("pe", lambda e, pi=pi, pk=pk, blk=blk: e.matmul(pbank(pi, pk), ones_b, sqb16[:, blk * 512:(blk + 1) * 512], start=True, stop=True),
                         [R_sqb, R_c2], [PR[pi][pk]])
                for blk in range(8):
                    pi, pk = blk // 2, blk % 2
                    P.op("act", lambda e, pi=pi, pk=pk, blk=blk, rsv=rsv: e.activation(rsv[:, blk * 512:(blk + 1) * 512], pbank(pi, pk), AF.Sqrt,
                                                                                        bias=epsc[:, 0:1], scale=1.0), [PR[pi][pk], R_c2], [R_rsq])
                P.op("dve", lambda e, rsv=rsv: e.reciprocal(rsv, rsv), [], [R_rsq])
                scl = (128.0 ** -0.5) if i == 0 else 1.0
                P.op("dve", lambda e, i=i, scl=scl, rsv=rsv: e.scalar_tensor_tensor(qkn[:, i, :], cv[:, i, :], scl, rsv, ALU.mult, ALU.mult),
                     [R_cv, R_rsq], [R_qkn])
            P.op("pool", lambda e: e.tensor_copy(vb16, cv[:, 2, :]), [R_cv], [R_vb16])
            P.dma("sp", dn_qT[h], qkn[:, 0, :], reads=[R_qkn], writes=[R_dnq])
            P.dma("sp", dn_kT[h], qkn[:, 1, :], reads=[R_qkn], writes=[R_dnk])
            for which, src, rsrc in ((0, qkn[:, 1, :], R_qkn), (1, vb16, R_vb16)):
                for g4 in range(4):
                    pi, pk = g4 % 2, g4 // 2
                    pv = psum[pi][:, pk * 512:(pk + 1) * 512].bitcast(BF16)
                    for q in range(8):
                        t = g4 * 8 + q
                        P.op("pe", lambda e, pv=pv, q=q, t=t, src=src: e.transpose(pv[:, q * 128:(q + 1) * 128], src[:, t * 128:(t + 1) * 128], ident_b),
                             [rsrc, R_c2], [PR[pi][pk]])
                    P.op("act" if g4 % 2 == 0 else "dve", lambda e, pv=pv, g4=g4, which=which: (
                        e.copy if g4 % 2 == 0 else e.tensor_copy)(tmst[:, which, g4 * 8:(g4 + 1) * 8, :], pv.rearrange("p (t d) -> p t d", t=8)),
                        [PR[pi][pk]], [R_tmst])
            for q4 in range(4):
                P.dma("sp", dn_ktm[q4 * 1024:(q4 + 1) * 1024].rearrange("(t p) h d -> p t h d", p=128)[:, :, h, :],
                      tmst[:, 0, q4 * 8:(q4 + 1) * 8, :], reads=[R_tmst], writes=[R_ktm])
                P.dma("sp", dn_vtm[q4 * 1024:(q4 + 1) * 1024].rearrange("(t p) h d -> p t h d", p=128)[:, :, h, :],
                      tmst[:, 1, q4 * 8:(q4 + 1) * 8, :], reads=[R_tmst], writes=[R_vtm])
        P.barrier()
        A.release(p1_mark)

        theta_bc = A.alloc((32,))
        posi = A.alloc((NT,), I32)
        posf = A.alloc((NT,))
        sinT = A.alloc((NT, 32))
        cosT = A.alloc((NT, 32))
        rt_tmp_mark = A.mark()
        ang = A.alloc((NT, 32))
        tmpa = A.alloc((NT, 32))
        R_tab = Res("rt_tab")
        P.dma("sp", theta_bc, c_theta.partition_broadcast(128), writes=[R_tab])
        P.dma("sp", posi, pos[:, :], writes=[R_tab])
        P.op("dve", lambda e: e.tensor_copy(posf, posi), [R_tab], [R_tab])
        P.op("dve", lambda e: e.tensor_tensor(ang, bc(posf.unsqueeze(2), (128, NT, 32)),
                                              bc(theta_bc.unsqueeze(1), (128, NT, 32)), ALU.mult), [R_tab], [R_tab])
        TWO_PI = 2.0 * np.pi
        pic = A.alloc((8,))
        P.op("pool", lambda e: e.memset(pic, -np.pi), [], [R_tab])
        ki = A.alloc((NT, 32), I32)
        kf = A.alloc((NT, 32))

        def sin_table(dst, shift):
            P.op("dve", lambda e: e.tensor_scalar(tmpa, ang, shift, 1.0 / TWO_PI, ALU.add, ALU.mult), [R_tab], [R_tab])
            P.op("dve", lambda e: e.tensor_copy(ki, tmpa), [R_tab], [R_tab])
            P.op("dve", lambda e: e.tensor_copy(kf, ki), [R_tab], [R_tab])
            P.op("dve", lambda e: e.tensor_scalar(tmpa, ang, shift, None, ALU.add), [R_tab], [R_tab])
            P.op("dve", lambda e: e.scalar_tensor_tensor(tmpa, kf, -TWO_PI, tmpa, ALU.mult, ALU.add), [R_tab], [R_tab])
            P.op("dve", lambda e: e.tensor_scalar(kf, tmpa, np.pi, TWO_PI, ALU.is_gt, ALU.mult), [R_tab], [R_tab])
            P.op("dve", lambda e: e.tensor_tensor(tmpa, tmpa, kf, ALU.subtract), [R_tab], [R_tab])
            P.op("dve", lambda e: e.tensor_scalar(kf, tmpa, -np.pi, TWO_PI, ALU.is_lt, ALU.mult), [R_tab], [R_tab])
            P.op("dve", lambda e: e.tensor_tensor(tmpa, tmpa, kf, ALU.add), [R_tab], [R_tab])
            P.op("act", lambda e: e.activation(dst, tmpa, AF.Sin), [R_tab], [R_tab])

        sin_table(sinT, 0.0)
        sin_table(cosT, 0.5 * np.pi)
        P.barrier()
        A.release(rt_tmp_mark)
        rtmask = A.alloc((8, 128))
        gq = A.alloc((8, 128), parts=64)
        gk = A.alloc((8,))
        gC = A.alloc((8,), parts=64)
        rtw = A.alloc((1024,))
        P.dma("sp", rtmask, c_rtmask[:, :, :], writes=[R_tab])
        P.dma("sp", gq, c_gq[:, :, :], writes=[R_tab])
        P.dma("sp", gk, c_gk[:, :], writes=[R_tab])
        P.dma("sp", gC, c_gC[:, :], writes=[R_tab])
        P.dma("sp", rtw, rt_norm_w.partition_broadcast(128), writes=[R_tab])
        Sst = A.alloc((8, 128), parts=64)
        Sbf = A.alloc((8, 128), BF16, parts=64)
        R_S = Res("S")
        R_Sbf = Res("Sbf")
        P.op("pool", lambda e: e.memset(Sst, 0.0), [], [R_S])
        P.op("pool", lambda e: e.memset(Sbf, 0.0), [], [R_Sbf])
        ld = [A.alloc((3072,), BF16) for _ in range(2)]
        R_ld = [Res("ld0"), Res("ld1")]
        qr = A.alloc((8, 64), BF16)
        kr = A.alloc((8, 64), BF16)
        kd = A.alloc((8, 64), BF16)
        sgt = A.alloc((1024,))
        ta = A.alloc((8, 32))
        tb_ = A.alloc((8, 32))
        qT = A.alloc((8, 128), BF16, parts=64)
        qdT = A.alloc((8, 128), BF16, parts=64)
        kT = A.alloc((8, 128), BF16, parts=64)
        PT = A.alloc((8, 128), BF16)
        osb = A.alloc((8, 128))
        sqb = A.alloc((8, 128))
        st8 = A.alloc((6, 8))
        R_qr, R_kr, R_kd, R_vb, R_sg, R_ta, R_tb, R_qT, R_qdT, R_kT, R_PT, R_osb, R_sqb, R_st8 = [Res() for _ in range(14)]

        def rotary(src, dst, rdst, t, rl):
            x1 = src[:, :, 0:32]
            x2 = src[:, :, 32:64]
            cs = bc(cosT[:, t, :].unsqueeze(1), (128, 8, 32))
            sn = bc(sinT[:, t, :].unsqueeze(1), (128, 8, 32))
            P.op("dve", lambda e: e.tensor_tensor(ta, x1, cs, ALU.mult), [rl, R_tab], [R_ta])
            P.op("pool", lambda e: e.tensor_tensor(tb_, x2, sn, ALU.mult), [rl, R_tab], [R_tb])
            P.op("dve", lambda e: e.tensor_tensor(dst[:, :, 0:32], ta, tb_, ALU.subtract), [R_ta, R_tb], [rdst])
            P.op("dve", lambda e: e.tensor_tensor(ta, x2, cs, ALU.mult), [rl, R_tab], [R_ta])
            P.op("pool", lambda e: e.tensor_tensor(tb_, x1, sn, ALU.mult), [rl, R_tab], [R_tb])
            P.op("dve", lambda e: e.tensor_tensor(dst[:, :, 32:64], ta, tb_, ALU.add), [R_ta, R_tb], [rdst])


        NG = DN_GROUPS
        HG = 8 // NG
        qTt = [A.alloc((8, 128), BF16) for _ in range(2)]
        kTt = [A.alloc((8, 128), BF16) for _ in range(2)]
        ktm = [A.alloc((8, 128), BF16) for _ in range(2)]
        vtm = [A.alloc((8, 128), BF16) for _ in range(2)]
        zt = [A.alloc((1024,), BF16) for _ in range(2)]
        R_in = [Res("dnin0"), Res("dnin1")]
        bf_names = ("Bm", "attnT", "qgT", "Xw", "Xu", "kdec", "Pa", "Pb", "Ma", "Mb", "Mta", "Mtb", "WT", "vnew",
                    "Da", "Db", "Bd", "Bdt", "Cb", "Cbt", "G", "G2", "Mtc")
        f_names = ("dd", "dS_", "GS", "GT", "KKg", "egb", "U")
        T_ = {}
        RR = [dict() for _ in range(NG)]
        for nme in bf_names + f_names:
            T_[nme] = A.alloc((8, 128), BF16 if nme in bf_names else F32)
            for g in range(NG):
                RR[g][nme] = Res(nme + str(g))
        for nme, al in (("osb2", "dd"), ("sq2", "dS_"), ("szl", "GS")):
            T_[nme] = T_[al]
            for g in range(NG):
                RR[g][nme] = RR[g][al]
        FB = ("Bm", "attnT", "qgT", "Xw", "Xu", "kdec")
        T2 = {nme: [T_[nme], A.alloc((8, 128), BF16)] for nme in FB}
        RR2 = [[{nme: (RR[g][nme] if sl_ == 0 else Res(nme + "b" + str(g))) for nme in FB} for sl_ in range(2)] for g in range(NG)]
        R_z = [Res("z0"), Res("z1")]
        st9 = A.alloc((4, 8))
        R_st9 = [Res("st9_%d" % g) for g in range(NG)]
        Sd = A.alloc((8, 128))
        Sdb = A.alloc((8, 128), BF16)
        R_Sd = [Res("Sd%d" % g) for g in range(NG)]
        R_Sdb = [Res("Sdb%d" % g) for g in range(NG)]
        P.op("pool", lambda e: e.memset(Sd, 0.0), [], R_Sd)
        P.op("pool", lambda e: e.memset(Sdb, 0.0), [], R_Sdb)
        bkc = [0]

        def nbk():
            n = bkc[0] % 8
            bkc[0] += 1
            return n

        def pb_(n):
            return psum[n // 2][:, (n % 2) * 512:(n % 2 + 1) * 512]

        def prb(n):
            return PR[n // 2][n % 2]


        def rt_loads(t):
            for hf in range(2):
                P.dma("pool", ld[t % 2][:, hf * 1536:(hf + 1) * 1536], pTM[t * 128:(t + 1) * 128, 1024 + hf * 1536:1024 + (hf + 1) * 1536],
                      reads=[R_pTM], writes=[R_ld[t % 2]])

        def rt_gen(t):
            l_ = ld[t % 2]
            rl = R_ld[t % 2]
            qv = l_[:, 0:512].rearrange("p (h d) -> p h d", h=8)
            kv = l_[:, 512:1024].rearrange("p (h d) -> p h d", h=8)
            vv = l_[:, 1024:2048].rearrange("p (h d) -> p h d", h=8)
            gv = l_[:, 2048:3072]
            rotary(qv, qr, R_qr, t, rl)
            rotary(kv, kr, R_kr, t, rl)
            P.op("pool", lambda e: e.tensor_tensor(kd, kr, bc(gk.unsqueeze(2), (128, 8, 64)), ALU.mult), [R_kr, R_tab], [R_kd])
            P.op("act", lambda e: e.activation(sgt, gv, AF.Silu), [rl], [R_sg])
            yield
            nq, nk = nbk(), nbk()
            pq = pb_(nq).bitcast(BF16)
            pk_ = pb_(nk).bitcast(BF16)
            for h in range(8):
                P.op("pe", lambda e, h=h: e.transpose(pq[0:64, h * 128:(h + 1) * 128], qr[:, h, :], ident_b), [R_qr, R_c2], [prb(nq)])
            for h in range(8):
                P.op("pe", lambda e, h=h: e.transpose(pk_[0:64, h * 128:(h + 1) * 128], kr[:, h, :], ident_b), [R_kr, R_c2], [prb(nk)])
            pq3 = pq[0:64, :].rearrange("p (h n) -> p h n", h=8)
            pk3 = pk_[0:64, :].rearrange("p (h n) -> p h n", h=8)
            P.op("act", lambda e: e.copy(qT, pq3), [prb(nq)], [R_qT])
            P.op("dve", lambda e: e.tensor_tensor(qdT, pq3, gq, ALU.mult), [prb(nq), R_tab], [R_qdT])
            P.op("act", lambda e: e.copy(kT, pk3), [prb(nk)], [R_kT])
            yield
            ns = [nbk(), nbk()]
            for h in range(8):
                P.op("pe", lambda e, h=h: e.matmul(pb_(ns[h // 4])[:, (h % 4) * 128:(h % 4 + 1) * 128], kT[:, h, :], qT[:, h, :], start=True, stop=True),
                     [R_kT, R_qT], [prb(ns[h // 4])])
            for k in range(2):
                P.op("dve", lambda e, k=k: e.tensor_tensor(
                    PT[:, k * 4:(k + 1) * 4, :], pb_(ns[k]).rearrange("p (h n) -> p h n", h=4),
                    rtmask[:, k * 4:(k + 1) * 4, :], ALU.mult), [prb(ns[k]), R_tab], [R_PT])
            yield
            no = [nbk(), nbk()]
            nd = [nbk(), nbk()]
            for h in range(8):
                P.op("pe", lambda e, h=h: e.matmul(pb_(no[h // 4])[:, (h % 4) * 128:(h % 4 + 1) * 128], PT[:, h, :], vv[:, h, :], start=True, stop=False),
                     [R_PT, rl], [prb(no[h // 4])])
                P.op("pe", lambda e, h=h: e.matmul(pb_(no[h // 4])[:, (h % 4) * 128:(h % 4 + 1) * 128], qdT[:, h, :], Sbf[:, h, :], start=False, stop=True),
                     [R_qdT, R_Sbf], [prb(no[h // 4])])
            for h in range(8):
                P.op("pe", lambda e, h=h: e.matmul(pb_(nd[h // 4])[0:64, (h % 4) * 128:(h % 4 + 1) * 128], kd[:, h, :], vv[:, h, :], start=True, stop=True),
                     [R_kd, rl], [prb(nd[h // 4])])
            for k in range(2):
                P.op("act", lambda e, k=k: e.copy(osb[:, k * 4:(k + 1) * 4, :], pb_(no[k]).rearrange("p (h n) -> p h n", h=4)),
                     [prb(no[k])], [R_osb])
            P.op("dve", lambda e: e.tensor_tensor(Sst, Sst, bc(gC.unsqueeze(2), (64, 8, 128)), ALU.mult), [R_tab], [R_S])
            for k in range(2):
                P.op("dve", lambda e, k=k: e.tensor_tensor(
                    Sst[:, k * 4:(k + 1) * 4, :], Sst[:, k * 4:(k + 1) * 4, :],
                    pb_(nd[k])[0:64, :].rearrange("p (h n) -> p h n", h=4), ALU.add), [prb(nd[k])], [R_S])
            P.op("act", lambda e: e.copy(Sbf, Sst), [R_S], [R_Sbf])
            yield
            P.op("dve", lambda e: e.tensor_reduce(st8[:, 0, :], osb, AX.X, ALU.add), [R_osb], [R_st8])
            P.op("pool", lambda e: e.tensor_tensor(sqb, osb, osb, ALU.mult), [R_osb], [R_sqb])
            P.op("dve", lambda e: e.tensor_reduce(st8[:, 1, :], sqb, AX.X, ALU.add), [R_sqb], [R_st8])
            P.op("dve", lambda e: e.tensor_scalar(st8[:, 2, :], st8[:, 0, :], 1.0 / 128, None, ALU.mult), [R_st8], [R_st8])
            P.op("dve", lambda e: e.tensor_tensor(st8[:, 3, :], st8[:, 2, :], st8[:, 2, :], ALU.mult), [R_st8], [R_st8])
            P.op("dve", lambda e: e.scalar_tensor_tensor(st8[:, 4, :], st8[:, 1, :], 1.0 / 128, st8[:, 3, :], ALU.mult, ALU.subtract),
                 [R_st8], [R_st8])
            P.op("act", lambda e: e.activation(st8[:, 5, :], st8[:, 4, :], AF.Sqrt, bias=epsc[:, 0:1], scale=1.0), [R_st8, R_c2], [R_st8])
            P.op("dve", lambda e: e.reciprocal(st8[:, 4, :], st8[:, 5, :]), [R_st8], [R_st8])
            yield
            P.op("dve", lambda e: e.tensor_tensor(osb, osb, bc(st8[:, 2, :].unsqueeze(2), (128, 8, 128)), ALU.subtract), [R_st8], [R_osb])
            P.op("dve", lambda e: e.tensor_tensor(osb, osb, bc(st8[:, 4, :].unsqueeze(2), (128, 8, 128)), ALU.mult), [R_st8], [R_osb])
            osf = osb.rearrange("p h n -> p (h n)")
            P.op("pool", lambda e: e.tensor_tensor(osf, osf, rtw, ALU.mult), [R_tab], [R_osb])
            P.op("pool", lambda e: e.tensor_tensor(sqb.rearrange("p h n -> p (h n)"), osf, sgt, ALU.mult), [R_osb, R_sg], [R_sqb])
            P.dma("sp", o_scr[t * 128:(t + 1) * 128, 1024:2048], sqb.rearrange("p h n -> p (h n)"), reads=[R_sqb], writes=[R_oscr])

        def gct_prep(t):
            nb = nbk()
            P.op("pe", lambda e: e.matmul(pb_(nb)[0:8, 0:128], gg[:, t, :], utri, start=True, stop=True), [R_dt, R_const], [prb(nb)])
            P.op("act", lambda e: e.copy(gcTt[t % 2][0:8, :], pb_(nb)[0:8, 0:128]), [prb(nb)], [R_gct[t % 2]])

        def dn_loads(t):
            b_ = t % 2
            rin = R_in[b_]
            tsl = slice(t * 128, (t + 1) * 128)
            P.dma("sp", qTt[b_], dn_qT[:, :, tsl].rearrange("h d n -> d h n"), reads=[R_dnq], writes=[rin])
            P.dma("sp", kTt[b_], dn_kT[:, :, tsl].rearrange("h d n -> d h n"), reads=[R_dnk], writes=[rin])
            P.dma("sp", ktm[b_], dn_ktm[tsl], reads=[R_ktm], writes=[rin])
            P.dma("sp", vtm[b_], dn_vtm[tsl], reads=[R_vtm], writes=[rin])

        def z_loads(t):
            P.dma("pool", zt[t % 2], pTM[t * 128:(t + 1) * 128, 0:1024], reads=[R_pTM], writes=[R_z[t % 2]])

        def dn_gen(t, g, part):
            b_ = t % 2
            rin = R_in[b_]
            rz = R_z[b_]
            tsl = slice(t * 128, (t + 1) * 128)
            hsl = slice(g * HG, (g + 1) * HG)
            Tl = dict(T_)
            rr = dict(RR[g])
            for nme in FB:
                Tl[nme] = T2[nme][b_]
                rr[nme] = RR2[g][b_][nme]
            q_, k_, km_, vm_ = qTt[b_], kTt[b_], ktm[b_], vtm[b_]
            Tg = lambda nme: Tl[nme][:, hsl, :]
            pvh = lambda n: pb_(n)[:, 0:HG * 128].rearrange("p (h n) -> p h n", h=HG)
            bch = lambda ap2: bc(ap2[:, hsl].unsqueeze(2), (128, HG, 128))
            bcm = lambda m: bc(m.unsqueeze(1), (128, HG, 128))
            I8 = bc(ident_b.unsqueeze(1), (128, HG, 128))

            def mmg(n, lhs, rhs, reads):
                for hl in range(HG):
                    h = g * HG + hl
                    l_ap, r_ap = lhs(h), rhs(h)
                    P.op("pe", lambda e, hl=hl, l_ap=l_ap, r_ap=r_ap: e.matmul(pb_(n)[:, hl * 128:(hl + 1) * 128], l_ap, r_ap, start=True, stop=True),
                         reads, [prb(n)])

            def ew(eng, fn, reads, writes):
                P.op(eng, fn, reads, writes)

            def dn_back():
                nTr = nbk()
                ptv = pb_(nTr).bitcast(BF16)
                for hl in range(HG):
                    h = g * HG + hl
                    P.op("pe", lambda e, hl=hl, h=h: e.transpose(ptv[:, hl * 128:(hl + 1) * 128], Tl["Bm"][:, h, :], ident_b), [rr["Bm"], R_c2], [prb(nTr)])
                ew("act", lambda e: e.copy(Tg("Mta"), ptv[:, 0:HG * 128].rearrange("p (h n) -> p h n", h=HG)), [prb(nTr)], [rr["Mta"]])
                ew("dve", lambda e: e.tensor_tensor(Tg("Bd"), Tg("Bm"), bcm(mBD), ALU.mult), [rr["Bm"], R_dt], [rr["Bd"]])
                ew("pool", lambda e: e.tensor_tensor(Tg("Bdt"), Tg("Mta"), bcm(mBDT), ALU.mult), [rr["Mta"], R_dt], [rr["Bdt"]])
                ew("dve", lambda e: e.tensor_tensor(Tg("Pa"), Tg("Bdt"), I8, ALU.add), [rr["Bdt"], R_c2], [rr["Pa"]])
                ew("pool", lambda e: e.tensor_tensor(Tg("Da"), Tg("Bd"), I8, ALU.add), [rr["Bd"], R_c2], [rr["Da"]])
                yield
                Mc, Mtc, Ec, Dc = "Bd", "Bdt", "Pa", "Da"
                for lev in range(2):
                    Mn = "Ma" if Mc != "Ma" else "Mb"
                    Mtn = "Mtb" if Mtc != "Mtb" else "Mtc"
                    En = "Pb" if Ec != "Pb" else "Pa"
                    Dn = "Db" if Dc != "Db" else "Da"
                    nM, nMt = nbk(), nbk()
                    mmg(nM, lambda h, Mtc=Mtc: Tl[Mtc][:, h, :], lambda h, Mc=Mc: Tl[Mc][:, h, :], [rr[Mtc], rr[Mc]])
                    mmg(nMt, lambda h, Mc=Mc: Tl[Mc][:, h, :], lambda h, Mtc=Mtc: Tl[Mtc][:, h, :], [rr[Mtc], rr[Mc]])
                    ew("act", lambda e, Mn=Mn, nM=nM: e.copy(Tg(Mn), pvh(nM)), [prb(nM)], [rr[Mn]])
                    ew("dve", lambda e, Mtn=Mtn, nMt=nMt: e.tensor_copy(Tg(Mtn), pvh(nMt)), [prb(nMt)], [rr[Mtn]])
                    yield
                    nE, nD = nbk(), nbk()
                    mmg(nE, lambda h, Mn=Mn: Tl[Mn][:, h, :], lambda h, Ec=Ec: Tl[Ec][:, h, :], [rr[Mn], rr[Ec]])
                    mmg(nD, lambda h, Mtn=Mtn: Tl[Mtn][:, h, :], lambda h, Dc=Dc: Tl[Dc][:, h, :], [rr[Mtn], rr[Dc]])
                    ew("dve", lambda e, En=En, Ec=Ec, nE=nE: e.tensor_tensor(Tg(En), pvh(nE), Tg(Ec), ALU.add), [prb(nE), rr[Ec]], [rr[En]])
                    ew("dve", lambda e, Dn=Dn, Dc=Dc, nD=nD: e.tensor_tensor(Tg(Dn), pvh(nD), Tg(Dc), ALU.add), [prb(nD), rr[Dc]], [rr[Dn]])
                    yield
                    Mc, Mtc, Ec, Dc = Mn, Mtn, En, Dn
                for bi in range(4):
                    En = "Pb" if Ec != "Pb" else "Pa"
                    Dn = "Db" if Dc != "Db" else "Da"
                    ew("dve", lambda e, bi=bi: e.tensor_tensor(Tg("Cb"), Tg("Bm"), bcm(mCs[bi]), ALU.mult), [rr["Bm"], R_dt], [rr["Cb"]])
                    nG = nbk()
                    mmg(nG, lambda h: Tl["Cb"][:, h, :], lambda h, Ec=Ec: Tl[Ec][:, h, :], [rr["Cb"], rr[Ec]])
                    ew("act", lambda e, nG=nG: e.copy(Tg("G"), pvh(nG)), [prb(nG)], [rr["G"]])
                    if bi < 3:
                        ew("pool", lambda e, bi=bi: e.tensor_tensor(Tg("Cbt"), Tg("Mta"), bcm(mCTs[bi]), ALU.mult), [rr["Mta"], R_dt], [rr["Cbt"]])
                        nG2 = nbk()
                        mmg(nG2, lambda h: Tl["Cbt"][:, h, :], lambda h, Dc=Dc: Tl[Dc][:, h, :], [rr["Cbt"], rr[Dc]])
                        ew("act", lambda e, nG2=nG2: e.copy(Tg("G2"), pvh(nG2)), [prb(nG2)], [rr["G2"]])
                    yield
                    nH = nbk()
                    mmg(nH, lambda h, Dc=Dc: Tl[Dc][:, h, :], lambda h: Tl["G"][:, h, :], [rr[Dc], rr["G"]])
                    ew("dve", lambda e, En=En, Ec=Ec, nH=nH: e.tensor_tensor(Tg(En), pvh(nH), Tg(Ec), ALU.add), [prb(nH), rr[Ec]], [rr[En]])
                    if bi < 3:
                        nH2 = nbk()
                        mmg(nH2, lambda h, Ec=Ec: Tl[Ec][:, h, :], lambda h: Tl["G2"][:, h, :], [rr[Ec], rr["G2"]])
                        ew("dve", lambda e, Dn=Dn, Dc=Dc, nH2=nH2: e.tensor_tensor(Tg(Dn), pvh(nH2), Tg(Dc), ALU.add), [prb(nH2), rr[Dc]], [rr[Dn]])
                        Dc = Dn
                    Ec = En
                    yield
                Pc = Ec
                nW, nU = nbk(), nbk()
                mmg(nW, lambda h: Tl["Xw"][:, h, :], lambda h: Tl[Pc][:, h, :], [rr["Xw"], rr[Pc]])
                mmg(nU, lambda h: Tl[Pc][:, h, :], lambda h: Tl["Xu"][:, h, :], [rr["Xu"], rr[Pc]])
                ew("act", lambda e: e.copy(Tg("WT"), pvh(nW)), [prb(nW)], [rr["WT"]])
                ew("act", lambda e: e.copy(Tg("U"), pvh(nU)), [prb(nU)], [rr["U"]])
                yield
                nWS = nbk()
                mmg(nWS, lambda h: Tl["WT"][:, h, :], lambda h: Sdb[:, h, :], [rr["WT"], R_Sdb[g]])
                ew("dve", lambda e: e.tensor_tensor(Tg("vnew"), Tg("U"), pvh(nWS), ALU.subtract), [prb(nWS), rr["U"]], [rr["vnew"]])
                yield
                nO, nDS = nbk(), nbk()
                for hl in range(HG):
                    h = g * HG + hl
                    P.op("pe", lambda e, hl=hl, h=h: e.matmul(pb_(nO)[:, hl * 128:(hl + 1) * 128], Tl["attnT"][:, h, :], Tl["vnew"][:, h, :], start=True, stop=False),
                         [rr["attnT"], rr["vnew"]], [prb(nO)])
                    P.op("pe", lambda e, hl=hl, h=h: e.matmul(pb_(nO)[:, hl * 128:(hl + 1) * 128], Tl["qgT"][:, h, :], Sdb[:, h, :], start=False, stop=True),
                         [rr["qgT"], R_Sdb[g]], [prb(nO)])
                mmg(nDS, lambda h: Tl["kdec"][:, h, :], lambda h: Tl["vnew"][:, h, :], [rr["kdec"], rr["vnew"]])
                ew("dve", lambda e: e.tensor_tensor(Sd[:, hsl, :], Sd[:, hsl, :], bch(ecd[:, t, :]), ALU.mult), [R_dt], [R_Sd[g]])
                ew("dve", lambda e: e.tensor_tensor(Sd[:, hsl, :], Sd[:, hsl, :], pvh(nDS), ALU.add), [prb(nDS)], [R_Sd[g]])
                ew("act", lambda e: e.copy(Sdb[:, hsl, :], Sd[:, hsl, :]), [R_Sd[g]], [R_Sdb[g]])
                ew("act", lambda e: e.copy(Tg("osb2"), pvh(nO)), [prb(nO)], [rr["osb2"]])
                yield
                ew("pool", lambda e: e.tensor_tensor(Tg("sq2"), Tg("osb2"), Tg("osb2"), ALU.mult), [rr["osb2"]], [rr["sq2"]])
                ew("dve", lambda e: e.tensor_reduce(st9[:, 0, hsl], Tg("sq2"), AX.X, ALU.add), [rr["sq2"]], [R_st9[g]])
                ew("act", lambda e: e.activation(st9[:, 1, hsl], st9[:, 0, hsl], AF.Sqrt, bias=epsc[:, 0:1], scale=1.0 / 128), [R_c2], [R_st9[g]])
                ew("dve", lambda e: e.reciprocal(st9[:, 2, hsl], st9[:, 1, hsl]), [], [R_st9[g]])
                ew("dve", lambda e: e.tensor_tensor(Tg("osb2"), Tg("osb2"), bch(st9[:, 2, :]), ALU.mult), [R_st9[g]], [rr["osb2"]])
                ew("pool", lambda e: e.tensor_tensor(Tg("osb2"), Tg("osb2"), bcm(dnw), ALU.mult), [R_dt], [rr["osb2"]])
                zsl = slice(g * HG * 128, (g + 1) * HG * 128)
                ew("act", lambda e: e.activation(Tg("szl").rearrange("p h n -> p (h n)"), zt[b_][:, zsl], AF.Silu), [rz], [rr["szl"]])
                ew("pool", lambda e: e.tensor_tensor(Tg("sq2"), Tg("osb2"), Tg("szl"), ALU.mult), [rr["osb2"], rr["szl"]], [rr["sq2"]])
                P.dma("sp", o_scr[tsl, zsl], Tg("sq2"), reads=[rr["sq2"]], writes=[R_oscr])


            if part == "back":
                yield from dn_back()
                return
            nKK, nQK, nBC = nbk(), nbk(), nbk()
            mmg(nKK, lambda h: k_[:, h, :], lambda h: k_[:, h, :], [rin])
            mmg(nQK, lambda h: k_[:, h, :], lambda h: q_[:, h, :], [rin])
            mmg(nBC, lambda h: oh8[:, h, :], lambda h: gcTt[b_][:, :], [R_dt, R_gct[b_]])
            ew("dve", lambda e: e.tensor_tensor(Tg("dd"), pvh(nBC), bch(gc_[:, t, :]), ALU.subtract), [prb(nBC), R_dt], [rr["dd"]])
            ew("act", lambda e: e.activation(Tg("egb"), pvh(nBC), AF.Exp), [prb(nBC)], [rr["egb"]])
            ew("dve", lambda e: e.tensor_scalar(Tg("dS_"), Tg("dd"), 0.0, None, ALU.max), [rr["dd"]], [rr["dS_"]])
            ew("act", lambda e: e.activation(Tg("GS"), Tg("dS_"), AF.Exp, scale=-1.0), [rr["dS_"]], [rr["GS"]])
            ew("dve", lambda e: e.tensor_scalar(Tg("dS_"), Tg("dd"), 0.0, None, ALU.min), [rr["dd"], rr["GS"]], [rr["dS_"]])
            ew("act", lambda e: e.activation(Tg("GT"), Tg("dS_"), AF.Exp), [rr["dS_"]], [rr["GT"]])
            ew("pool", lambda e: e.tensor_tensor(Tg("GS"), Tg("GS"), bcm(maskS), ALU.mult), [R_dt], [rr["GS"]])
            ew("pool", lambda e: e.tensor_tensor(Tg("GT"), Tg("GT"), bcm(maskT), ALU.mult), [R_dt], [rr["GT"]])
            ew("dve", lambda e: e.tensor_tensor(Tg("KKg"), pvh(nKK), Tg("GS"), ALU.mult), [prb(nKK), rr["GS"]], [rr["KKg"]])
            ew("pool", lambda e: e.tensor_tensor(Tg("Bm"), Tg("KKg"), bch(negb[:, t, :]), ALU.mult), [rr["KKg"], R_dt], [rr["Bm"]])
            ew("dve", lambda e: e.tensor_tensor(Tg("attnT"), pvh(nQK), Tg("GT"), ALU.mult), [prb(nQK), rr["GT"]], [rr["attnT"]])
            ew("pool", lambda e: e.tensor_tensor(Tg("qgT"), q_[:, hsl, :], Tg("egb"), ALU.mult), [rin, rr["egb"]], [rr["qgT"]])
            ew("pool", lambda e: e.tensor_tensor(Tg("Xw"), km_[:, hsl, :], bch(bw[:, t, :]), ALU.mult), [rin, R_dt], [rr["Xw"]])
            ew("pool", lambda e: e.tensor_tensor(Tg("Xu"), vm_[:, hsl, :], bch(beta[:, t, :]), ALU.mult), [rin, R_dt], [rr["Xu"]])
            ew("pool", lambda e: e.tensor_tensor(Tg("kdec"), km_[:, hsl, :], bch(kds[:, t, :]), ALU.mult), [rin, R_dt], [rr["kdec"]])
        def run_gens(gens):
            while gens:
                for gg_ in list(gens):
                    try:
                        next(gg_)
                    except StopIteration:
                        gens.remove(gg_)

        dn_loads(0)
        dn_loads(1)
        z_loads(0)
        rt_loads(0)
        gct_prep(0)
        gct_prep(1)
        run_gens([dn_gen(0, g, "front") for g in range(NG)])
        for t in range(NT):
            if t + 2 < NT:
                dn_loads(t + 2)
                gct_prep(t + 2)
            if t + 1 < NT:
                z_loads(t + 1)
                rt_loads(t + 1)
            gens = [dn_gen(t, g, "back") for g in range(NG)]
            if t + 1 < NT:
                gens += [dn_gen(t + 1, g, "front") for g in range(NG)]
            gens.append(rt_gen(t))
            run_gens(gens)
        P.barrier()
        A.release(dn_mark)
        if dbg == "dn":
            P.dma("sp", out[0:128, 0:128], dnw, final=True)
            P.run()
            return nc, dbg_out


        b_mark = A.mark()
        R_bc = Res("b_const")
        idxt = A.alloc((8,), I32)
        P.dma("sp", idxt, own_idx[:, :], writes=[R_bc])
        wr = A.alloc((NKC, 64))
        P.dma("sp", wr, w_router.rearrange("(c p) n -> p c n", p=128), writes=[R_bc])
        rb = A.alloc((64,))
        P.dma("sp", rb, router_bias.partition_broadcast(128), writes=[R_bc])
        Wc = A.alloc((8, 65))
        R_Wc = Res("Wc")
        P.op("pool", lambda e: e.memset(Wc, 1.0), [], [R_Wc])
        h2T = A.alloc((NKC, OWN), BF16)
        R_h2T = [Res("h2T%d" % i) for i in range(8)]
        moe_mark = A.mark()
        gm = A.alloc((D,))
        tmpr = A.alloc((D,))
        P.dma("sp", gm, mod_scr[0:1, 2 * D:3 * D].partition_broadcast(128), reads=[R_modscr], writes=[R_bc])
        P.dma("sp", tmpr, g_post1.partition_broadcast(128), writes=[R_bc])
        P.op("dve", lambda e: e.tensor_tensor(gm, gm, tmpr, ALU.mult), [], [R_bc])
        wo = A.alloc((NKC, D), BF16)
        R_wo = Res("wo")
        w_out_v = w_out.rearrange("(c p) n -> p c n", p=128)
        for q4 in range(4):
            P.dma("pool", wo[:, :, q4 * 512:(q4 + 1) * 512], w_out_v[:, :, q4 * 512:(q4 + 1) * 512], writes=[R_wo])
        og = A.alloc((D,))
        ogb = A.alloc((D,), BF16)
        oT = A.alloc((NKC, 128), BF16)
        ysb = A.alloc((D,))
        xot = A.alloc((D,))
        x1t = A.alloc((D,))
        xn2 = A.alloc((D,))
        hTf = A.alloc((NKC, 128))
        rsb = A.alloc((4,))
        rt_ = A.alloc((16, 64))
        R_og, R_ogb, R_oT, R_ysb, R_xot, R_x1t, R_xn2, R_hTf, R_rsb, R_rt = [Res() for _ in range(10)]
        R_x1scr = Res("x1scr")
        sc_, sel_, eq_, sel2_, selm_, ch_ = [rt_[:, i, :] for i in range(6)]
        m1_ = rt_[:, 6, 0:8]
        m2_ = rt_[:, 6, 8:16]
        gs_ = rt_[:, 6, 16:24]
        keep_ = rt_[:, 6, 24:32]
        kt_ = rt_[:, 6, 32:40]
        top8 = rt_[:, 7, 0:8]
        top8b = rt_[:, 7, 8:16]
        den_ = rt_[:, 7, 16:17]
        rden_ = rt_[:, 7, 17:18]

        def rms_rstd2(src, rsrc, col):
            P.op("pool", lambda e: e.memset(rsb[:, col:col + 2], 0.0), [], [R_rsb])
            P.op("act", lambda e: e.activation(xn2, src, AF.Square, accum_out=rsb[:, col:col + 1]), [rsrc], [R_xn2, R_rsb])
            P.op("act", lambda e: e.activation(rsb[:, col + 1:col + 2], rsb[:, col:col + 1], AF.Sqrt, bias=epsc[:, 0:1], scale=1.0 / D), [R_c2], [R_rsb])
            P.op("dve", lambda e: e.reciprocal(rsb[:, col:col + 1], rsb[:, col + 1:col + 2]), [], [R_rsb])

        def phase_b_tile(tt):
            tsl = slice(tt * 128, (tt + 1) * 128)
            P.op("pool", lambda e: e.indirect_dma_start(out=og, out_offset=None, in_=o_scr[:, :],
                                                        in_offset=bass.IndirectOffsetOnAxis(ap=idxt[:, tt:tt + 1], axis=0)),
                 [R_oscr, R_bc], [R_og], dma=True)
            P.dma("sp", xot, xo[tsl, :], writes=[R_xot])
            P.op("act", lambda e: e.copy(ogb, og), [R_og], [R_ogb])
            for half in range(2):
                pv = psum[0][:, half * 512:(half + 1) * 512].bitcast(BF16)
                for q in range(8):
                    kc = half * 8 + q
                    P.op("pe", lambda e, pv=pv, q=q, kc=kc: e.transpose(pv[:, q * 128:(q + 1) * 128], ogb[:, kc * 128:(kc + 1) * 128], ident_b),
                         [R_ogb, R_c2], [PR[0][half]])
                P.op("act" if half == 0 else "dve", lambda e, pv=pv, half=half: (e.copy if half == 0 else e.tensor_copy)(
                    oT[:, half * 8:(half + 1) * 8, :], pv.rearrange("p (c n) -> p c n", c=8)), [PR[0][half]], [R_oT])
            for db in range(4):
                pi, pk = 1 + db // 2, db % 2
                for kc in range(NKC):
                    P.op("pe", lambda e, kc=kc, db=db, pi=pi, pk=pk: e.matmul(pbank(pi, pk), oT[:, kc, :], wo[:, kc, db * 512:(db + 1) * 512],
                                                                            start=(kc == 0), stop=(kc == NKC - 1)), [R_oT, R_wo], [PR[pi][pk]])
                P.op("act", lambda e, db=db, pi=pi, pk=pk: e.copy(ysb[:, db * 512:(db + 1) * 512], pbank(pi, pk)), [PR[pi][pk]], [R_ysb])
            rms_rstd2(ysb, R_ysb, 0)
            P.op("dve", lambda e: e.scalar_tensor_tensor(ysb, ysb, rsb[:, 0:1], gm, ALU.mult, ALU.mult), [R_rsb, R_bc], [R_ysb])
            P.op("dve", lambda e: e.tensor_tensor(x1t, ysb, xot, ALU.add), [R_ysb, R_xot], [R_x1t])
            P.dma("sp", x1_scr[tsl, :], x1t, reads=[R_x1t], writes=[R_x1scr])
            rms_rstd2(x1t, R_x1t, 2)
            P.op("dve", lambda e: e.tensor_scalar(xn2, x1t, rsb[:, 2:3], None, ALU.mult), [R_x1t, R_rsb], [R_xn2])
            for g4 in range(4):
                pi, pk = (g4 % 2), (g4 // 2)
                for q in range(4):
                    kc = g4 * 4 + q
                    P.op("pe", lambda e, pi=pi, pk=pk, q=q, kc=kc: e.transpose(pbank(pi, pk)[:, q * 128:(q + 1) * 128], xn2[:, kc * 128:(kc + 1) * 128], ident_f),
                         [R_xn2, R_const], [PR[pi][pk]])
                sl4 = slice(g4 * 4, (g4 + 1) * 4)
                P.op("dve", lambda e, pi=pi, pk=pk, sl4=sl4: e.tensor_tensor(
                    hTf[:, sl4, :], pbank(pi, pk).rearrange("p (c n) -> p c n", c=4), bc(a2[:, sl4].unsqueeze(2), (128, 4, 128)), ALU.mult),
                    [PR[pi][pk], R_a], [R_hTf])
            P.op("pool", lambda e: e.tensor_tensor(hTf, hTf, bc(s2.unsqueeze(2), (128, NKC, 128)), ALU.add), [R_modfm], [R_hTf])
            P.op("act", lambda e: e.copy(h2T[:, :, tsl], hTf), [R_hTf], [R_h2T[tt]])
            for kc in range(NKC):
                P.op("pe", lambda e, kc=kc: e.matmul(pbank(2, 0)[:, 0:64], hTf[:, kc, :], wr[:, kc, :], start=(kc == 0), stop=(kc == NKC - 1)),
                     [R_hTf, R_bc], [PR[2][0]])
            V = lambda fn, rd=(): P.op("dve", fn, list(rd) + [R_rt, R_bc], [R_rt])
            P.op("act", lambda e: e.activation(sc_, pbank(2, 0)[:, 0:64], AF.Sigmoid), [PR[2][0]], [R_rt])
            V(lambda e: e.tensor_tensor(sel_, sc_, rb, ALU.add))
            g3 = lambda ap: ap.rearrange("p (g k) -> p g k", g=8)
            V(lambda e: e.tensor_reduce(m1_, g3(sel_), AX.X, ALU.max))
            V(lambda e: e.tensor_tensor(g3(eq_), g3(sel_), bc(m1_.unsqueeze(2), (128, 8, 8)), ALU.is_equal))
            V(lambda e: e.scalar_tensor_tensor(sel2_, eq_, -1.0e9, sel_, ALU.mult, ALU.add))
            V(lambda e: e.tensor_reduce(m2_, g3(sel2_), AX.X, ALU.max))
            V(lambda e: e.tensor_tensor(gs_, m1_, m2_, ALU.add))
            V(lambda e: e.max(top8, gs_))
            V(lambda e: e.tensor_scalar(keep_, gs_, top8[:, 3:4], None, ALU.is_ge))
            V(lambda e: e.tensor_scalar(kt_, keep_, 1.0e3, -1.0e3, ALU.mult, ALU.add))
            V(lambda e: e.tensor_tensor(g3(selm_), g3(sel_), bc(keep_.unsqueeze(2), (128, 8, 8)), ALU.mult))
            V(lambda e: e.tensor_tensor(g3(selm_), g3(selm_), bc(kt_.unsqueeze(2), (128, 8, 8)), ALU.add))
            V(lambda e: e.max(top8b, selm_))
            V(lambda e: e.tensor_scalar(ch_, selm_, top8b[:, 7:8], None, ALU.is_ge))
            V(lambda e: e.tensor_tensor(ch_, ch_, sc_, ALU.mult))
            V(lambda e: e.tensor_reduce(den_, ch_, AX.X, ALU.add))
            V(lambda e: e.reciprocal(rden_, den_))
            P.op("dve", lambda e: e.tensor_scalar(Wc[:, tt, 0:64], ch_, rden_, 2.5, ALU.mult, ALU.mult), [R_rt], [R_Wc])

        for tt in range(8):
            phase_b_tile(tt)
        if dbg == "b":
            P.dma("sp", dbg_a[:, 0:520], Wc.rearrange("p t e -> p (t e)"), reads=[R_Wc], final=True)
            P.dma("sp", dbg_a[:, 1024:1536], h2T[:, 0, :].bitcast(F32), reads=R_h2T, final=True)
            P.dma("sp", out[0:128, :], x1t, reads=[R_x1t], final=True)
            P.run()
            return nc, dbg_out
        P.barrier()
        A.release(moe_mark)


        NE = 65 if MOE_EXPERTS is None else MOE_EXPERTS
        acc = A.alloc((8, D))
        R_acc = [Res("acc%d" % i) for i in range(8)]
        P.op("pool", lambda e: e.memset(acc, 0.0), [], R_acc)
        wg = [A.alloc((NKC, 512), BF16) for _ in range(2)]
        wu = [A.alloc((NKC, 512), BF16) for _ in range(2)]
        wd = [A.alloc((4, D), BF16) for _ in range(1)]
        R_wg = [Res("wg0"), Res("wg1")]
        R_wu = [Res("wu0"), Res("wu1")]
        R_wd = [Res("wd0")]
        actT = A.alloc((4, OWN), BF16)
        R_act = [[Res("act%d_%d" % (f, tb)) for tb in range(2)] for f in range(4)]
        sgm = [A.alloc((512,), BF16) for _ in range(2)]
        R_sgm = [Res("sgm0"), Res("sgm1")]
        bcnt = [0]

        def nbank():
            n = bcnt[0] % 8
            bcnt[0] += 1
            return n // 2, n % 2

        def moe_expert(ex):
            b_ = ex % 2
            g_, u_, d_ = wg[b_], wu[b_], wd[0]
            rg_, ru_, rd_ = R_wg[b_], R_wu[b_], R_wd[0]
            P.dma("pool", g_, w_gate[ex].rearrange("(c p) n -> p c n", p=128), writes=[rg_])
            P.dma("pool", u_, w_up[ex].rearrange("(c p) n -> p c n", p=128), writes=[ru_])
            P.dma("pool", d_, w_down[ex].rearrange("(c p) n -> p c n", p=128), writes=[rd_])
            scnt = 0
            for f in range(4):
                for tb in range(2):
                    gi, gk_ = nbank()
                    ui, uk_ = nbank()
                    for kc in range(NKC):
                        P.op("pe", lambda e, kc=kc, f=f, tb=tb, gi=gi, gk_=gk_: e.matmul(
                            pbank(gi, gk_), g_[:, kc, f * 128:(f + 1) * 128], h2T[:, kc, tb * 512:(tb + 1) * 512],
                            start=(kc == 0), stop=(kc == NKC - 1)), [rg_] + R_h2T[tb * 4:(tb + 1) * 4], [PR[gi][gk_]])
                    for kc in range(NKC):
                        P.op("pe", lambda e, kc=kc, f=f, tb=tb, ui=ui, uk_=uk_: e.matmul(
                            pbank(ui, uk_), u_[:, kc, f * 128:(f + 1) * 128], h2T[:, kc, tb * 512:(tb + 1) * 512],
                            start=(kc == 0), stop=(kc == NKC - 1)), [ru_] + R_h2T[tb * 4:(tb + 1) * 4], [PR[ui][uk_]])
                    sg_ = sgm[scnt % 2]
                    rs_ = R_sgm[scnt % 2]
                    scnt += 1
                    P.op("act", lambda e, sg_=sg_, gi=gi, gk_=gk_: e.activation(sg_, pbank(gi, gk_), AF.Silu), [PR[gi][gk_]], [rs_])
                    P.op("dve", lambda e, sg_=sg_, ui=ui, uk_=uk_, f=f, tb=tb: e.tensor_tensor(
                        actT[:, f, tb * 512:(tb + 1) * 512], pbank(ui, uk_), sg_, ALU.mult), [PR[ui][uk_], rs_], [R_act[f][tb]])
            for tt in range(8):
                for db in range(4):
                    di, dk_ = nbank()
                    for f in range(4):
                        P.op("pe", lambda e, f=f, tt=tt, db=db, di=di, dk_=dk_: e.matmul(
                            pbank(di, dk_), actT[:, f, tt * 128:(tt + 1) * 128], d_[:, f, db * 512:(db + 1) * 512],
                            start=(f == 0), stop=(f == 3)), [R_act[f][tt // 4], rd_], [PR[di][dk_]])
                    P.op("dve", lambda e, tt=tt, db=db, di=di, dk_=dk_: e.scalar_tensor_tensor(
                        acc[:, tt, db * 512:(db + 1) * 512], pbank(di, dk_), Wc[:, tt, ex:ex + 1], acc[:, tt, db * 512:(db + 1) * 512],
                        ALU.mult, ALU.add), [PR[di][dk_], R_Wc], [R_acc[tt]])

        for ex in range(NE):
            moe_expert(ex)
        P.barrier()
        A.release(moe_mark)
        acc2 = A.alloc((8, D))
        gf = A.alloc((D,))
        tmpf = A.alloc((D,))
        R_gf = Res("gf")
        P.dma("sp", gf, mod_scr[0:1, 5 * D:6 * D].partition_broadcast(128), reads=[R_modscr], writes=[R_gf])
        P.dma("sp", tmpf, g_post2.partition_broadcast(128), writes=[R_gf])
        P.op("dve", lambda e: e.tensor_tensor(gf, gf, tmpf, ALU.mult), [], [R_gf])
        x1l = [A.alloc((D,)) for _ in range(2)]
        R_x1l = [Res("x1l0"), Res("x1l1")]
        junk = A.alloc((D,))
        R_junk = Res("junk")
        rsf = A.alloc((8, 2))
        R_rsf = Res("rsf")
        P.op("pool", lambda e: e.memset(rsf, 0.0), [], [R_rsf])
        for tt in range(8):
            tsl = slice(tt * 128, (tt + 1) * 128)
            xl = x1l[tt % 2]
            rxl = R_x1l[tt % 2]
            P.dma("sp", xl, x1_scr[tsl, :], reads=[R_x1scr], writes=[rxl])
            at = acc2[:, tt, :]
            P.op("act", lambda e, at=at, tt=tt: e.activation(junk, at, AF.Square, accum_out=rsf[:, tt, 0:1]), [R_acc[tt]], [R_junk, R_rsf])
            P.op("act", lambda e, tt=tt: e.activation(rsf[:, tt, 1:2], rsf[:, tt, 0:1], AF.Sqrt, bias=epsc[:, 0:1], scale=1.0 / D), [R_c2], [R_rsf])
            P.op("dve", lambda e, tt=tt: e.reciprocal(rsf[:, tt, 0:1], rsf[:, tt, 1:2]), [], [R_rsf])
            P.op("dve", lambda e, at=at, tt=tt: e.scalar_tensor_tensor(at, at, rsf[:, tt, 0:1], gf, ALU.mult, ALU.mult), [R_rsf, R_gf], [R_acc[tt]])
            P.op("dve", lambda e, at=at, xl=xl: e.tensor_tensor(xl, at, xl, ALU.add), [R_acc[tt]], [rxl])
            P.dma("sp", out[tsl, :], xl, reads=[rxl], final=True)
        P.run()
    return nc, dbg_out


W_IN_ORDER = None


def _w_in_perm():
    sp = np.cumsum([0, 1024, 1024, 1024, 1024, 8, 8, 512, 512, 1024, 1024])
    seg = lambda i: np.arange(sp[i], sp[i + 1])
    return np.concatenate([seg(0), seg(1), seg(2), seg(3), seg(6), seg(7), seg(8), seg(9), seg(4), seg(5)])


def host_inputs(inputs, core, light=False):
    b, j = core // 4, core % 4
    f = lambda a: np.ascontiguousarray(a, dtype=np.float32)
    fm = lambda v: f(np.asarray(v).reshape(NKC, 128).T)
    m = {}
    x = np.asarray(inputs["x"])
    m["xb"] = f(x[b])
    m["xo"] = f(x[b, j * OWN:(j + 1) * OWN])
    m["cT"] = fm(inputs["c"][b])
    m["pos"] = np.ascontiguousarray(np.asarray(inputs["positions"])[b].reshape(NT, 128).T.astype(np.int32))
    m["w_ada"] = f(inputs["w_ada"][0])
    m["b_ada"] = f(inputs["b_ada"][0]).reshape(1, -1)
    m["g_pre1"] = fm(inputs["pre_norm_mix"][0])
    m["g_pre2"] = fm(inputs["pre_norm_ffn"][0])
    m["g_post1"] = f(inputs["post_norm_mix"][0]).reshape(1, -1)
    m["g_post2"] = f(inputs["post_norm_ffn"][0]).reshape(1, -1)
    m["w_in"] = f(np.asarray(inputs["w_in"][0])[:, _w_in_perm()])
    cw = np.asarray(inputs["conv_w"][0])
    m["conv_w"] = f(cw.T.reshape(24, 128, 4).transpose(1, 0, 2))
    m["a_log"] = f(inputs["a_log"][0]).reshape(1, 8)
    m["dt_bias"] = f(inputs["dt_bias"][0]).reshape(1, 8)
    m["dn_norm_w"] = f(inputs["dn_norm_w"][0]).reshape(1, 128)
    m["rt_norm_w"] = f(inputs["rt_norm_w"][0]).reshape(1, 1024)
    m["w_out"] = f(inputs["w_out"][0])
    m["w_router"] = f(inputs["w_router"][0])
    m["router_bias"] = f(inputs["router_bias"][0]).reshape(1, 64)
    if not light:
      m["w_gate"] = f(np.concatenate([np.asarray(inputs["w_gate_exp"][0]), np.asarray(inputs["w_gate_sh"])], 0))
      m["w_up"] = f(np.concatenate([np.asarray(inputs["w_up_exp"][0]), np.asarray(inputs["w_up_sh"])], 0))
      m["w_down"] = f(np.concatenate([np.asarray(inputs["w_down_exp"][0]), np.asarray(inputs["w_down_sh"])], 0))
    m["own_idx"] = np.ascontiguousarray((j * OWN + np.arange(OWN)).reshape(8, 128).T.astype(np.int32))
    i = np.arange(128)
    m["c_ident"] = f(np.eye(128))
    m["c_utri"] = f(i[:, None] <= i[None, :])
    m["c_maskS"] = f(i[None, :] < i[:, None])
    m["c_maskT"] = f(i[None, :] >= i[:, None])
    oh = np.zeros((128, 8, 128), np.float32)
    for h in range(8):
        oh[h, h, :] = 1.0
    m["c_onehot8"] = oh
    lg = np.log(1.0 - 2.0 ** (-5.0 - np.arange(8, dtype=np.float64)))
    diff = (i[None, :] - i[:, None]).astype(np.float64)
    rtm = np.where(diff[:, None, :] >= 0, np.exp(np.maximum(diff[:, None, :], 0) * lg[None, :, None]), 0.0) * 0.125
    m["c_rtmask"] = f(rtm)
    m["c_gq"] = f(np.broadcast_to(np.exp((i[None, None, :] + 1.0) * lg[None, :, None]), (64, 8, 128)))
    m["c_gk"] = f(np.exp((127.0 - i[:, None]) * lg[None, :]) * 0.125)
    m["c_gC"] = f(np.broadcast_to(np.exp(128.0 * lg)[None, :], (64, 8)))
    mBDh = ((i[:, None] // 8) == (i[None, :] // 8)) & (i[None, :] < i[:, None])
    m["c_mBD"] = f(np.stack([mBDh, mBDh.T]))
    mcs = []
    for bsz in (8, 16, 32, 64):
        same = (i[:, None] // (2 * bsz)) == (i[None, :] // (2 * bsz))
        mcs.append(same & ((i[:, None] % (2 * bsz)) >= bsz) & ((i[None, :] % (2 * bsz)) < bsz))
    m["c_mC"] = f(np.stack(mcs + [x.T for x in mcs]))
    m["c_theta"] = f(1.0 / (10000.0 ** np.linspace(0.0, 1.0, 32, dtype=np.float32))).reshape(1, 32)
    return m


def kernel(**inputs):
    nc, _ = build_program(DBG)
    cores = list(range(8)) if DBG_CORES is None else DBG_CORES
    in_maps = [host_inputs(inputs, c, light=DBG not in (None, "moe")) for c in cores]
    res = run_bass_kernel_spmd(nc, in_maps, core_ids=list(range(len(cores))))
    if DBG is not None:
        return res
    outp = np.zeros((2, S, D), np.float32)
    for k, c in enumerate(cores):
        b, j = c // 4, c % 4
        outp[b, j * OWN:(j + 1) * OWN] = res.results[k]["out"]
    return outp
```
